# Optimizing a Trainium2 kernel written in Bass

```python
import math
import jax
import jax.numpy as jnp
from jax import lax
import numpy as np

D_MODEL = 2048
BATCH = 1
SEQ = 8192
DEPTH = 2

CHUNK = 64
Q_BLOCK = 128
EPS = 1e-6
SB_HEADS = 4
HEAD_DIM = 128
SB_WIDTH = SB_HEADS * HEAD_DIM
FOX_HEADS = 4
FOX_WIDTH = FOX_HEADS * HEAD_DIM
SSM_HEAD_DIM = 64
SSM_HEADS = 16
SSM_INNER = SSM_HEADS * SSM_HEAD_DIM
SSM_GROUPS = 2
SSM_STATE = 128
CONV_WIDTH = 4
SSM_CONV_DIM = SSM_INNER + 2 * SSM_GROUPS * SSM_STATE
N_BRANCH = 3
IN_PROJ_SIZES = (SB_WIDTH, SB_WIDTH, SB_WIDTH,
                 FOX_WIDTH, FOX_WIDTH, FOX_WIDTH, FOX_HEADS,
                 SSM_INNER, SSM_CONV_DIM, SSM_HEADS,
                 N_BRANCH * D_MODEL)
IN_PROJ_WIDTH = sum(IN_PROJ_SIZES)
N_EXPERTS = 16
N_EXPERT_GROUPS = 4
EXPERTS_PER_GROUP = N_EXPERTS // N_EXPERT_GROUPS
TOPK_GROUPS = 1
TOP_K = 2
D_FF_EXPERT = 1024

kernel_name = 'hybrid_sb_fox_ssd_moe_encoder'


def rms_norm(x, gain):
    xf = x.astype(jnp.float32)
    y = xf * lax.rsqrt(jnp.mean(xf * xf, axis=-1, keepdims=True) + EPS)
    return (y * gain.astype(jnp.float32)).astype(x.dtype)


def split_heads(u, n_heads):
    b, s, _ = u.shape
    return u.reshape(b, s, n_heads, -1)


def query_blocks(u):
    b, s = u.shape[:2]
    u = u.reshape((b, s // Q_BLOCK, Q_BLOCK) + u.shape[2:])
    return jnp.moveaxis(u, 1, 0)


def merge_query_blocks(u):
    u = jnp.moveaxis(u, 0, 1)
    return u.reshape((u.shape[0], -1) + u.shape[3:])


def stick_breaking_attention(q, k, v):
    s_len, d = q.shape[1], q.shape[-1]
    scale = d ** -0.5
    k_pos = jnp.arange(s_len)

    def block(args):
        q_blk, blk = args
        q_pos = blk * Q_BLOCK + jnp.arange(Q_BLOCK)
        z = jnp.einsum('bqhd,bkhd->bhqk', q_blk, k).astype(jnp.float32) * scale
        past = k_pos[None, :] < q_pos[:, None]
        log_keep = jnp.where(past, jax.nn.log_sigmoid(-z), 0.0)
        suffix = lax.cumsum(log_keep, axis=3, reverse=True)
        between = jnp.concatenate([suffix[..., 1:], jnp.zeros_like(suffix[..., :1])], axis=-1)
        weight = jnp.where(past, jnp.exp(jax.nn.log_sigmoid(z) + between), 0.0)
        return jnp.einsum('bhqk,bkhd->bqhd', weight.astype(v.dtype), v)

    n_blk = s_len // Q_BLOCK
    out = lax.map(block, (query_blocks(q), jnp.arange(n_blk)))
    return merge_query_blocks(out)


def forgetting_attention(q, k, v, log_f):
    s_len, d = q.shape[1], q.shape[-1]
    scale = d ** -0.5
    k_pos = jnp.arange(s_len)
    f_cum = lax.cumsum(log_f.astype(jnp.float32), axis=1)
    f_key = jnp.swapaxes(f_cum, 1, 2)[:, :, None, :]

    def block(args):
        q_blk, fq_blk, blk = args
        q_pos = blk * Q_BLOCK + jnp.arange(Q_BLOCK)
        logits = (jnp.einsum('bqhd,bkhd->bhqk', q_blk, k).astype(jnp.float32) * scale
                  + jnp.swapaxes(fq_blk, 1, 2)[..., None] - f_key)
        mask = k_pos[None, :] <= q_pos[:, None]
        p = jax.nn.softmax(jnp.where(mask, logits, -jnp.inf), axis=-1)
        return jnp.einsum('bhqk,bkhd->bqhd', p.astype(v.dtype), v)

    n_blk = s_len // Q_BLOCK
    out = lax.map(block, (query_blocks(q), query_blocks(f_cum), jnp.arange(n_blk)))
    return merge_query_blocks(out)


def causal_depthwise_conv(u, w, bias):
    width, ch = w.shape
    out = lax.conv_general_dilated(u, w[:, None, :], window_strides=(1,),
                                   padding=[(width - 1, 0)],
                                   dimension_numbers=('NWC', 'WIO', 'NWC'),
                                   feature_group_count=ch)
    return out + bias


def ssd_chunked(x, dt, a, b_mat, c_mat):
    bsz, s_len, n_h, p = x.shape
    g, n = b_mat.shape[-2:]
    hg = n_h // g
    nc = s_len // CHUNK
    xc = (x * dt[..., None]).reshape(bsz, nc, CHUNK, g, hg, p)
    log_a = (dt * a).reshape(bsz, nc, CHUNK, g, hg)
    bc = b_mat.reshape(bsz, nc, CHUNK, g, n)
    cc = c_mat.reshape(bsz, nc, CHUNK, g, n)
    a_cum = lax.cumsum(log_a, axis=2)
    idx = jnp.arange(CHUNK)
    causal = (idx[:, None] >= idx[None, :])[:, :, None, None]
    seg = a_cum[:, :, :, None] - a_cum[:, :, None, :]
    decay = jnp.exp(jnp.where(causal, seg, -jnp.inf))
    cb = jnp.einsum('bclgn,bcsgn->bclsg', cc, bc)
    y_diag = jnp.einsum('bclsg,bclsgh,bcsghp->bclghp', cb, decay, xc)
    to_end = jnp.exp(a_cum[:, :, -1:] - a_cum)
    states = jnp.einsum('bclgn,bclgh,bclghp->bcghpn', bc, to_end, xc)
    chunk_decay = jnp.exp(a_cum[:, :, -1])

    def step(h, inp):
        st, dec = inp
        return h * dec[..., None, None] + st, h

    h0 = jnp.zeros_like(states[:, 0])
    _, h_in = lax.scan(step, h0, (jnp.moveaxis(states, 1, 0), jnp.moveaxis(chunk_decay, 1, 0)))
    h_in = jnp.moveaxis(h_in, 0, 1)
    y_off = jnp.einsum('bclgn,bcghpn,bclgh->bclghp', cc, h_in, jnp.exp(a_cum))
    return (y_diag + y_off).reshape(bsz, s_len, n_h, p)


def mamba2_mixer(z, xbc, dt_raw, conv_w, conv_b, dt_bias, a_log, d_skip, g_norm):
    b, s, _ = z.shape
    xbc = jax.nn.silu(causal_depthwise_conv(xbc, conv_w, conv_b))
    xs, b_mat, c_mat = jnp.split(xbc, [SSM_INNER, SSM_INNER + SSM_GROUPS * SSM_STATE], axis=-1)
    x_h = xs.reshape(b, s, SSM_HEADS, SSM_HEAD_DIM)
    dt = jax.nn.softplus((dt_raw + dt_bias).astype(jnp.float32))
    a = -jnp.exp(a_log.astype(jnp.float32))
    y = ssd_chunked(x_h, dt, a,
                    b_mat.reshape(b, s, SSM_GROUPS, SSM_STATE),
                    c_mat.reshape(b, s, SSM_GROUPS, SSM_STATE))
    y = y + d_skip.astype(jnp.float32)[:, None] * x_h
    y = y.reshape(b, s, SSM_INNER).astype(z.dtype)
    return rms_norm(y * jax.nn.silu(z), g_norm)


def grouped_moe(h, w_router, b_router, w_gate, w_up, w_down):
    b, s, d = h.shape
    t = h.reshape(b * s, d)
    n_tok = t.shape[0]
    affinity = jax.nn.sigmoid((t @ w_router).astype(jnp.float32))
    sel = (affinity + b_router.astype(jnp.float32)).reshape(n_tok, N_EXPERT_GROUPS, EXPERTS_PER_GROUP)
    group_score = lax.top_k(sel, 2)[0].sum(-1)
    _, g_idx = lax.top_k(group_score, TOPK_GROUPS)
    group_mask = jax.nn.one_hot(g_idx, N_EXPERT_GROUPS, dtype=jnp.float32).sum(1) > 0
    masked = jnp.where(group_mask[:, :, None], sel, -jnp.inf).reshape(n_tok, N_EXPERTS)
    _, e_idx = lax.top_k(masked, TOP_K)
    w_sel = jnp.take_along_axis(affinity, e_idx, axis=-1)
    w_sel = w_sel / jnp.sum(w_sel, axis=-1, keepdims=True)
    combine = jnp.sum(jax.nn.one_hot(e_idx, N_EXPERTS, dtype=jnp.float32) * w_sel[..., None], axis=1)
    combine = combine.astype(t.dtype)
    y = jnp.zeros_like(t)
    for e in range(N_EXPERTS):
        act = jax.nn.silu(t @ w_gate[e]) * (t @ w_up[e])
        y = y + combine[:, e:e + 1] * (act @ w_down[e])
    return y.reshape(b, s, d)


def setup_inputs(seed: int = 0) -> dict:
    key = jax.random.key(seed)
    ks = iter(jax.random.split(key, 40))
    L, D, E, F = DEPTH, D_MODEL, N_EXPERTS, D_FF_EXPERT

    def nrm(shape, scale):
        return jax.random.normal(next(ks), shape, jnp.float32) * scale

    x = nrm((BATCH, SEQ, D), 1.0)
    c = nrm((BATCH, D), 1.0)
    w_ada = nrm((L, D, 6 * D), 0.5 * D ** -0.5)
    b_ada = nrm((L, 6 * D), 0.02)
    g_norm_mix = 1.0 + nrm((L, D), 0.02)
    w_in = nrm((L, D, IN_PROJ_WIDTH), D ** -0.5)
    b_fgate = 3.0 + nrm((L, FOX_HEADS), 0.5)
    g_q_fox = 1.0 + nrm((L, HEAD_DIM), 0.02)
    g_k_fox = 1.0 + nrm((L, HEAD_DIM), 0.02)
    conv_w = nrm((L, CONV_WIDTH, SSM_CONV_DIM), CONV_WIDTH ** -0.5)
    conv_b = nrm((L, SSM_CONV_DIM), 0.02)
    dt0 = jnp.exp(jax.random.uniform(next(ks), (L, SSM_HEADS), jnp.float32,
                                     math.log(1e-3), math.log(1e-1)))
    dt_bias = dt0 + jnp.log(-jnp.expm1(-dt0))
    a_log = jnp.log(jax.random.uniform(next(ks), (L, SSM_HEADS), jnp.float32, 1.0, 16.0))
    d_skip = 1.0 + nrm((L, SSM_HEADS), 0.02)
    g_ssm_norm = 1.0 + nrm((L, SSM_INNER), 0.02)
    w_branch_sb = nrm((L, SB_WIDTH, D), SB_WIDTH ** -0.5)
    w_branch_fox = nrm((L, FOX_WIDTH, D), FOX_WIDTH ** -0.5)
    w_branch_ssm = nrm((L, SSM_INNER, D), SSM_INNER ** -0.5)
    w_out = nrm((L, D, D), D ** -0.5)
    g_norm_ffn = 1.0 + nrm((L, D), 0.02)
    w_router = nrm((D, E), D ** -0.5)
    b_router = nrm((E,), 0.01)
    w_e_gate = nrm((L, E, D, F), D ** -0.5)
    w_e_up = nrm((L, E, D, F), D ** -0.5)
    w_e_down = nrm((L, E, F, D), F ** -0.5)
    return {'x': x, 'c': c, 'w_ada': w_ada, 'b_ada': b_ada, 'g_norm_mix': g_norm_mix,
            'w_in': w_in, 'b_fgate': b_fgate, 'g_q_fox': g_q_fox, 'g_k_fox': g_k_fox,
            'conv_w': conv_w, 'conv_b': conv_b, 'dt_bias': dt_bias, 'a_log': a_log,
            'd_skip': d_skip, 'g_ssm_norm': g_ssm_norm, 'w_branch_sb': w_branch_sb,
            'w_branch_fox': w_branch_fox, 'w_branch_ssm': w_branch_ssm, 'w_out': w_out,
            'g_norm_ffn': g_norm_ffn, 'w_router': w_router, 'b_router': b_router,
            'w_e_gate': w_e_gate, 'w_e_up': w_e_up, 'w_e_down': w_e_down}


def reference(x, c, w_ada, b_ada, g_norm_mix, w_in, b_fgate, g_q_fox, g_k_fox,
              conv_w, conv_b, dt_bias, a_log, d_skip, g_ssm_norm, w_branch_sb,
              w_branch_fox, w_branch_ssm, w_out, g_norm_ffn, w_router, b_router,
              w_e_gate, w_e_up, w_e_down):
    b, s, d = x.shape
    offsets = []
    acc = 0
    for size in IN_PROJ_SIZES[:-1]:
        acc += size
        offsets.append(acc)
    cond = jax.nn.silu(c)
    for layer in range(DEPTH):
        mod = (cond @ w_ada[layer] + b_ada[layer])[:, None, :]
        shift_m, scale_m, gate_m, shift_f, scale_f, gate_f = jnp.split(mod, 6, axis=-1)

        h = rms_norm(x, g_norm_mix[layer]) * (1.0 + scale_m) + shift_m
        (sb_q, sb_k, sb_v, fx_q, fx_k, fx_v, fx_f,
         ssm_z, ssm_xbc, ssm_dt, gate_logits) = jnp.split(h @ w_in[layer], offsets, axis=-1)

        o_sb = stick_breaking_attention(split_heads(sb_q, SB_HEADS), split_heads(sb_k, SB_HEADS),
                                        split_heads(sb_v, SB_HEADS)).reshape(b, s, SB_WIDTH)
        log_f = jax.nn.log_sigmoid((fx_f + b_fgate[layer]).astype(jnp.float32))
        o_fox = forgetting_attention(rms_norm(split_heads(fx_q, FOX_HEADS), g_q_fox[layer]),
                                     rms_norm(split_heads(fx_k, FOX_HEADS), g_k_fox[layer]),
                                     split_heads(fx_v, FOX_HEADS), log_f).reshape(b, s, FOX_WIDTH)
        o_ssm = mamba2_mixer(ssm_z, ssm_xbc, ssm_dt, conv_w[layer], conv_b[layer],
                             dt_bias[layer], a_log[layer], d_skip[layer], g_ssm_norm[layer])

        gates = jax.nn.sigmoid(gate_logits).reshape(b, s, N_BRANCH, d)
        merged = (gates[:, :, 0] * (o_sb @ w_branch_sb[layer])
                  + gates[:, :, 1] * (o_fox @ w_branch_fox[layer])
                  + gates[:, :, 2] * (o_ssm @ w_branch_ssm[layer]))
        x = x + gate_m * (merged @ w_out[layer])

        h = rms_norm(x, g_norm_ffn[layer]) * (1.0 + scale_f) + shift_f
        x = x + gate_f * grouped_moe(h, w_router, b_router, w_e_gate[layer],
                                     w_e_up[layer], w_e_down[layer])
    return x
```

```python
from contextlib import ExitStack
import numpy as np
import concourse.bass as bass
import concourse.mybir as mybir
from concourse.bass_utils import run_bass_kernel_spmd

F32 = mybir.dt.float32
BF16 = mybir.dt.bfloat16
AF = mybir.ActivationFunctionType
ALU = mybir.AluOpType
AX = mybir.AxisListType

NCORES = 8
D = 2048
S = 8192
TPC = S // NCORES
EPS = 1e-6
NEXP = 16
FF = 1024


class Dep:
    __slots__ = ("lw", "rd", "dsem", "dcount", "name")

    def __init__(self, name=""):
        self.lw = None
        self.rd = []
        self.dsem = None
        self.dcount = 0
        self.name = name


class Tile:
    def __init__(self, t, name):
        self.t = t
        self.d = Dep(name)

    def __getitem__(self, k):
        return self.t[k]


class EngState:
    def __init__(self, e, sem, name):
        self.e = e
        self.sem = sem
        self.count = 0
        self.name = name
        self.seen = {}


class Ctx:
    def __init__(self, nc, es):
        self.nc = nc
        self.es = es
        self.engs = {}
        for nm, e in (("pe", nc.tensor), ("act", nc.scalar), ("dve", nc.vector), ("pool", nc.gpsimd), ("sp", nc.sync)):
            self.engs[nm] = EngState(e, es.enter_context(nc.semaphore("s_" + nm)), nm)
        self.outdeps = []
        self.nsem = 5

    def sb(self, name, shape, dtype):
        return Tile(self.es.enter_context(self.nc.sbuf_tensor("t_" + name, list(shape), dtype)), name)

    def ps(self, name, shape=(128, 512), dtype=F32):
        return Tile(self.es.enter_context(self.nc.psum_tensor("p_" + name, list(shape), dtype)), name)

    def _collect(self, E, reads, writes):
        need = []
        for d in reads:
            if d.lw is not None:
                need.append(d.lw)
        for d in writes:
            if d.lw is not None:
                need.append(d.lw)
            need.extend(d.rd)
        for src in need:
            if src[0] == "eng":
                _, en, ticket = src
                if en == E.name and en in ("pe", "sp"):
                    continue
                F = self.engs[en]
                assert ticket <= F.count, f"wait on pending ticket {en} {ticket} > {F.count}"
                key = "e_" + en
                if E.seen.get(key, 0) >= ticket:
                    continue
                E.e.wait_ge(F.sem, ticket)
                E.seen[key] = ticket
            else:
                _, dd, cnt = src
                key = id(dd)
                if E.seen.get(key, 0) >= cnt:
                    continue
                E.e.wait_ge(dd.dsem, 16 * cnt)
                E.seen[key] = cnt

    def op(self, eng, fn, reads=(), writes=(), inc=True):
        E = self.engs[eng]
        reads = [r.d if isinstance(r, Tile) else r for r in reads]
        writes = [w.d if isinstance(w, Tile) else w for w in writes]
        self._collect(E, reads, writes)
        inst = fn(E.e)
        if inc:
            inst.then_inc(E.sem, 1)
            E.count += 1
            ticket = E.count
        else:
            ticket = E.count + 1
        rec = ("eng", eng, ticket)
        for d in writes:
            d.lw = rec
            d.rd = []
        for d in reads:
            d.rd.append(rec)
        return inst

    def dma(self, queue, out, in_, dep, load, **kw):
        Q = self.engs[queue]
        dep = dep.d if isinstance(dep, Tile) else dep
        if dep.dsem is None:
            dep.dsem = self.es.enter_context(self.nc.semaphore("d%d" % self.nsem))
            self.nsem += 1
        if load:
            self._collect(Q, [], [dep])
        else:
            self._collect(Q, [dep], [])
        inst = Q.e.dma_start(out=out, in_=in_, **kw)
        inst.then_inc(dep.dsem, 16)
        dep.dcount += 1
        rec = ("dma", dep, dep.dcount)
        if load:
            dep.lw = rec
            dep.rd = []
        else:
            dep.rd.append(rec)
            if dep not in self.outdeps:
                self.outdeps.append(dep)
        return inst

    def finish(self):
        E = self.engs["sp"]
        for dep in self.outdeps:
            E.e.wait_ge(dep.dsem, 16 * dep.dcount)


def act(cx, out, in_, func, reads, writes, eng="act", **kw):
    return cx.op(eng, lambda e: e.activation(out=out, in_=in_, func=func, **kw), reads, writes)


MCOLS = 6 * D // NCORES


def build_M():
    nc = bass.Bass("TRN2", target_bir_lowering=False)
    c_in = nc.dram_tensor("c", [128, 16], F32, kind="ExternalInput").ap()
    w = nc.dram_tensor("w", [2, 16, 128, MCOLS], F32, kind="ExternalInput").ap()
    b = nc.dram_tensor("b", [1, 2 * MCOLS], F32, kind="ExternalInput").ap()
    o = nc.dram_tensor("o", [1, 2 * MCOLS], F32, kind="ExternalOutput").ap()
    with ExitStack() as es:
        cx = Ctx(nc, es)
        ct = cx.sb("ct", [128, 16], F32)
        cond = cx.sb("cond", [128, 16], F32)
        bt = cx.sb("bt", [1, 2 * MCOLS], F32)
        ot = cx.sb("ot", [1, 2 * MCOLS], F32)
        wsl = [cx.sb("w%d" % i, [128, MCOLS], F32) for i in range(4)]
        pss = [cx.ps("ps%d" % i) for i in range(3)]
        cx.dma("sp", ct[:], c_in, ct, True)
        cx.dma("sp", bt[:], b, bt, True)
        act(cx, cond[:], ct[:], AF.Silu, [ct], [cond])
        i = 0
        for l in range(2):
            for kc in range(16):
                ws = wsl[i % 4]
                i += 1
                cx.dma("sp" if i % 2 else "pool", ws[:], w[l, kc], ws, True)
                for n in range(3):
                    cx.op("pe", lambda e, n=n, ws=ws, kc=kc: e.matmul(pss[n][0:1, :], lhsT=cond[:, kc:kc + 1], rhs=ws[:, n * 512:(n + 1) * 512],
                                                                     start=(kc == 0), stop=(kc == 15)),
                          [cond, ws], [pss[n]], inc=True)
            for n in range(3):
                sl = slice(l * MCOLS + n * 512, l * MCOLS + (n + 1) * 512)
                cx.op("dve", lambda e, n=n, sl=sl: e.tensor_tensor(out=ot[0:1, sl], in0=pss[n][0:1, :], in1=bt[0:1, sl], op=ALU.add),
                      [pss[n], bt], [ot])
        cx.dma("sp", o, ot[:], ot, False)
        cx.finish()
    return nc


def run_M(c, w_ada, b_ada):
    nc = build_M()
    c_l = np.ascontiguousarray(c.reshape(16, 128).T)
    in_maps = []
    for k in range(NCORES):
        sl = slice(k * MCOLS, (k + 1) * MCOLS)
        in_maps.append({
            "c": c_l,
            "w": np.ascontiguousarray(w_ada[:, :, sl].reshape(2, 16, 128, MCOLS)),
            "b": np.ascontiguousarray(b_ada[:, sl].reshape(1, 2 * MCOLS)),
        })
    res = run_bass_kernel_spmd(nc, in_maps, core_ids=list(range(NCORES)))
    mod = np.zeros((2, 6 * D), np.float32)
    for k in range(NCORES):
        r = res.results[k]["o"].reshape(2, MCOLS)
        mod[:, k * MCOLS:(k + 1) * MCOLS] = r
    return mod


NFC = 93
A_KIND = (["copy"] * 12 + ["qkn"] * 8 + ["copy"] * 4 + ["silu"] * 8 + ["f32"] * 12 + ["sig"] * 48 + ["small"])
OB_ROWS = (24 + 8 + 48) * 128
OF_ROWS = 12 * 128 + 64


def a_col_index():
    idx = -np.ones(NFC * 128, np.int64)
    idx[0:3072] = np.arange(0, 3072)
    idx[3072:3072 + 1024] = np.arange(3076, 3076 + 1024)
    idx[4096:4096 + 1536] = np.arange(4100, 4100 + 1536)
    idx[5632:5632 + 6144] = np.arange(5652, 5652 + 6144)
    base = 92 * 128
    idx[base:base + 4] = np.arange(3072, 3076)
    idx[base + 32:base + 48] = np.arange(5636, 5652)
    return idx


def build_A():
    nc = bass.Bass("TRN2", target_bir_lowering=False)
    T = TPC
    xT = nc.dram_tensor("xT", [16, 128, T], F32, kind="ExternalInput").ap()
    w = nc.dram_tensor("w", [NFC, 128, 16 * 128], F32, kind="ExternalInput").ap()
    vec = nc.dram_tensor("vec", [128, 64], F32, kind="ExternalInput").ap()
    ones_d = nc.dram_tensor("ones", [128, 128], F32, kind="ExternalInput").ap()
    ob = nc.dram_tensor("ob", [OB_ROWS // 128, 128, T], BF16, kind="ExternalOutput").ap()
    of = nc.dram_tensor("of", [OF_ROWS, T], F32, kind="ExternalOutput").ap()
    with ExitStack() as es:
        cx = Ctx(nc, es)
        xt = cx.sb("xt", [128, 16, T], F32)
        hT = cx.sb("hT", [128, 16, T], BF16)
        vt = cx.sb("vt", [128, 64], F32)
        gs = cx.sb("gs", [128, 16], F32)
        nsb = cx.sb("nsb", [128, 1], F32)
        ones = cx.sb("onesf", [128, 128], F32)
        sq = [cx.sb("sq%d" % i, [128, T], F32) for i in range(2)]
        rstd = cx.sb("rstd", [128, T], F32)
        tmp = [cx.sb("tmp%d" % i, [128, T], F32) for i in range(2)]
        wsl = [cx.sb("w%d" % i, [128, 16 * 128], BF16) for i in range(4)]
        stb = [cx.sb("stb%d" % i, [128, T], BF16) for i in range(3)]
        stf = [cx.sb("stf%d" % i, [128, T], F32) for i in range(2)]
        ps = [cx.ps("ps%d" % i) for i in range(8)]
        xdeps = [Dep("x%d" % k) for k in range(16)]
        hdeps = [Dep("h%d" % k) for k in range(16)]

        cx.dma("sp", vt[:], vec, vt, True)
        cx.dma("sp", ones[:], ones_d, ones, True)
        for kc in range(16):
            cx.dma("sp" if kc % 2 else "act", xt[:, kc, :], xT[kc], xdeps[kc], True)
        def wload(fc):
            cx.dma("pool", wsl[fc % 4][:], w[fc], wsl[fc % 4], True)
        for fc in range(4):
            wload(fc)
        cx.op("dve", lambda e: e.scalar_tensor_tensor(out=gs[:], in0=vt[:, 16:32], scalar=1.0, in1=vt[:, 0:16], op0=ALU.add, op1=ALU.mult), [vt], [gs])
        cx.op("dve", lambda e: e.tensor_scalar(out=nsb[:], in0=vt[:, 50:51], scalar1=-1.0, scalar2=None, op0=ALU.mult), [vt], [nsb])
        for kc in range(16):
            s = sq[kc % 2]
            act(cx, s[:], xt[:, kc, :], AF.Square, [xdeps[kc]], [s])
            for tt in range(2):
                cx.op("pe", lambda e, s=s, tt=tt, kc=kc: e.matmul(ps[tt][:, :], lhsT=ones[:], rhs=s[:, tt * 512:(tt + 1) * 512], start=(kc == 0), stop=(kc == 15)),
                      [ones, s], [ps[tt]])
        for tt in range(2):
            act(cx, rstd[:, tt * 512:(tt + 1) * 512], ps[tt][:, :], AF.Sqrt, [ps[tt]], [rstd], scale=1.0 / D, bias=EPS)
        cx.op("dve", lambda e: e.reciprocal(out=rstd[:], in_=rstd[:]), [rstd], [rstd])
        for kc in range(16):
            tm = tmp[kc % 2]
            cx.op("pool", lambda e, kc=kc, tm=tm: e.tensor_tensor(out=tm[:], in0=xt[:, kc, :], in1=rstd[:], op=ALU.mult), [xdeps[kc], rstd], [tm])
            cx.op("dve", lambda e, kc=kc, tm=tm: e.tensor_scalar(out=hT[:, kc, :], in0=tm[:], scalar1=gs[:, kc:kc + 1], scalar2=vt[:, 32 + kc:33 + kc],
                                                                 op0=ALU.mult, op1=ALU.add), [tm, gs, vt], [hdeps[kc]])
        nb = 0
        nf = 0
        pi = 0
        for fc in range(NFC):
            kind = A_KIND[fc]
            ws = wsl[fc % 4]
            M = 64 if kind == "small" else 128
            pts = []
            for tt in range(2):
                p = ps[pi % 8]
                pi += 1
                pts.append(p)
                for kc in range(16):
                    cx.op("pe", lambda e, p=p, kc=kc, tt=tt, ws=ws, M=M: e.matmul(p[0:M, :], lhsT=ws[:, kc * 128:kc * 128 + M], rhs=hT[:, kc, tt * 512:(tt + 1) * 512],
                                                                                 start=(kc == 0), stop=(kc == 15)),
                          [ws, hdeps[kc]], [p], inc=(kc == 15))
            if fc + 4 < NFC:
                wload(fc + 4)
            if kind in ("copy", "silu", "sig", "qkn"):
                sb_ = stb[nb % 3]
                for tt in range(2):
                    p = pts[tt]
                    sl = slice(tt * 512, (tt + 1) * 512)
                    if kind == "copy":
                        if tt == 0:
                            act(cx, sb_[:, sl], p[:, :], AF.Copy, [p], [sb_])
                        else:
                            cx.op("dve", lambda e, p=p, sl=sl, sb_=sb_: e.tensor_copy(out=sb_[:, sl], in_=p[:, :]), [p], [sb_])
                    elif kind == "silu":
                        act(cx, sb_[:, sl], p[:, :], AF.Silu, [p], [sb_])
                    elif kind == "sig":
                        act(cx, sb_[:, sl], p[:, :], AF.Sigmoid, [p], [sb_])
                    else:
                        s = sq[tt]
                        p2 = ps[pi % 8]
                        pi += 1
                        act(cx, s[:, 0:512], p[:, :], AF.Square, [p], [s])
                        cx.op("pe", lambda e, s=s, p2=p2: e.matmul(p2[:, :], lhsT=ones[:], rhs=s[:, 0:512], start=True, stop=True), [ones, s], [p2])
                        act(cx, s[:, 512:1024], p2[:, :], AF.Sqrt, [p2], [s], scale=1.0 / 128, bias=EPS)
                        cx.op("dve", lambda e, s=s: e.reciprocal(out=s[:, 512:1024], in_=s[:, 512:1024]), [s], [s])
                        gcol = 48 if fc < 16 else 49
                        cx.op("dve", lambda e, s=s, p=p, sl=sl, sb_=sb_, gcol=gcol: e.scalar_tensor_tensor(out=sb_[:, sl], in0=p[:, :], scalar=vt[:, gcol:gcol + 1], in1=s[:, 512:1024],
                                                                                                     op0=ALU.mult, op1=ALU.mult), [p, s, vt], [sb_])
                cx.dma("sp", ob[nb], sb_[:], sb_, False)
                nb += 1
            elif kind == "f32":
                sf = stf[nf % 2]
                for tt in range(2):
                    p = pts[tt]
                    sl = slice(tt * 512, (tt + 1) * 512)
                    if tt == 0:
                        act(cx, sf[:, sl], p[:, :], AF.Copy, [p], [sf])
                    else:
                        cx.op("dve", lambda e, p=p, sl=sl, sf=sf: e.tensor_copy(out=sf[:, sl], in_=p[:, :]), [p], [sf])
                cx.dma("sp", of[nf * 128:(nf + 1) * 128, :], sf[:], sf, False)
                nf += 1
            else:
                sf = stf[nf % 2]
                for tt in range(2):
                    p = pts[tt]
                    sl = slice(tt * 512, (tt + 1) * 512)
                    act(cx, sf[0:32, sl], p[0:32, :], AF.Exp, [p], [sf], scale=-1.0, bias=nsb[0:32, :])
                    act(cx, sf[32:64, sl], p[32:64, :], AF.Exp, [p], [sf], scale=1.0, bias=vt[32:64, 50:51])
                    act(cx, sf[0:64, sl], sf[0:64, sl], AF.Ln, [sf], [sf], scale=1.0, bias=1.0)
                    cx.op("dve", lambda e, sf=sf, sl=sl: e.tensor_scalar(out=sf[0:32, sl], in0=sf[0:32, sl], scalar1=-1.0, scalar2=None, op0=ALU.mult), [sf], [sf])
                cx.dma("sp", of[12 * 128:12 * 128 + 64, :], sf[0:64, :], sf, False)
        cx.finish()
    return nc


_A_CACHE = {}


def run_A(x_tok, w_in_l, mod_l, g_norm_l, b_fgate_l, g_q_l, g_k_l, dt_bias_l):
    if "nc" not in _A_CACHE:
        _A_CACHE["nc"] = build_A()
    nc = _A_CACHE["nc"]
    idx = a_col_index()
    wp = np.zeros((D, NFC * 128), np.float32)
    wp[:, idx >= 0] = w_in_l[:, idx[idx >= 0]]
    wp = np.ascontiguousarray(wp.reshape(16, 128, NFC, 128).transpose(2, 1, 0, 3)).reshape(NFC, 128, 16 * 128)
    vec = np.zeros((128, 64), np.float32)
    vec[:, 0:16] = g_norm_l.reshape(16, 128).T
    vec[:, 16:32] = mod_l[D:2 * D].reshape(16, 128).T
    vec[:, 32:48] = mod_l[0:D].reshape(16, 128).T
    vec[:, 48] = g_q_l
    vec[:, 49] = g_k_l
    vec[0:4, 50] = b_fgate_l
    vec[32:48, 50] = dt_bias_l
    ones = np.ones((128, 128), np.float32)
    in_maps = []
    for k in range(NCORES):
        xs = x_tok[k * TPC:(k + 1) * TPC, :]
        in_maps.append({"xT": np.ascontiguousarray(xs.T).reshape(16, 128, TPC), "w": wp, "vec": vec, "ones": ones})
    res = run_bass_kernel_spmd(nc, in_maps, core_ids=list(range(NCORES)))
    ob = np.concatenate([np.asarray(res.results[k]["ob"]).reshape(OB_ROWS, TPC) for k in range(NCORES)], axis=1)
    of = np.concatenate([np.asarray(res.results[k]["of"]) for k in range(NCORES)], axis=1)
    return ob, of


NQB = 32
NKB = 64
SCALE = 128 ** -0.5
NEG = -30000.0


def _barrier(cx):
    names = ["pe", "act", "dve", "pool", "sp"]
    for a in names:
        E = cx.engs[a]
        for b in names:
            if a == b:
                continue
            F = cx.engs[b]
            if F.count > 0 and E.seen.get("e_" + b, 0) < F.count:
                E.e.wait_ge(F.sem, F.count)
                E.seen["e_" + b] = F.count


def build_B():
    nc = bass.Bass("TRN2", target_bir_lowering=False)
    dr = lambda n, s, dt, k="ExternalInput": nc.dram_tensor(n, list(s), dt, kind=k).ap()
    sqT_d = dr("sqT", [128, NQB * 128], BF16)
    skT_d = dr("skT", [128, S], BF16)
    sv_d = dr("sv", [128, NKB, 128], BF16)
    fqT_d = dr("fqT", [128, NQB * 128], BF16)
    fkT_d = dr("fkT", [128, S], BF16)
    fv_d = dr("fv", [128, NKB, 130], BF16)
    logf_d = dr("logf", [128, NKB], F32)
    mf_d = dr("mf", [128, 4 * 128], F32)
    cst_d = dr("cst", [128, 5 * 128], F32)
    sv_small_d = dr("small", [128, 32], F32)
    raw_d = dr("raw", [3, 128, S], F32)
    dt_d = dr("dt", [128, NKB * 2], F32)
    zs_d = dr("zs", [128, NKB, 128], BF16)
    osb_d = dr("osb", [128, NQB * 128], BF16, "ExternalOutput")
    ofx_d = dr("ofx", [128, NQB, 128], BF16, "ExternalOutput")
    yss_d = dr("yss", [128, NKB, 128], F32, "ExternalOutput")
    with ExitStack() as es:
        cx = Ctx(nc, es)
        cst = cx.sb("cst", [128, 5 * 128], F32)
        tri_s = cst[:, 0:128]
        tri_i = cst[:, 128:256]
        ones = cst[:, 256:384]
        ident = cst[:, 384:512]
        negm = cst[:, 512:640]
        mf = cx.sb("mf", [128, 4 * 128], F32)
        mb = cx.sb("mb", [128, 4 * 128], BF16)
        identb = cx.sb("identb", [128, 128], BF16)
        sm = cx.sb("sm", [128, 32], F32)
        ps = [cx.ps("ps%d" % i) for i in range(7)]
        psb = cx.ps("psb", [128, 1024], BF16)
        cx.dma("sp", cst[:], cst_d, cst, True)
        cx.dma("sp", mf[:], mf_d, mf, True)
        cx.dma("sp", sm[:], sv_small_d, sm, True)
        cx.op("dve", lambda e: e.tensor_copy(out=mb[:], in_=mf[:]), [mf], [mb])
        cx.op("dve", lambda e: e.tensor_copy(out=identb[:], in_=ident), [cst], [identb])

        with ExitStack() as es2:
            outer = cx.es
            cx.es = es2
            raw = cx.sb("raw", [128, S + 3], F32)
            acc = cx.sb("acc", [128, S], F32)
            xs = cx.sb("xs", [128, S], F32)
            BT = cx.sb("BT", [128, S], BF16)
            CT = cx.sb("CT", [128, S], BF16)
            dt = cx.sb("dt", [128, NKB * 2], F32)
            dta = cx.sb("dta", [128, NKB * 2], F32)
            nacum = cx.sb("nacum", [128, NKB * 2], F32)
            zs = cx.sb("zs", [128, NKB, 128], BF16)
            aneg = cx.sb("aneg", [128, 2], F32)
            hT = [cx.sb("hT%d" % i, [128, 64], F32) for i in range(2)]
            hTb = [cx.sb("hTb%d" % i, [128, 64], BF16) for i in range(2)]
            cbs = [cx.sb("cbs%d" % i, [128, 128], F32) for i in range(2)]
            xtok = [cx.sb("xtok%d" % i, [128, 128], F32) for i in range(2)]
            btok = [cx.sb("btok%d" % i, [128, 128], BF16) for i in range(2)]
            dbc = [cx.sb("dbc%d" % i, [128, 128], F32) for i in range(2)]
            tng = [cx.sb("tng%d" % i, [128, 128], F32) for i in range(2)]
            dec = [cx.sb("dec%d" % i, [128, 128], F32) for i in range(2)]
            eac = [cx.sb("eac%d" % i, [128, 128], F32) for i in range(2)]
            MT = [cx.sb("MT%d" % i, [128, 128], BF16) for i in range(2)]
            CsT = [cx.sb("CsT%d" % i, [128, 128], BF16) for i in range(2)]
            xdt = [cx.sb("xdt%d" % i, [128, 64], BF16) for i in range(2)]
            xw = [cx.sb("xw%d" % i, [128, 64], BF16) for i in range(2)]
            yt = [cx.sb("yt%d" % i, [128, 64], F32) for i in range(2)]
            yst = [cx.sb("yst%d" % i, [128, 8, 128], F32) for i in range(2)]
            cx.es = outer
            cx.dma("sp", dt[:], dt_d, dt, True)
            cx.dma("sp", zs[:], zs_d, zs, True)
            cx.op("pool", lambda e: e.memset(raw[:, 0:3], 0.0), [], [raw])
            for i in range(2):
                cx.op("pool", lambda e, i=i: e.memset(hT[i][:], 0.0), [], [hT[i]])
            act(cx, aneg[:], sm[:, 1:3], AF.Exp, [sm], [aneg])
            cx.op("dve", lambda e: e.tensor_scalar(out=aneg[:], in0=aneg[:], scalar1=-1.0, scalar2=None, op0=ALU.mult), [aneg], [aneg])
            dt3 = dt[:].rearrange("p (b h) -> p b h", h=2)
            dta3 = dta[:].rearrange("p (b h) -> p b h", h=2)
            for hh in range(2):
                cx.op("dve", lambda e, hh=hh: e.tensor_scalar(out=dta3[:, :, hh], in0=dt3[:, :, hh], scalar1=aneg[:, hh:hh + 1], scalar2=None, op0=ALU.mult), [dt, aneg], [dta])
            cx.op("pe", lambda e: e.matmul(ps[0][:, 0:128], lhsT=tri_i, rhs=dta[:], start=True, stop=True), [cst, dta], [ps[0]])
            cx.op("dve", lambda e: e.tensor_scalar(out=nacum[:], in0=ps[0][:, 0:128], scalar1=-1.0, scalar2=None, op0=ALU.mult), [ps[0]], [nacum])
            for g, dst in enumerate((xs, BT, CT)):
                cx.dma("sp", raw[:, 3:3 + S], raw_d[g], raw, True)
                c0 = 8 + 4 * g
                cx.op("dve", lambda e, c0=c0, g=g: e.tensor_scalar(out=acc[:], in0=raw[:, 3:3 + S], scalar1=sm[:, c0 + 3:c0 + 4], scalar2=sm[:, 20 + g:21 + g], op0=ALU.mult, op1=ALU.add),
                      [raw, sm], [acc])
                for wi in (2, 1, 0):
                    cx.op("dve", lambda e, c0=c0, wi=wi: e.scalar_tensor_tensor(out=acc[:], in0=raw[:, wi:wi + S], scalar=sm[:, c0 + wi:c0 + wi + 1], in1=acc[:], op0=ALU.mult, op1=ALU.add),
                          [raw, sm, acc], [acc])
                act(cx, dst[:], acc[:], AF.Silu, [acc], [dst])
            for ck in range(NKB):
                b2 = ck % 2
                blk = slice(ck * 128, (ck + 1) * 128)
                cx.op("pe", lambda e, blk=blk: e.matmul(ps[1][:, 0:128], lhsT=BT[:, blk], rhs=CT[:, blk], start=True, stop=True), [BT, CT], [ps[1]])
                act(cx, cbs[b2][:], ps[1][:, 0:128], AF.Copy, [ps[1]], [cbs[b2]])
                cx.op("pe", lambda e, blk=blk: e.transpose(ps[2][:, 0:128], xs[:, blk], ident), [xs, cst], [ps[2]])
                cx.op("dve", lambda e, b2=b2: e.tensor_copy(out=xtok[b2][:], in_=ps[2][:, 0:128]), [ps[2]], [xtok[b2]])
                cx.op("pe", lambda e, blk=blk: e.transpose(psb[:, 0:128], BT[:, blk], identb[:]), [BT, identb], [psb])
                act(cx, btok[b2][:], psb[:, 0:128], AF.Copy, [psb], [btok[b2]])
                for hh in range(2):
                    col = ck * 2 + hh
                    hs = slice(hh * 64, (hh + 1) * 64)
                    k2 = col % 2
                    cx.op("dve", lambda e, k2=k2, col=col: e.tensor_scalar(out=dbc[k2][:], in0=ones, scalar1=dta[:, col:col + 1], scalar2=None, op0=ALU.mult), [cst, dta], [dbc[k2]])
                    cx.op("pe", lambda e, k2=k2: e.matmul(ps[3 + k2][:, 0:128], lhsT=dbc[k2][:], rhs=tri_i, start=True, stop=True), [dbc[k2], cst], [ps[3 + k2]])
                    pab = ps[3 + k2]
                    cx.op("dve", lambda e, k2=k2, pab=pab: e.tensor_tensor(out=tng[k2][:], in0=pab[:, 0:128], in1=negm, op=ALU.add), [pab, cst], [tng[k2]])
                    act(cx, dec[k2][:], tng[k2][:], AF.Exp, [tng[k2], nacum], [dec[k2]], bias=nacum[:, col:col + 1], scale=1.0)
                    act(cx, eac[k2][:], pab[:, 0:128], AF.Exp, [pab], [eac[k2]])
                    cx.op("dve", lambda e, k2=k2, b2=b2: e.tensor_tensor(out=MT[k2][:], in0=cbs[b2][:], in1=dec[k2][:], op=ALU.mult), [cbs[b2], dec[k2]], [MT[k2]])
                    cx.op("dve", lambda e, k2=k2, b2=b2, hs=hs, col=col: e.tensor_scalar(out=xdt[k2][:], in0=xtok[b2][:, hs], scalar1=dt[:, col:col + 1], scalar2=None, op0=ALU.mult),
                          [xtok[b2], dt], [xdt[k2]])
                    cx.op("pool", lambda e, k2=k2, blk=blk: e.tensor_tensor(out=CsT[k2][:], in0=CT[:, blk], in1=eac[k2][:], op=ALU.mult), [CT, eac[k2]], [CsT[k2]])
                    act(cx, hTb[hh][:], hT[hh][:], AF.Copy, [hT[hh]], [hTb[hh]])
                    py = ps[5]
                    cx.op("pe", lambda e, k2=k2: e.matmul(py[:, hs.start:hs.stop], lhsT=MT[k2][:], rhs=xdt[k2][:], start=True, stop=False) if False else
                          e.matmul(py[:, 0:64], lhsT=MT[k2][:], rhs=xdt[k2][:], start=True, stop=False), [MT[k2], xdt[k2]], [py], inc=False)
                    cx.op("pe", lambda e, k2=k2, hh=hh: e.matmul(py[:, 0:64], lhsT=CsT[k2][:], rhs=hTb[hh][:], start=False, stop=True), [CsT[k2], hTb[hh]], [py])
                    cx.op("dve", lambda e, k2=k2: e.tensor_scalar(out=xw[k2][:], in0=xdt[k2][:], scalar1=dec[k2][:, 127:128], scalar2=None, op0=ALU.mult), [xdt[k2], dec[k2]], [xw[k2]])
                    pss_ = ps[6]
                    cx.op("pe", lambda e, k2=k2, b2=b2: e.matmul(pss_[:, 0:64], lhsT=btok[b2][:], rhs=xw[k2][:], start=True, stop=True), [btok[b2], xw[k2]], [pss_])
                    cx.op("dve", lambda e, k2=k2, hh=hh: e.scalar_tensor_tensor(out=hT[hh][:], in0=hT[hh][:], scalar=eac[k2][:, 127:128], in1=pss_[:, 0:64], op0=ALU.mult, op1=ALU.add),
                          [hT[hh], eac[k2], pss_], [hT[hh]])
                    cx.op("dve", lambda e, k2=k2, b2=b2, hs=hs, hh=hh: e.scalar_tensor_tensor(out=yt[k2][:], in0=xtok[b2][:, hs], scalar=sm[:, 3 + hh:4 + hh], in1=py[:, 0:64], op0=ALU.mult, op1=ALU.add),
                          [xtok[b2], sm, py], [yt[k2]])
                    ys = yst[(ck // 8) % 2]
                    cx.op("pool", lambda e, k2=k2, ys=ys, ck=ck, hs=hs: e.tensor_tensor(out=ys[:, ck % 8, hs], in0=yt[k2][:], in1=zs[:, ck, hs], op=ALU.mult), [yt[k2], zs], [ys])
                if ck % 8 == 7:
                    ys = yst[(ck // 8) % 2]
                    cx.dma("sp", yss_d[:, ck - 7:ck + 1, :], ys[:], ys, False)
            for ys in yst:
                cx.engs["sp"].e.wait_ge(ys.d.dsem, 16 * ys.d.dcount)
            spE = cx.engs["sp"]
            spE.e.engine_nop().then_inc(spE.sem, 1) if hasattr(spE.e, "engine_nop") else spE.e.nop().then_inc(spE.sem, 1)
            spE.count += 1
            _barrier(cx)

        skT = cx.sb("skT", [128, S], BF16)
        sqT = cx.sb("sqT", [128, NQB * 128], BF16)
        sv = cx.sb("sv", [128, NKB, 128], BF16)
        fkT = cx.sb("fkT", [128, S], BF16)
        fqT = cx.sb("fqT", [128, NQB * 128], BF16)
        fv = cx.sb("fv", [128, NKB, 130], BF16)
        logf = cx.sb("logf", [128, NKB], F32)
        tot = cx.sb("tot", [128, NKB], F32)
        pre = cx.sb("pre", [128, NKB + 2], F32)
        negfc = cx.sb("negfc", [128, NKB], F32)
        fcs = cx.sb("fcs", [128, NQB], F32)
        fdf = cx.sb("fdf", [128, NQB], F32)
        bias_all = cx.sb("bias_all", [128, NQB, NKB], F32)
        R = 3
        Et = [cx.sb("E%d" % i, [128, 128], F32) for i in range(R)]
        Lp = [cx.sb("Lp%d" % i, [128, 128], F32) for i in range(R)]
        t1 = [cx.sb("t1%d" % i, [128, 128], F32) for i in range(R)]
        t2 = [cx.sb("t2%d" % i, [128, 128], F32) for i in range(R)]
        Wt = [cx.sb("W%d" % i, [128, 128], BF16) for i in range(R)]
        Lsum = [cx.sb("Lsum%d" % i, [128, 128], F32) for i in range(2)]
        osb_st = [cx.sb("osbst%d" % i, [128, 8 * 128], BF16) for i in range(2)]
        ofx_st = [cx.sb("ofxst%d" % i, [128, 8, 128], BF16) for i in range(2)]
        rec = [cx.sb("rec%d" % i, [128, 1], F32) for i in range(2)]
        for t_, d_, q in ((skT, skT_d, "sp"), (sqT, sqT_d, "act"), (sv, sv_d, "sp"), (fkT, fkT_d, "act"), (fqT, fqT_d, "sp"), (fv, fv_d, "act"), (logf, logf_d, "sp")):
            cx.dma(q, t_[:], d_, t_, True)

        tiles = [(i, kb) for i in range(NQB) for kb in range(2 * i + 1, -1, -1)]
        NT = len(tiles)
        Zp = [ps[0], ps[1]]
        Bp = [ps[2], ps[3]]
        Op = [ps[4], ps[5]]

        def sb_s0(n):
            i, kb = tiles[n]
            z = Zp[n % 2]
            cx.op("pe", lambda e: e.matmul(z[:, 0:128], lhsT=skT[:, kb * 128:(kb + 1) * 128], rhs=sqT[:, i * 128:(i + 1) * 128], start=True, stop=True), [skT, sqT], [z])

        def sb_s1(n):
            i, kb = tiles[n]
            z = Zp[n % 2]
            r = n % R
            act(cx, Et[r][:], z[:, 0:128], AF.Exp, [z], [Et[r]], scale=SCALE)
            act(cx, Lp[r][:], Et[r][:], AF.Ln, [Et[r]], [Lp[r]], bias=1.0, scale=1.0)
            if kb >= 2 * i:
                mi = 0 if kb == 2 * i else 1
                cx.op("dve", lambda e: e.tensor_tensor(out=Lp[r][:], in0=Lp[r][:], in1=mf[:, mi * 128:(mi + 1) * 128], op=ALU.mult), [Lp[r], mf], [Lp[r]])
            cx.op("dve", lambda e: e.scalar_tensor_tensor(out=t1[r][:], in0=z[:, 0:128], scalar=SCALE, in1=Lp[r][:], op0=ALU.mult, op1=ALU.subtract), [z, Lp[r]], [t1[r]])

        def sb_s2(n):
            i, kb = tiles[n]
            r = n % R
            bp = Bp[n % 2]
            first = (kb == 2 * i + 1)
            ls = Lsum[i % 2]
            cx.op("pe", lambda e: e.matmul(bp[:, 0:128], lhsT=tri_s, rhs=Lp[r][:], start=True, stop=first), [cst, Lp[r]], [bp], inc=first)
            if not first:
                cx.op("pe", lambda e: e.matmul(bp[:, 0:128], lhsT=ones, rhs=ls[:], start=False, stop=True), [cst, ls], [bp])
            if first:
                cx.op("pool", lambda e: e.tensor_copy(out=ls[:], in_=Lp[r][:]), [Lp[r]], [ls])
            elif kb > 0:
                cx.op("pool", lambda e: e.tensor_tensor(out=ls[:], in0=ls[:], in1=Lp[r][:], op=ALU.add), [ls, Lp[r]], [ls])

        def sb_s3(n):
            i, kb = tiles[n]
            r = n % R
            bp = Bp[n % 2]
            cx.op("dve", lambda e: e.tensor_tensor(out=t2[r][:], in0=t1[r][:], in1=bp[:, 0:128], op=ALU.subtract), [t1[r], bp], [t2[r]])
            act(cx, Wt[r][:], t2[r][:], AF.Exp, [t2[r]], [Wt[r]])
            if kb >= 2 * i:
                mi = 0 if kb == 2 * i else 1
                cx.op("pool", lambda e: e.tensor_tensor(out=Wt[r][:], in0=Wt[r][:], in1=mb[:, mi * 128:(mi + 1) * 128], op=ALU.mult), [Wt[r], mb], [Wt[r]])

        def sb_s4(n):
            i, kb = tiles[n]
            r = n % R
            o = Op[i % 2]
            first = (kb == 2 * i + 1)
            last = (kb == 0)
            cx.op("pe", lambda e: e.matmul(o[:, 0:128], lhsT=sv[:, kb, :], rhs=Wt[r][:], start=first, stop=last), [sv, Wt[r]], [o], inc=last)
            if last:
                st = osb_st[(i // 8) % 2]
                act(cx, st[:, (i % 8) * 128:(i % 8 + 1) * 128], o[:, 0:128], AF.Copy, [o], [st])
                if i % 8 == 7:
                    cx.dma("sp", osb_d[:, (i - 7) * 128:(i + 1) * 128], st[:], st, False)

        for step in range(NT + 2):
            if step < NT:
                sb_s0(step)
                sb_s1(step)
            if 0 <= step - 1 < NT:
                sb_s2(step - 1)
                sb_s3(step - 1)
            if 0 <= step - 2 < NT:
                sb_s4(step - 2)

        cx.op("pe", lambda e: e.matmul(ps[6][:, 0:NKB], lhsT=tri_i, rhs=logf[:], start=True, stop=True), [cst, logf], [ps[6]])
        cx.op("pe", lambda e: e.matmul(ps[6][:, 128:128 + NKB], lhsT=ones, rhs=logf[:], start=True, stop=True), [cst, logf], [ps[6]])
        cx.op("dve", lambda e: e.tensor_copy(out=tot[:], in_=ps[6][:, 128:128 + NKB]), [ps[6]], [tot])
        cx.op("dve", lambda e: e.memset(pre[:, 0:1], 0.0), [], [pre])
        for b in range(NKB):
            cx.op("dve", lambda e, b=b: e.tensor_tensor(out=pre[:, b + 1:b + 2], in0=pre[:, b:b + 1], in1=tot[:, b:b + 1], op=ALU.add), [pre, tot], [pre])
        cx.op("dve", lambda e: e.scalar_tensor_tensor(out=negfc[:], in0=ps[6][:, 0:NKB], scalar=-1.0, in1=pre[:, 0:NKB], op0=ALU.mult, op1=ALU.subtract), [ps[6], pre], [negfc])
        pre_e = pre[:, 1:1 + 2 * NQB].rearrange("p (i two) -> p i two", two=2)
        cx.op("dve", lambda e: e.tensor_tensor(out=fdf[:], in0=pre_e[:, :, 1], in1=pre_e[:, :, 0], op=ALU.subtract), [pre], [fdf])
        cx.op("dve", lambda e: e.scalar_tensor_tensor(out=fcs[:], in0=fdf[:], scalar=sm[:, 0:1], in1=pre_e[:, :, 0], op0=ALU.mult, op1=ALU.add), [fdf, sm, pre], [fcs])
        for i in range(NQB):
            cx.op("dve", lambda e, i=i: e.tensor_scalar(out=bias_all[:, i, :], in0=negfc[:], scalar1=fcs[:, i:i + 1], scalar2=0.0, op0=ALU.add, op1=ALU.min), [negfc, fcs], [bias_all])

        ftiles = [(i, kb) for i in range(NQB) for kb in range(0, 2 * i + 2)]
        NF = len(ftiles)
        Pt = Wt

        def fx_s0(n):
            i, kb = ftiles[n]
            z = Zp[n % 2]
            cx.op("pe", lambda e: e.matmul(z[:, 0:128], lhsT=fkT[:, kb * 128:(kb + 1) * 128], rhs=fqT[:, i * 128:(i + 1) * 128], start=True, stop=True), [fkT, fqT], [z])

        def fx_s1(n):
            i, kb = ftiles[n]
            z = Zp[n % 2]
            r = n % R
            act(cx, Pt[r][:], z[:, 0:128], AF.Exp, [z, bias_all], [Pt[r]], scale=SCALE, bias=bias_all[:, i, kb:kb + 1])
            if kb >= 2 * i:
                mi = 2 if kb == 2 * i else 3
                cx.op("dve", lambda e: e.tensor_tensor(out=Pt[r][:], in0=Pt[r][:], in1=mb[:, mi * 128:(mi + 1) * 128], op=ALU.mult), [Pt[r], mb], [Pt[r]])

        def fx_s2(n):
            i, kb = ftiles[n]
            r = n % R
            o = Op[i % 2]
            first = (kb == 0)
            last = (kb == 2 * i + 1)
            cx.op("pe", lambda e: e.matmul(o[:, 0:129], lhsT=Pt[r][:], rhs=fv[:, kb, 0:129], start=first, stop=last), [Pt[r], fv], [o], inc=last)
            if last:
                st = ofx_st[(i // 8) % 2]
                rc = rec[i % 2]
                cx.op("dve", lambda e: e.reciprocal(out=rc[:], in_=o[:, 128:129]), [o], [rc])
                cx.op("dve", lambda e: e.tensor_scalar(out=st[:, i % 8, :], in0=o[:, 0:128], scalar1=rc[:, 0:1], scalar2=None, op0=ALU.mult), [o, rc], [st])
                if i % 8 == 7:
                    cx.dma("sp", ofx_d[:, i - 7:i + 1, :], st[:], st, False)

        for step in range(NF + 1):
            if step < NF:
                fx_s0(step)
                fx_s1(step)
            if 0 <= step - 1 < NF:
                fx_s2(step - 1)
        cx.finish()
    return nc


_B_CACHE = {}


def _b_consts():
    j = np.arange(128)
    tri_s = (j[:, None] > j[None, :]).astype(np.float32)
    tri_i = (j[:, None] <= j[None, :]).astype(np.float32)
    ones = np.ones((128, 128), np.float32)
    ident = np.eye(128, dtype=np.float32)
    negm = np.where(j[None, :] >= j[:, None], 0.0, NEG).astype(np.float32)
    return np.concatenate([tri_s, tri_i, ones, ident, negm], axis=1)


def run_B(ob, of, conv_w_l, conv_b_l, a_log_l, d_skip_l):
    if "nc" not in _B_CACHE:
        _B_CACHE["nc"] = build_B()
    nc = _B_CACHE["nc"]
    bf = ob.dtype
    cst = _b_consts()
    kk = np.arange(128)
    d_strict = (kk[:, None] < kk[None, :]).astype(np.float32)
    d_incl = (kk[:, None] <= kk[None, :]).astype(np.float32)
    zeros = np.zeros((128, 128), np.float32)
    onesm = np.ones((128, 128), np.float32)
    in_maps = []
    for c in range(NCORES):
        h, u, g = c % 4, c // 4, c // 4
        blk = lambda a: a.reshape(128, NKB, 128)
        sel = lambda a: np.ascontiguousarray(blk(a)[:, u::2, :]).reshape(128, NQB * 128)
        tokm = lambda a: np.ascontiguousarray(a.T.reshape(NKB, 128, 128).transpose(1, 0, 2))
        fvt = np.zeros((128, NKB, 130), bf)
        fvt[:, :, 0:128] = tokm(ob[2560 + h * 128:2560 + (h + 1) * 128])
        fvt[:, :, 128] = 1.0
        if u == 0:
            mf = np.concatenate([d_strict, zeros, d_incl, zeros], axis=1)
        else:
            mf = np.concatenate([onesm, d_strict, onesm, d_incl], axis=1)
        small = np.zeros((128, 32), np.float32)
        small[:, 0] = u
        for hh in range(2):
            small[:, 1 + hh] = a_log_l[2 * c + hh]
            small[:, 3 + hh] = d_skip_l[2 * c + hh]
        chs = [np.arange(2 * c * 64, 2 * c * 64 + 128), 1024 + g * 128 + np.arange(128), 1280 + g * 128 + np.arange(128)]
        for gi, ch in enumerate(chs):
            small[:, 8 + 4 * gi:12 + 4 * gi] = conv_w_l[:, ch].T
            small[:, 20 + gi] = conv_b_l[ch]
        raw = np.stack([of[ch, :] for ch in chs]).astype(np.float32)
        dtc = np.stack([of[1568 + 2 * c + hh, :].reshape(NKB, 128).T for hh in range(2)], axis=-1)
        in_maps.append({
            "sqT": sel(ob[h * 128:(h + 1) * 128]), "skT": np.ascontiguousarray(ob[512 + h * 128:512 + (h + 1) * 128]),
            "sv": tokm(ob[1024 + h * 128:1024 + (h + 1) * 128]),
            "fqT": sel(ob[1536 + h * 128:1536 + (h + 1) * 128]), "fkT": np.ascontiguousarray(ob[2048 + h * 128:2048 + (h + 1) * 128]),
            "fv": fvt, "logf": np.ascontiguousarray(of[1536 + h].reshape(NKB, 128).T),
            "mf": mf, "cst": cst, "small": small, "raw": np.ascontiguousarray(raw),
            "dt": np.ascontiguousarray(dtc.reshape(128, NKB * 2)),
            "zs": tokm(ob[3072 + 2 * c * 64:3072 + 2 * c * 64 + 128]),
        })
    res = run_bass_kernel_spmd(nc, in_maps, core_ids=list(range(NCORES)))
    o_sbT = np.zeros((512, NKB, 128), bf)
    o_fox = np.zeros((NKB, 128, 512), bf)
    y_ssm = np.zeros((NKB, 128, 1024), np.float32)
    for c in range(NCORES):
        h, u = c % 4, c // 4
        r = res.results[c]
        o_sbT[h * 128:(h + 1) * 128, u::2, :] = np.asarray(r["osb"]).reshape(128, NQB, 128)
        o_fox[u::2, :, h * 128:(h + 1) * 128] = np.asarray(r["ofx"]).transpose(1, 0, 2)
        y_ssm[:, :, 2 * c * 64:2 * c * 64 + 128] = np.asarray(r["yss"]).transpose(1, 0, 2)
    return o_sbT.reshape(512, S), o_fox.reshape(S, 512), y_ssm.reshape(S, 1024)


BIG = 1.0e4


def build_C():
    nc = bass.Bass("TRN2", target_bir_lowering=False)
    T = TPC
    dr = lambda n, s, dt, k="ExternalInput": nc.dram_tensor(n, list(s), dt, kind=k).ap()
    xT_d = dr("xT", [16, 128, T], F32)
    yT_d = dr("yT", [8, 128, T], F32)
    osb_d = dr("osbT", [4, 128, T], BF16)
    ofx_d = dr("ofxT", [4, 128, T], BF16)
    gat_d = dr("gates", [48, 128, T], BF16)
    vec_d = dr("vec", [128, 128], F32)
    cst_d = dr("cst", [128, 3 * 128], F32)
    selE_d = dr("selE", [16, 16 * 128], F32)
    wbr_d = dr("wbr", [16, 128, 16 * 128], F32)
    wo_d = dr("wo", [16, 128, 16 * 128], F32)
    wr_d = dr("wr", [128, 16 * 16], F32)
    wg_d = dr("wg", [NEXP, 8, 128, 16 * 128], F32)
    wu_d = dr("wu", [NEXP, 8, 128, 16 * 128], F32)
    wd_d = dr("wd", [NEXP, 2, 4, 128, D], F32)
    xo_d = dr("xo", [16, 128, T], F32, "ExternalOutput")
    with ExitStack() as es:
        cx = Ctx(nc, es)
        xt = cx.sb("xt", [128, 16, T], F32)
        h2 = cx.sb("h2", [128, 16, T], BF16)
        vt = cx.sb("vt", [128, 128], F32)
        cst = cx.sb("cst", [128, 3 * 128], F32)
        ones = cst[:, 0:128]
        ident = cst[:, 128:256]
        brt = cst[:, 256:384]
        selE = cx.sb("selE", [16, 16 * 128], F32)
        gs2 = cx.sb("gs2", [128, 16], F32)
        comb = cx.sb("comb", [128, 128], F32)
        combT = cx.sb("combT", [16, T], F32)
        ps = [cx.ps("ps%d" % i) for i in range(8)]
        xdeps = [Dep("x%d" % k) for k in range(16)]
        hdeps = [Dep("h%d" % k) for k in range(16)]
        cx.dma("sp", vt[:], vec_d, vt, True)
        cx.dma("sp", cst[:], cst_d, cst, True)
        cx.dma("sp", selE[:], selE_d, selE, True)
        for kc in range(16):
            cx.dma("sp" if kc % 2 else "act", xt[:, kc, :], xT_d[kc], xdeps[kc], True)
        cx.op("dve", lambda e: e.scalar_tensor_tensor(out=gs2[:], in0=vt[:, 16:32], scalar=1.0, in1=vt[:, 0:16], op0=ALU.add, op1=ALU.mult), [vt], [gs2])

        with ExitStack() as es2:
            outer = cx.es
            cx.es = es2
            osb = cx.sb("osb", [128, 4, T], BF16)
            ofx = cx.sb("ofx", [128, 4, T], BF16)
            oss = cx.sb("oss", [128, 8, T], BF16)
            mrg = h2
            sq = [cx.sb("sq%d" % i, [128, T], F32) for i in range(2)]
            rstd = cx.sb("rstd", [128, T], F32)
            tmp = [cx.sb("tmp%d" % i, [128, T], F32) for i in range(2)]
            gsl = [cx.sb("g%d" % i, [128, 3, T], BF16) for i in range(2)]
            wsl = [cx.sb("w%d" % i, [128, 16 * 128], BF16) for i in range(3)]
            m1 = [cx.sb("m1%d" % i, [128, 512], F32) for i in range(2)]
            m2 = [cx.sb("m2%d" % i, [128, 512], F32) for i in range(2)]
            wr = cx.sb("wr", [128, 16 * 16], F32)
            lgT = cx.sb("lgT", [16, T], F32)
            aff = cx.sb("aff", [128, 128], F32)
            sel = cx.sb("sel", [128, 128], F32)
            sel2 = cx.sb("sel2", [128, 128], F32)
            eq = cx.sb("eq", [128, 128], F32)
            r32 = [cx.sb("r32%d" % i, [128, 32], F32) for i in range(5)]
            r8 = [cx.sb("r8%d" % i, [128, 8], F32) for i in range(4)]
            cx.es = outer
            mdeps = hdeps
            cx.dma("sp", osb[:], osb_d.rearrange("k p t -> p k t"), osb, True)
            cx.dma("act", ofx[:], ofx_d.rearrange("k p t -> p k t"), ofx, True)
            cx.dma("sp", wr[:], wr_d, wr, True)
            wq = []

            def wload(src, i, n):
                wsx = wsl[n % 3]
                cx.dma("pool", wsx[:], src[i], wsx, True)
                return wsx
            for kc in range(8):
                s = sq[kc % 2]
                yc = tmp[kc % 2]
                cx.dma("sp" if kc % 2 else "act", yc[:], yT_d[kc], yc, True)
                act(cx, s[:], yc[:], AF.Square, [yc], [s])
                for tt in range(2):
                    cx.op("pe", lambda e, s=s, tt=tt, kc=kc: e.matmul(ps[tt][:, :], lhsT=ones, rhs=s[:, tt * 512:(tt + 1) * 512], start=(kc == 0), stop=(kc == 7)), [cst, s], [ps[tt]])
            for tt in range(2):
                act(cx, rstd[:, tt * 512:(tt + 1) * 512], ps[tt][:, :], AF.Sqrt, [ps[tt]], [rstd], scale=1.0 / 1024, bias=EPS)
            cx.op("dve", lambda e: e.reciprocal(out=rstd[:], in_=rstd[:]), [rstd], [rstd])
            for kc in range(8):
                yc = tmp[kc % 2]
                cx.dma("sp" if kc % 2 else "act", yc[:], yT_d[kc], yc, True)
                cx.op("dve", lambda e, kc=kc, yc=yc: e.scalar_tensor_tensor(out=oss[:, kc, :], in0=yc[:], scalar=vt[:, 80 + kc:81 + kc], in1=rstd[:], op0=ALU.mult, op1=ALU.mult),
                      [yc, vt, rstd], [oss])
            nw = 0
            pi = 2
            for dc in range(16):
                wsx = wload(wbr_d, dc, nw)
                nw += 1
                g = gsl[dc % 2]
                for br in range(3):
                    cx.dma("sp" if br % 2 else "act", g[:, br, :], gat_d[br * 16 + dc], g, True)
                for tt in range(2):
                    sl = slice(tt * 512, (tt + 1) * 512)
                    pp = [ps[(pi + k) % 8] for k in range(3)]
                    pi += 3
                    for br, (src, k0, nk) in enumerate(((osb, 0, 4), (ofx, 4, 4), (oss, 8, 8))):
                        for kk in range(nk):
                            cx.op("pe", lambda e, p=pp[br], src=src, kk=kk, k0=k0, nk=nk, wsx=wsx, sl=sl: e.matmul(p[:, :], lhsT=wsx[:, (k0 + kk) * 128:(k0 + kk + 1) * 128], rhs=src[:, kk, sl],
                                                                                                                     start=(kk == 0), stop=(kk == nk - 1)), [wsx, src], [pp[br]], inc=(kk == nk - 1))
                    a, b = m1[tt], m2[tt]
                    cx.op("dve", lambda e, a=a, g=g, sl=sl, p=pp[0]: e.tensor_tensor(out=a[:], in0=p[:, :], in1=g[:, 0, sl], op=ALU.mult), [pp[0], g], [a])
                    cx.op("dve", lambda e, b=b, g=g, sl=sl, p=pp[1]: e.tensor_tensor(out=b[:], in0=p[:, :], in1=g[:, 1, sl], op=ALU.mult), [pp[1], g], [b])
                    cx.op("pool", lambda e, a=a, b=b: e.tensor_tensor(out=a[:], in0=a[:], in1=b[:], op=ALU.add), [a, b], [a])
                    cx.op("dve", lambda e, b=b, g=g, sl=sl, p=pp[2]: e.tensor_tensor(out=b[:], in0=p[:, :], in1=g[:, 2, sl], op=ALU.mult), [pp[2], g], [b])
                    cx.op("pool", lambda e, a=a, b=b, dc=dc, sl=sl: e.tensor_tensor(out=mrg[:, dc, sl], in0=a[:], in1=b[:], op=ALU.add), [a, b], [mdeps[dc]])
            for dc in range(16):
                wsx = wload(wo_d, dc, nw)
                nw += 1
                for tt in range(2):
                    sl = slice(tt * 512, (tt + 1) * 512)
                    p = ps[pi % 8]
                    pi += 1
                    for kc in range(16):
                        cx.op("pe", lambda e, p=p, kc=kc, wsx=wsx, sl=sl: e.matmul(p[:, :], lhsT=wsx[:, kc * 128:(kc + 1) * 128], rhs=mrg[:, kc, sl], start=(kc == 0), stop=(kc == 15)),
                              [wsx, mdeps[kc]], [p], inc=(kc == 15))
                    cx.op("dve", lambda e, p=p, dc=dc, sl=sl: e.scalar_tensor_tensor(out=xt[:, dc, sl], in0=p[:, :], scalar=vt[:, 48 + dc:49 + dc], in1=xt[:, dc, sl], op0=ALU.mult, op1=ALU.add),
                          [p, vt, xdeps[dc]], [xdeps[dc]])
            for kc in range(16):
                s = sq[kc % 2]
                act(cx, s[:], xt[:, kc, :], AF.Square, [xdeps[kc]], [s])
                for tt in range(2):
                    cx.op("pe", lambda e, s=s, tt=tt, kc=kc: e.matmul(ps[tt][:, :], lhsT=ones, rhs=s[:, tt * 512:(tt + 1) * 512], start=(kc == 0), stop=(kc == 15)), [cst, s], [ps[tt]])
            for tt in range(2):
                act(cx, rstd[:, tt * 512:(tt + 1) * 512], ps[tt][:, :], AF.Sqrt, [ps[tt]], [rstd], scale=1.0 / D, bias=EPS)
            cx.op("dve", lambda e: e.reciprocal(out=rstd[:], in_=rstd[:]), [rstd], [rstd])
            for kc in range(16):
                tm = tmp[kc % 2]
                hf = sq[kc % 2]
                cx.op("pool", lambda e, kc=kc, tm=tm: e.tensor_tensor(out=tm[:], in0=xt[:, kc, :], in1=rstd[:], op=ALU.mult), [xdeps[kc], rstd], [tm])
                cx.op("dve", lambda e, kc=kc, tm=tm, hf=hf: e.tensor_scalar(out=hf[:], in0=tm[:], scalar1=gs2[:, kc:kc + 1], scalar2=vt[:, 32 + kc:33 + kc], op0=ALU.mult, op1=ALU.add),
                      [tm, gs2, vt], [hf])
                act(cx, h2[:, kc, :], hf[:], AF.Copy, [hf], [hdeps[kc]])
                for tt in range(2):
                    cx.op("pe", lambda e, hf=hf, tt=tt, kc=kc: e.matmul(ps[2 + tt][0:16, :], lhsT=wr[:, kc * 16:(kc + 1) * 16], rhs=hf[:, tt * 512:(tt + 1) * 512], start=(kc == 0), stop=(kc == 15)),
                          [wr, hf], [ps[2 + tt]])
            for tt in range(2):
                act(cx, lgT[:, tt * 512:(tt + 1) * 512], ps[2 + tt][0:16, :], AF.Copy, [ps[2 + tt]], [lgT])
            for b in range(8):
                cx.op("pe", lambda e, b=b: e.transpose(ps[4][:, b * 16:(b + 1) * 16], lgT[:, b * 128:(b + 1) * 128], ident[0:16, 0:16]), [lgT, cst], [ps[4]])
            act(cx, aff[:], ps[4][:, 0:128], AF.Sigmoid, [ps[4]], [aff])
            dv = lambda fn, r, w: cx.op("dve", fn, r, w)
            v3 = lambda t_, a, b: t_[:].rearrange("p (a b) -> p a b", a=a, b=b)
            bc = lambda t_, a, b: t_[:].unsqueeze(2).to_broadcast([128, a, b])
            dv(lambda e: e.tensor_tensor(out=sel[:], in0=aff[:], in1=brt, op=ALU.add), [aff, cst], [sel])
            dv(lambda e: e.tensor_reduce(out=r32[0][:], in_=v3(sel, 32, 4), axis=AX.X, op=ALU.max), [sel], [r32[0]])
            dv(lambda e: e.tensor_tensor(out=v3(eq, 32, 4), in0=v3(sel, 32, 4), in1=bc(r32[0], 32, 4), op=ALU.is_equal), [sel, r32[0]], [eq])
            dv(lambda e: e.scalar_tensor_tensor(out=sel2[:], in0=eq[:], scalar=-BIG, in1=sel[:], op0=ALU.mult, op1=ALU.add), [eq, sel], [sel2])
            dv(lambda e: e.tensor_reduce(out=r32[1][:], in_=v3(sel2, 32, 4), axis=AX.X, op=ALU.max), [sel2], [r32[1]])
            dv(lambda e: e.tensor_tensor(out=r32[2][:], in0=r32[0][:], in1=r32[1][:], op=ALU.add), [r32[0], r32[1]], [r32[2]])
            dv(lambda e: e.tensor_reduce(out=r8[0][:], in_=v3(r32[2], 8, 4), axis=AX.X, op=ALU.max), [r32[2]], [r8[0]])
            dv(lambda e: e.tensor_tensor(out=v3(r32[3], 8, 4), in0=v3(r32[2], 8, 4), in1=bc(r8[0], 8, 4), op=ALU.is_equal), [r32[2], r8[0]], [r32[3]])
            dv(lambda e: e.tensor_scalar(out=r32[4][:], in0=r32[3][:], scalar1=-1.0, scalar2=BIG, op0=ALU.add, op1=ALU.mult), [r32[3]], [r32[4]])
            dv(lambda e: e.tensor_tensor(out=v3(sel2, 32, 4), in0=v3(sel, 32, 4), in1=bc(r32[4], 32, 4), op=ALU.add), [sel, r32[4]], [sel2])
            dv(lambda e: e.tensor_reduce(out=r8[1][:], in_=v3(sel2, 8, 16), axis=AX.X, op=ALU.max), [sel2], [r8[1]])
            dv(lambda e: e.tensor_tensor(out=v3(eq, 8, 16), in0=v3(sel2, 8, 16), in1=bc(r8[1], 8, 16), op=ALU.is_equal), [sel2, r8[1]], [eq])
            dv(lambda e: e.scalar_tensor_tensor(out=sel[:], in0=eq[:], scalar=-BIG, in1=sel2[:], op0=ALU.mult, op1=ALU.add), [eq, sel2], [sel])
            dv(lambda e: e.tensor_reduce(out=r8[2][:], in_=v3(sel, 8, 16), axis=AX.X, op=ALU.max), [sel], [r8[2]])
            dv(lambda e: e.tensor_tensor(out=v3(sel2, 8, 16), in0=v3(sel, 8, 16), in1=bc(r8[2], 8, 16), op=ALU.is_equal), [sel, r8[2]], [sel2])
            dv(lambda e: e.tensor_tensor(out=eq[:], in0=eq[:], in1=sel2[:], op=ALU.add), [eq, sel2], [eq])
            dv(lambda e: e.tensor_tensor(out=sel[:], in0=eq[:], in1=aff[:], op=ALU.mult), [eq, aff], [sel])
            dv(lambda e: e.tensor_reduce(out=r8[3][:], in_=v3(sel, 8, 16), axis=AX.X, op=ALU.add), [sel], [r8[3]])
            dv(lambda e: e.reciprocal(out=r8[3][:], in_=r8[3][:]), [r8[3]], [r8[3]])
            dv(lambda e: e.tensor_tensor(out=v3(comb, 8, 16), in0=v3(sel, 8, 16), in1=bc(r8[3], 8, 16), op=ALU.mult), [sel, r8[3]], [comb])
            for b in range(8):
                cx.op("pe", lambda e, b=b: e.transpose(ps[5 + b // 4][0:16, (b % 4) * 128:(b % 4 + 1) * 128], comb[:, b * 16:(b + 1) * 16], ident), [comb, cst], [ps[5 + b // 4]])
            for tt in range(2):
                act(cx, combT[:, tt * 512:(tt + 1) * 512], ps[5 + tt][0:16, :], AF.Copy, [ps[5 + tt]], [combT])
            spE = cx.engs["sp"]
            _barrier(cx)

        wg = [cx.sb("wg%d" % i, [128, 16 * 128], BF16) for i in range(3)]
        wu = [cx.sb("wu%d" % i, [128, 16 * 128], BF16) for i in range(3)]
        wd = [cx.sb("wd%d" % i, [128, 4, D], BF16) for i in range(2)]
        actT = [cx.sb("actT%d" % i, [128, 4, T], BF16) for i in range(2)]
        cbc = [cx.sb("cbc%d" % i, [128, T], F32) for i in range(2)]
        sT = [cx.sb("sT%d" % i, [128, 512], F32) for i in range(2)]
        aT = [cx.sb("aT%d" % i, [128, 512], F32) for i in range(2)]
        nq = 0
        pi = 0
        units = [(e, hf) for e in range(NEXP) for hf in range(2)]

        def load_gu(e, fc, n):
            cx.dma("pool", wg[n % 3][:], wg_d[e, fc], wg[n % 3], True)
            cx.dma("pool", wu[n % 3][:], wu_d[e, fc], wu[n % 3], True)

        chunks = [(e, hf, f4) for (e, hf) in units for f4 in range(4)]
        for n in range(2):
            e, hf, f4 = chunks[n]
            load_gu(e, hf * 4 + f4, n)
        cx.dma("pool", wd[0][:], wd_d[0, 0].rearrange("f p d -> p f d"), wd[0], True)
        for ui, (e, hf) in enumerate(units):
            if hf == 0:
                cb = cbc[e % 2]
                for tt in range(2):
                    p = ps[6 + tt]
                    cx.op("pe", lambda e_, p=p, tt=tt, e=e: e_.matmul(p[:, :], lhsT=selE[:, e * 128:(e + 1) * 128], rhs=combT[:, tt * 512:(tt + 1) * 512], start=True, stop=True), [selE, combT], [p])
                    act(cx, cb[:, tt * 512:(tt + 1) * 512], p[:, :], AF.Copy, [p], [cb])
            cb = cbc[e % 2]
            at = actT[ui % 2]
            if ui + 1 < len(units):
                e2, hf2 = units[ui + 1]
                cx.dma("pool", wd[(ui + 1) % 2][:], wd_d[e2, hf2].rearrange("f p d -> p f d"), wd[(ui + 1) % 2], True)
            for f4 in range(4):
                n = ui * 4 + f4
                wgx, wux = wg[n % 3], wu[n % 3]
                for tt in range(2):
                    sl = slice(tt * 512, (tt + 1) * 512)
                    pg, pu = ps[pi % 4], ps[(pi + 1) % 4]
                    pi += 2
                    for kc in range(16):
                        cx.op("pe", lambda e_, pg=pg, kc=kc, wgx=wgx, sl=sl: e_.matmul(pg[:, :], lhsT=wgx[:, kc * 128:(kc + 1) * 128], rhs=h2[:, kc, sl], start=(kc == 0), stop=(kc == 15)),
                              [wgx, hdeps[kc]], [pg], inc=(kc == 15))
                    for kc in range(16):
                        cx.op("pe", lambda e_, pu=pu, kc=kc, wux=wux, sl=sl: e_.matmul(pu[:, :], lhsT=wux[:, kc * 128:(kc + 1) * 128], rhs=h2[:, kc, sl], start=(kc == 0), stop=(kc == 15)),
                              [wux, hdeps[kc]], [pu], inc=(kc == 15))
                    s_, a_ = sT[tt], aT[tt]
                    act(cx, s_[:], pg[:, :], AF.Silu, [pg], [s_])
                    cx.op("dve", lambda e_, a_=a_, s_=s_, pu=pu: e_.tensor_tensor(out=a_[:], in0=pu[:, :], in1=s_[:], op=ALU.mult), [pu, s_], [a_])
                    cx.op("pool", lambda e_, a_=a_, at=at, f4=f4, sl=sl, cb=cb: e_.tensor_tensor(out=at[:, f4, sl], in0=a_[:], in1=cb[:, sl], op=ALU.mult), [a_, cb], [at])
                if n + 2 < len(chunks):
                    e3, hf3, f43 = chunks[n + 2]
                    load_gu(e3, hf3 * 4 + f43, n + 2)
            wdx = wd[ui % 2]
            for dc in range(16):
                for tt in range(2):
                    sl = slice(tt * 512, (tt + 1) * 512)
                    p = ps[4 + (pi % 2)]
                    pi += 1
                    for f4 in range(4):
                        cx.op("pe", lambda e_, p=p, f4=f4, dc=dc, sl=sl, wdx=wdx, at=at: e_.matmul(p[:, :], lhsT=wdx[:, f4, dc * 128:(dc + 1) * 128], rhs=at[:, f4, sl], start=(f4 == 0), stop=(f4 == 3)),
                              [wdx, at], [p], inc=(f4 == 3))
                    cx.op("dve", lambda e_, p=p, dc=dc, sl=sl: e_.scalar_tensor_tensor(out=xt[:, dc, sl], in0=p[:, :], scalar=vt[:, 64 + dc:65 + dc], in1=xt[:, dc, sl], op0=ALU.mult, op1=ALU.add),
                          [p, vt, xdeps[dc]], [xdeps[dc]])
        for dc in range(16):
            cx.dma("sp" if dc % 2 else "act", xo_d[dc], xt[:, dc, :], xdeps[dc], False)
        cx.finish()
    return nc


_C_CACHE = {}


def _wlayout(wmat):
    K_, N_ = wmat.shape
    return np.ascontiguousarray(wmat.reshape(K_ // 128, 128, N_ // 128, 128).transpose(2, 1, 0, 3)).reshape(N_ // 128, 128, (K_ // 128) * 128)


def run_C(x_tok, o_sbT, o_fox, y_ssm, gatesT, mod_l, g_ffn_l, g_ssm_l, wb_sb, wb_fox, wb_ssm, w_out_l, w_router, b_router, wg_l, wu_l, wd_l):
    if "nc" not in _C_CACHE:
        _C_CACHE["nc"] = build_C()
    nc = _C_CACHE["nc"]
    T = TPC
    vec = np.zeros((128, 128), np.float32)
    col = lambda v: v.reshape(-1, 128).T
    vec[:, 0:16] = col(g_ffn_l)
    vec[:, 16:32] = col(mod_l[4 * D:5 * D])
    vec[:, 32:48] = col(mod_l[3 * D:4 * D])
    vec[:, 48:64] = col(mod_l[2 * D:3 * D])
    vec[:, 64:80] = col(mod_l[5 * D:6 * D])
    vec[:, 80:88] = col(g_ssm_l)
    cst = np.zeros((128, 384), np.float32)
    cst[:, 0:128] = 1.0
    cst[:, 128:256] = np.eye(128, dtype=np.float32)
    cst[:, 256:384] = np.tile(b_router[None, :], (128, 8))
    selE = np.zeros((16, 16 * 128), np.float32)
    for e in range(16):
        selE[e, e * 128:(e + 1) * 128] = 1.0
    wbr = _wlayout(np.concatenate([wb_sb, wb_fox, wb_ssm], axis=0))
    wo = _wlayout(w_out_l)
    wr = np.ascontiguousarray(w_router.reshape(16, 128, 16).transpose(1, 0, 2)).reshape(128, 256)
    lay = lambda w: np.ascontiguousarray(w.reshape(NEXP, 16, 128, 8, 128).transpose(0, 3, 2, 1, 4)).reshape(NEXP, 8, 128, 16 * 128)
    wg = lay(wg_l)
    wu = lay(wu_l)
    wd = np.ascontiguousarray(wd_l).reshape(NEXP, 2, 4, 128, D)
    in_maps = []
    for c in range(NCORES):
        ts = slice(c * T, (c + 1) * T)
        in_maps.append({
            "xT": np.ascontiguousarray(x_tok[ts].T).reshape(16, 128, T),
            "yT": np.ascontiguousarray(y_ssm[ts].T).reshape(8, 128, T),
            "osbT": np.ascontiguousarray(o_sbT[:, ts]).reshape(4, 128, T),
            "ofxT": np.ascontiguousarray(o_fox[ts].T).reshape(4, 128, T),
            "gates": np.ascontiguousarray(gatesT[:, ts]).reshape(48, 128, T),
            "vec": vec, "cst": cst, "selE": selE, "wbr": wbr, "wo": wo, "wr": wr, "wg": wg, "wu": wu, "wd": wd,
        })
    res = run_bass_kernel_spmd(nc, in_maps, core_ids=list(range(NCORES)))
    xo = np.zeros((S, D), np.float32)
    for c in range(NCORES):
        xo[c * T:(c + 1) * T] = np.asarray(res.results[c]["xo"]).reshape(D, T).T
    return xo


def kernel(x, c, w_ada, b_ada, g_norm_mix, w_in, b_fgate, g_q_fox, g_k_fox, conv_w, conv_b, dt_bias, a_log, d_skip,
           g_ssm_norm, w_branch_sb, w_branch_fox, w_branch_ssm, w_out, g_norm_ffn, w_router, b_router, w_e_gate, w_e_up, w_e_down):
    f = lambda a: np.asarray(a, dtype=np.float32)
    x_tok = f(x)[0]
    mod = run_M(f(c), f(w_ada), f(b_ada))
    for l in range(2):
        ob, of = run_A(x_tok, f(w_in[l]), mod[l], f(g_norm_mix[l]), f(b_fgate[l]), f(g_q_fox[l]), f(g_k_fox[l]), f(dt_bias[l]))
        o_sbT, o_fox, y_ssm = run_B(ob, of, f(conv_w[l]), f(conv_b[l]), f(a_log[l]), f(d_skip[l]))
        x_tok = run_C(x_tok, o_sbT, o_fox, y_ssm, ob[4096:], mod[l], f(g_norm_ffn[l]), f(g_ssm_norm[l]), f(w_branch_sb[l]), f(w_branch_fox[l]),
                      f(w_branch_ssm[l]), f(w_out[l]), f(w_router), f(b_router), f(w_e_gate[l]), f(w_e_up[l]), f(w_e_down[l]))
    return x_tok[None].astype(np.float32)
```

```python
from contextlib import ExitStack
import numpy as np
import concourse.bass as bass
import concourse.mybir as mybir
from concourse.bass_utils import run_bass_kernel_spmd

F32 = mybir.dt.float32
BF16 = mybir.dt.bfloat16
AF = mybir.ActivationFunctionType
ALU = mybir.AluOpType
AX = mybir.AxisListType

NCORES = 8
D = 2048
S = 8192
TPC = S // NCORES
EPS = 1e-6
NEXP = 16
FF = 1024


class Dep:
    __slots__ = ("lw", "rd", "dsem", "dcount", "name")

    def __init__(self, name=""):
        self.lw = None
        self.rd = []
        self.dsem = None
        self.dcount = 0
        self.name = name


class Tile:
    def __init__(self, t, name):
        self.t = t
        self.d = Dep(name)

    def __getitem__(self, k):
        return self.t[k]


class EngState:
    def __init__(self, e, sem, name):
        self.e = e
        self.sem = sem
        self.count = 0
        self.name = name
        self.seen = {}


class Ctx:
    def __init__(self, nc, es):
        self.nc = nc
        self.es = es
        self.engs = {}
        for nm, e in (("pe", nc.tensor), ("act", nc.scalar), ("dve", nc.vector), ("pool", nc.gpsimd), ("sp", nc.sync)):
            self.engs[nm] = EngState(e, es.enter_context(nc.semaphore("s_" + nm)), nm)
        self.outdeps = []
        self.nsem = 5

    def sb(self, name, shape, dtype):
        return Tile(self.es.enter_context(self.nc.sbuf_tensor("t_" + name, list(shape), dtype)), name)

    def ps(self, name, shape=(128, 512), dtype=F32):
        return Tile(self.es.enter_context(self.nc.psum_tensor("p_" + name, list(shape), dtype)), name)

    def _collect(self, E, reads, writes):
        need = []
        for d in reads:
            if d.lw is not None:
                need.append(d.lw)
        for d in writes:
            if d.lw is not None:
                need.append(d.lw)
            need.extend(d.rd)
        for src in need:
            if src[0] == "eng":
                _, en, ticket = src
                if en == E.name and en in ("pe", "sp"):
                    continue
                F = self.engs[en]
                assert ticket <= F.count, f"wait on pending ticket {en} {ticket} > {F.count}"
                key = "e_" + en
                if E.seen.get(key, 0) >= ticket:
                    continue
                E.e.wait_ge(F.sem, ticket)
                E.seen[key] = ticket
            else:
                _, dd, cnt = src
                key = id(dd)
                if E.seen.get(key, 0) >= cnt:
                    continue
                E.e.wait_ge(dd.dsem, 16 * cnt)
                E.seen[key] = cnt

    def op(self, eng, fn, reads=(), writes=(), inc=True):
        E = self.engs[eng]
        reads = [r.d if isinstance(r, Tile) else r for r in reads]
        writes = [w.d if isinstance(w, Tile) else w for w in writes]
        self._collect(E, reads, writes)
        inst = fn(E.e)
        if inc:
            inst.then_inc(E.sem, 1)
            E.count += 1
            ticket = E.count
        else:
            ticket = E.count + 1
        rec = ("eng", eng, ticket)
        for d in writes:
            d.lw = rec
            d.rd = []
        for d in reads:
            d.rd.append(rec)
        return inst

    def dma(self, queue, out, in_, dep, load, **kw):
        Q = self.engs[queue]
        dep = dep.d if isinstance(dep, Tile) else dep
        if dep.dsem is None:
            dep.dsem = self.es.enter_context(self.nc.semaphore("d%d" % self.nsem))
            self.nsem += 1
        if load:
            self._collect(Q, [], [dep])
        else:
            self._collect(Q, [dep], [])
        inst = Q.e.dma_start(out=out, in_=in_, **kw)
        inst.then_inc(dep.dsem, 16)
        dep.dcount += 1
        rec = ("dma", dep, dep.dcount)
        if load:
            dep.lw = rec
            dep.rd = []
        else:
            dep.rd.append(rec)
            if dep not in self.outdeps:
                self.outdeps.append(dep)
        return inst

    def finish(self):
        E = self.engs["sp"]
        for dep in self.outdeps:
            E.e.wait_ge(dep.dsem, 16 * dep.dcount)


def act(cx, out, in_, func, reads, writes, eng="act", **kw):
    return cx.op(eng, lambda e: e.activation(out=out, in_=in_, func=func, **kw), reads, writes)


MCOLS = 6 * D // NCORES


def build_M():
    nc = bass.Bass("TRN2", target_bir_lowering=False)
    c_in = nc.dram_tensor("c", [128, 16], F32, kind="ExternalInput").ap()
    w = nc.dram_tensor("w", [2, 16, 128, MCOLS], F32, kind="ExternalInput").ap()
    b = nc.dram_tensor("b", [1, 2 * MCOLS], F32, kind="ExternalInput").ap()
    o = nc.dram_tensor("o", [1, 2 * MCOLS], F32, kind="ExternalOutput").ap()
    with ExitStack() as es:
        cx = Ctx(nc, es)
        ct = cx.sb("ct", [128, 16], F32)
        cond = cx.sb("cond", [128, 16], F32)
        bt = cx.sb("bt", [1, 2 * MCOLS], F32)
        ot = cx.sb("ot", [1, 2 * MCOLS], F32)
        wsl = [cx.sb("w%d" % i, [128, MCOLS], F32) for i in range(4)]
        pss = [cx.ps("ps%d" % i) for i in range(3)]
        cx.dma("sp", ct[:], c_in, ct, True)
        cx.dma("sp", bt[:], b, bt, True)
        act(cx, cond[:], ct[:], AF.Silu, [ct], [cond])
        i = 0
        for l in range(2):
            for kc in range(16):
                ws = wsl[i % 4]
                i += 1
                cx.dma("sp" if i % 2 else "pool", ws[:], w[l, kc], ws, True)
                for n in range(3):
                    cx.op("pe", lambda e, n=n, ws=ws, kc=kc: e.matmul(pss[n][0:1, :], lhsT=cond[:, kc:kc + 1], rhs=ws[:, n * 512:(n + 1) * 512],
                                                                     start=(kc == 0), stop=(kc == 15)),
                          [cond, ws], [pss[n]], inc=True)
            for n in range(3):
                sl = slice(l * MCOLS + n * 512, l * MCOLS + (n + 1) * 512)
                cx.op("dve", lambda e, n=n, sl=sl: e.tensor_tensor(out=ot[0:1, sl], in0=pss[n][0:1, :], in1=bt[0:1, sl], op=ALU.add),
                      [pss[n], bt], [ot])
        cx.dma("sp", o, ot[:], ot, False)
        cx.finish()
    return nc


def run_M(c, w_ada, b_ada):
    nc = build_M()
    c_l = np.ascontiguousarray(c.reshape(16, 128).T)
    in_maps = []
    for k in range(NCORES):
        sl = slice(k * MCOLS, (k + 1) * MCOLS)
        in_maps.append({
            "c": c_l,
            "w": np.ascontiguousarray(w_ada[:, :, sl].reshape(2, 16, 128, MCOLS)),
            "b": np.ascontiguousarray(b_ada[:, sl].reshape(1, 2 * MCOLS)),
        })
    res = run_bass_kernel_spmd(nc, in_maps, core_ids=list(range(NCORES)))
    mod = np.zeros((2, 6 * D), np.float32)
    for k in range(NCORES):
        r = res.results[k]["o"].reshape(2, MCOLS)
        mod[:, k * MCOLS:(k + 1) * MCOLS] = r
    return mod


NFC = 93
A_KIND = (["copy"] * 12 + ["qkn"] * 8 + ["copy"] * 4 + ["silu"] * 8 + ["f32"] * 12 + ["sig"] * 48 + ["small"])
OB_ROWS = (24 + 8 + 48) * 128
OF_ROWS = 12 * 128 + 64


def a_col_index():
    idx = -np.ones(NFC * 128, np.int64)
    idx[0:3072] = np.arange(0, 3072)
    idx[3072:3072 + 1024] = np.arange(3076, 3076 + 1024)
    idx[4096:4096 + 1536] = np.arange(4100, 4100 + 1536)
    idx[5632:5632 + 6144] = np.arange(5652, 5652 + 6144)
    base = 92 * 128
    idx[base:base + 4] = np.arange(3072, 3076)
    idx[base + 32:base + 48] = np.arange(5636, 5652)
    return idx


def build_A():
    nc = bass.Bass("TRN2", target_bir_lowering=False)
    T = TPC
    xT = nc.dram_tensor("xT", [16, 128, T], F32, kind="ExternalInput").ap()
    w = nc.dram_tensor("w", [NFC, 128, 16 * 128], F32, kind="ExternalInput").ap()
    vec = nc.dram_tensor("vec", [128, 64], F32, kind="ExternalInput").ap()
    ones_d = nc.dram_tensor("ones", [128, 128], F32, kind="ExternalInput").ap()
    ob = nc.dram_tensor("ob", [OB_ROWS // 128, 128, T], BF16, kind="ExternalOutput").ap()
    of = nc.dram_tensor("of", [OF_ROWS, T], F32, kind="ExternalOutput").ap()
    with ExitStack() as es:
        cx = Ctx(nc, es)
        xt = cx.sb("xt", [128, 16, T], F32)
        hT = cx.sb("hT", [128, 16, T], BF16)
        vt = cx.sb("vt", [128, 64], F32)
        gs = cx.sb("gs", [128, 16], F32)
        nsb = cx.sb("nsb", [128, 1], F32)
        ones = cx.sb("onesf", [128, 128], F32)
        sq = [cx.sb("sq%d" % i, [128, T], F32) for i in range(2)]
        rstd = cx.sb("rstd", [128, T], F32)
        tmp = [cx.sb("tmp%d" % i, [128, T], F32) for i in range(2)]
        wsl = [cx.sb("w%d" % i, [128, 16 * 128], BF16) for i in range(4)]
        stb = [cx.sb("stb%d" % i, [128, T], BF16) for i in range(3)]
        stf = [cx.sb("stf%d" % i, [128, T], F32) for i in range(2)]
        ps = [cx.ps("ps%d" % i) for i in range(8)]
        xdeps = [Dep("x%d" % k) for k in range(16)]
        hdeps = [Dep("h%d" % k) for k in range(16)]

        cx.dma("sp", vt[:], vec, vt, True)
        cx.dma("sp", ones[:], ones_d, ones, True)
        for kc in range(16):
            cx.dma("sp" if kc % 2 else "act", xt[:, kc, :], xT[kc], xdeps[kc], True)
        def wload(fc):
            cx.dma("pool", wsl[fc % 4][:], w[fc], wsl[fc % 4], True)
        for fc in range(4):
            wload(fc)
        cx.op("dve", lambda e: e.scalar_tensor_tensor(out=gs[:], in0=vt[:, 16:32], scalar=1.0, in1=vt[:, 0:16], op0=ALU.add, op1=ALU.mult), [vt], [gs])
        cx.op("dve", lambda e: e.tensor_scalar(out=nsb[:], in0=vt[:, 50:51], scalar1=-1.0, scalar2=None, op0=ALU.mult), [vt], [nsb])
        for kc in range(16):
            s = sq[kc % 2]
            act(cx, s[:], xt[:, kc, :], AF.Square, [xdeps[kc]], [s])
            for tt in range(2):
                cx.op("pe", lambda e, s=s, tt=tt, kc=kc: e.matmul(ps[tt][:, :], lhsT=ones[:], rhs=s[:, tt * 512:(tt + 1) * 512], start=(kc == 0), stop=(kc == 15)),
                      [ones, s], [ps[tt]])
        for tt in range(2):
            act(cx, rstd[:, tt * 512:(tt + 1) * 512], ps[tt][:, :], AF.Sqrt, [ps[tt]], [rstd], scale=1.0 / D, bias=EPS)
        cx.op("dve", lambda e: e.reciprocal(out=rstd[:], in_=rstd[:]), [rstd], [rstd])
        for kc in range(16):
            tm = tmp[kc % 2]
            cx.op("pool", lambda e, kc=kc, tm=tm: e.tensor_tensor(out=tm[:], in0=xt[:, kc, :], in1=rstd[:], op=ALU.mult), [xdeps[kc], rstd], [tm])
            cx.op("dve", lambda e, kc=kc, tm=tm: e.tensor_scalar(out=hT[:, kc, :], in0=tm[:], scalar1=gs[:, kc:kc + 1], scalar2=vt[:, 32 + kc:33 + kc],
                                                                 op0=ALU.mult, op1=ALU.add), [tm, gs, vt], [hdeps[kc]])
        nb = 0
        nf = 0
        pi = 0
        for fc in range(NFC):
            kind = A_KIND[fc]
            ws = wsl[fc % 4]
            M = 64 if kind == "small" else 128
            pts = []
            for tt in range(2):
                p = ps[pi % 8]
                pi += 1
                pts.append(p)
                for kc in range(16):
                    cx.op("pe", lambda e, p=p, kc=kc, tt=tt, ws=ws, M=M: e.matmul(p[0:M, :], lhsT=ws[:, kc * 128:kc * 128 + M], rhs=hT[:, kc, tt * 512:(tt + 1) * 512],
                                                                                 start=(kc == 0), stop=(kc == 15)),
                          [ws, hdeps[kc]], [p], inc=(kc == 15))
            if fc + 4 < NFC:
                wload(fc + 4)
            if kind in ("copy", "silu", "sig", "qkn"):
                sb_ = stb[nb % 3]
                for tt in range(2):
                    p = pts[tt]
                    sl = slice(tt * 512, (tt + 1) * 512)
                    if kind == "copy":
                        if tt == 0:
                            act(cx, sb_[:, sl], p[:, :], AF.Copy, [p], [sb_])
                        else:
                            cx.op("dve", lambda e, p=p, sl=sl, sb_=sb_: e.tensor_copy(out=sb_[:, sl], in_=p[:, :]), [p], [sb_])
                    elif kind == "silu":
                        act(cx, sb_[:, sl], p[:, :], AF.Silu, [p], [sb_])
                    elif kind == "sig":
                        act(cx, sb_[:, sl], p[:, :], AF.Sigmoid, [p], [sb_])
                    else:
                        s = sq[tt]
                        p2 = ps[pi % 8]
                        pi += 1
                        act(cx, s[:, 0:512], p[:, :], AF.Square, [p], [s])
                        cx.op("pe", lambda e, s=s, p2=p2: e.matmul(p2[:, :], lhsT=ones[:], rhs=s[:, 0:512], start=True, stop=True), [ones, s], [p2])
                        act(cx, s[:, 512:1024], p2[:, :], AF.Sqrt, [p2], [s], scale=1.0 / 128, bias=EPS)
                        cx.op("dve", lambda e, s=s: e.reciprocal(out=s[:, 512:1024], in_=s[:, 512:1024]), [s], [s])
                        gcol = 48 if fc < 16 else 49
                        cx.op("dve", lambda e, s=s, p=p, sl=sl, sb_=sb_, gcol=gcol: e.scalar_tensor_tensor(out=sb_[:, sl], in0=p[:, :], scalar=vt[:, gcol:gcol + 1], in1=s[:, 512:1024],
                                                                                                     op0=ALU.mult, op1=ALU.mult), [p, s, vt], [sb_])
                cx.dma("sp", ob[nb], sb_[:], sb_, False)
                nb += 1
            elif kind == "f32":
                sf = stf[nf % 2]
                for tt in range(2):
                    p = pts[tt]
                    sl = slice(tt * 512, (tt + 1) * 512)
                    if tt == 0:
                        act(cx, sf[:, sl], p[:, :], AF.Copy, [p], [sf])
                    else:
                        cx.op("dve", lambda e, p=p, sl=sl, sf=sf: e.tensor_copy(out=sf[:, sl], in_=p[:, :]), [p], [sf])
                cx.dma("sp", of[nf * 128:(nf + 1) * 128, :], sf[:], sf, False)
                nf += 1
            else:
                sf = stf[nf % 2]
                for tt in range(2):
                    p = pts[tt]
                    sl = slice(tt * 512, (tt + 1) * 512)
                    act(cx, sf[0:32, sl], p[0:32, :], AF.Exp, [p], [sf], scale=-1.0, bias=nsb[0:32, :])
                    act(cx, sf[32:64, sl], p[32:64, :], AF.Exp, [p], [sf], scale=1.0, bias=vt[32:64, 50:51])
                    act(cx, sf[0:64, sl], sf[0:64, sl], AF.Ln, [sf], [sf], scale=1.0, bias=1.0)
                    cx.op("dve", lambda e, sf=sf, sl=sl: e.tensor_scalar(out=sf[0:32, sl], in0=sf[0:32, sl], scalar1=-1.0, scalar2=None, op0=ALU.mult), [sf], [sf])
                cx.dma("sp", of[12 * 128:12 * 128 + 64, :], sf[0:64, :], sf, False)
        cx.finish()
    return nc


_A_CACHE = {}


def run_A(x_tok, w_in_l, mod_l, g_norm_l, b_fgate_l, g_q_l, g_k_l, dt_bias_l):
    if "nc" not in _A_CACHE:
        _A_CACHE["nc"] = build_A()
    nc = _A_CACHE["nc"]
    idx = a_col_index()
    wp = np.zeros((D, NFC * 128), np.float32)
    wp[:, idx >= 0] = w_in_l[:, idx[idx >= 0]]
    wp = np.ascontiguousarray(wp.reshape(16, 128, NFC, 128).transpose(2, 1, 0, 3)).reshape(NFC, 128, 16 * 128)
    vec = np.zeros((128, 64), np.float32)
    vec[:, 0:16] = g_norm_l.reshape(16, 128).T
    vec[:, 16:32] = mod_l[D:2 * D].reshape(16, 128).T
    vec[:, 32:48] = mod_l[0:D].reshape(16, 128).T
    vec[:, 48] = g_q_l
    vec[:, 49] = g_k_l
    vec[0:4, 50] = b_fgate_l
    vec[32:48, 50] = dt_bias_l
    ones = np.ones((128, 128), np.float32)
    in_maps = []
    for k in range(NCORES):
        xs = x_tok[k * TPC:(k + 1) * TPC, :]
        in_maps.append({"xT": np.ascontiguousarray(xs.T).reshape(16, 128, TPC), "w": wp, "vec": vec, "ones": ones})
    res = run_bass_kernel_spmd(nc, in_maps, core_ids=list(range(NCORES)))
    ob = np.concatenate([np.asarray(res.results[k]["ob"]).reshape(OB_ROWS, TPC) for k in range(NCORES)], axis=1)
    of = np.concatenate([np.asarray(res.results[k]["of"]) for k in range(NCORES)], axis=1)
    return ob, of


NQB = 32
NKB = 64
SCALE = 128 ** -0.5
NEG = -30000.0


def _barrier(cx):
    names = ["pe", "act", "dve", "pool", "sp"]
    for a in names:
        E = cx.engs[a]
        for b in names:
            if a == b:
                continue
            F = cx.engs[b]
            if F.count > 0 and E.seen.get("e_" + b, 0) < F.count:
                E.e.wait_ge(F.sem, F.count)
                E.seen["e_" + b] = F.count


B_PHASES = {"ssm": True, "sb": True, "fox": True}


def build_B():
    nc = bass.Bass("TRN2", target_bir_lowering=False)
    dr = lambda n, s, dt, k="ExternalInput": nc.dram_tensor(n, list(s), dt, kind=k).ap()
    sqT_d = dr("sqT", [128, NQB * 128], BF16)
    skT_d = dr("skT", [128, S], BF16)
    sv_d = dr("sv", [128, NKB, 128], BF16)
    fqT_d = dr("fqT", [128, NQB * 128], BF16)
    fkT_d = dr("fkT", [128, S], BF16)
    fv_d = dr("fv", [128, NKB, 130], BF16)
    logf_d = dr("logf", [128, NKB], F32)
    mf_d = dr("mf", [128, 4 * 128], F32)
    cst_d = dr("cst", [128, 5 * 128], F32)
    sv_small_d = dr("small", [128, 32], F32)
    raw_d = dr("raw", [3, 128, S], F32)
    dt_d = dr("dt", [128, NKB * 2], F32)
    zs_d = dr("zs", [128, NKB, 128], BF16)
    osb_d = dr("osb", [128, NQB * 128], BF16, "ExternalOutput")
    ofx_d = dr("ofx", [128, NQB, 128], BF16, "ExternalOutput")
    yss_d = dr("yss", [128, NKB, 128], F32, "ExternalOutput")
    with ExitStack() as es:
        cx = Ctx(nc, es)
        cst = cx.sb("cst", [128, 5 * 128], F32)
        tri_s = cst[:, 0:128]
        tri_i = cst[:, 128:256]
        ones = cst[:, 256:384]
        ident = cst[:, 384:512]
        negm = cst[:, 512:640]
        mf = cx.sb("mf", [128, 4 * 128], F32)
        mb = cx.sb("mb", [128, 4 * 128], BF16)
        identb = cx.sb("identb", [128, 128], BF16)
        sm = cx.sb("sm", [128, 32], F32)
        ps = [cx.ps("ps%d" % i) for i in range(7)]
        cx.dma("sp", cst[:], cst_d, cst, True)
        cx.dma("sp", mf[:], mf_d, mf, True)
        cx.dma("sp", sm[:], sv_small_d, sm, True)
        cx.op("dve", lambda e: e.tensor_copy(out=mb[:], in_=mf[:]), [mf], [mb])
        cx.op("dve", lambda e: e.tensor_copy(out=identb[:], in_=ident), [cst], [identb])

        with ExitStack() as es2:
            outer = cx.es
            cx.es = es2
            raw = cx.sb("raw", [128, S + 3], F32)
            acc = cx.sb("acc", [128, S], F32)
            xs = cx.sb("xs", [128, S], F32)
            BT = cx.sb("BT", [128, S], BF16)
            CT = cx.sb("CT", [128, S], BF16)
            dt = cx.sb("dt", [128, NKB * 2], F32)
            dta = cx.sb("dta", [128, NKB * 2], F32)
            nacum = cx.sb("nacum", [128, NKB * 2], F32)
            zs = cx.sb("zs", [128, NKB, 128], BF16)
            aneg = cx.sb("aneg", [128, 2], F32)
            hT = [cx.sb("hT%d" % i, [128, 64], F32) for i in range(2)]
            hTb = [cx.sb("hTb%d" % i, [128, 64], BF16) for i in range(2)]
            cbs = [cx.sb("cbs%d" % i, [128, 128], F32) for i in range(2)]
            xtok = [cx.sb("xtok%d" % i, [128, 128], F32) for i in range(2)]
            btok = [cx.sb("btok%d" % i, [128, 128], BF16) for i in range(2)]
            dbc = [cx.sb("dbc%d" % i, [128, 128], F32) for i in range(2)]
            tng = [cx.sb("tng%d" % i, [128, 128], F32) for i in range(2)]
            dec = [cx.sb("dec%d" % i, [128, 128], F32) for i in range(2)]
            eac = [cx.sb("eac%d" % i, [128, 128], F32) for i in range(2)]
            MT = [cx.sb("MT%d" % i, [128, 128], BF16) for i in range(2)]
            CsT = [cx.sb("CsT%d" % i, [128, 128], BF16) for i in range(2)]
            xdt = [cx.sb("xdt%d" % i, [128, 64], BF16) for i in range(2)]
            xw = [cx.sb("xw%d" % i, [128, 64], BF16) for i in range(2)]
            yt = [cx.sb("yt%d" % i, [128, 64], F32) for i in range(2)]
            yst = [cx.sb("yst%d" % i, [128, 8, 128], F32) for i in range(2)]
            psb = cx.ps("psb", [128, 1024], BF16)
            cx.es = outer
            cx.dma("sp", dt[:], dt_d, dt, True)
            cx.dma("sp", zs[:], zs_d, zs, True)
            cx.op("pool", lambda e: e.memset(raw[:, 0:3], 0.0), [], [raw])
            for i in range(2):
                cx.op("pool", lambda e, i=i: e.memset(hT[i][:], 0.0), [], [hT[i]])
            act(cx, aneg[:], sm[:, 1:3], AF.Exp, [sm], [aneg])
            cx.op("dve", lambda e: e.tensor_scalar(out=aneg[:], in0=aneg[:], scalar1=-1.0, scalar2=None, op0=ALU.mult), [aneg], [aneg])
            dt3 = dt[:].rearrange("p (b h) -> p b h", h=2)
            dta3 = dta[:].rearrange("p (b h) -> p b h", h=2)
            for hh in range(2):
                cx.op("dve", lambda e, hh=hh: e.tensor_scalar(out=dta3[:, :, hh], in0=dt3[:, :, hh], scalar1=aneg[:, hh:hh + 1], scalar2=None, op0=ALU.mult), [dt, aneg], [dta])
            cx.op("pe", lambda e: e.matmul(ps[0][:, 0:128], lhsT=tri_i, rhs=dta[:], start=True, stop=True), [cst, dta], [ps[0]])
            cx.op("dve", lambda e: e.tensor_scalar(out=nacum[:], in0=ps[0][:, 0:128], scalar1=-1.0, scalar2=None, op0=ALU.mult), [ps[0]], [nacum])
            for g, dst in enumerate((xs, BT, CT)):
                cx.dma("sp", raw[:, 3:3 + S], raw_d[g], raw, True)
                c0 = 8 + 4 * g
                cx.op("dve", lambda e, c0=c0, g=g: e.tensor_scalar(out=acc[:], in0=raw[:, 3:3 + S], scalar1=sm[:, c0 + 3:c0 + 4], scalar2=sm[:, 20 + g:21 + g], op0=ALU.mult, op1=ALU.add),
                      [raw, sm], [acc])
                for wi in (2, 1, 0):
                    cx.op("dve", lambda e, c0=c0, wi=wi: e.scalar_tensor_tensor(out=acc[:], in0=raw[:, wi:wi + S], scalar=sm[:, c0 + wi:c0 + wi + 1], in1=acc[:], op0=ALU.mult, op1=ALU.add),
                          [raw, sm, acc], [acc])
                act(cx, dst[:], acc[:], AF.Silu, [acc], [dst])
            for ck in (range(NKB) if B_PHASES["ssm"] else []):
                b2 = ck % 2
                blk = slice(ck * 128, (ck + 1) * 128)
                cx.op("pe", lambda e, blk=blk: e.matmul(ps[1][:, 0:128], lhsT=BT[:, blk], rhs=CT[:, blk], start=True, stop=True), [BT, CT], [ps[1]])
                act(cx, cbs[b2][:], ps[1][:, 0:128], AF.Copy, [ps[1]], [cbs[b2]])
                cx.op("pe", lambda e, blk=blk: e.transpose(ps[2][:, 0:128], xs[:, blk], ident), [xs, cst], [ps[2]])
                cx.op("dve", lambda e, b2=b2: e.tensor_copy(out=xtok[b2][:], in_=ps[2][:, 0:128]), [ps[2]], [xtok[b2]])
                cx.op("pe", lambda e, blk=blk: e.transpose(psb[:, 0:128], BT[:, blk], identb[:]), [BT, identb], [psb])
                act(cx, btok[b2][:], psb[:, 0:128], AF.Copy, [psb], [btok[b2]])
                for hh in range(2):
                    col = ck * 2 + hh
                    hs = slice(hh * 64, (hh + 1) * 64)
                    k2 = col % 2
                    cx.op("dve", lambda e, k2=k2, col=col: e.tensor_scalar(out=dbc[k2][:], in0=ones, scalar1=dta[:, col:col + 1], scalar2=None, op0=ALU.mult), [cst, dta], [dbc[k2]])
                    cx.op("pe", lambda e, k2=k2: e.matmul(ps[3 + k2][:, 0:128], lhsT=dbc[k2][:], rhs=tri_i, start=True, stop=True), [dbc[k2], cst], [ps[3 + k2]])
                    pab = ps[3 + k2]
                    cx.op("dve", lambda e, k2=k2, pab=pab: e.tensor_tensor(out=tng[k2][:], in0=pab[:, 0:128], in1=negm, op=ALU.add), [pab, cst], [tng[k2]])
                    act(cx, dec[k2][:], tng[k2][:], AF.Exp, [tng[k2], nacum], [dec[k2]], bias=nacum[:, col:col + 1], scale=1.0)
                    act(cx, eac[k2][:], pab[:, 0:128], AF.Exp, [pab], [eac[k2]])
                    cx.op("dve", lambda e, k2=k2, b2=b2: e.tensor_tensor(out=MT[k2][:], in0=cbs[b2][:], in1=dec[k2][:], op=ALU.mult), [cbs[b2], dec[k2]], [MT[k2]])
                    cx.op("dve", lambda e, k2=k2, b2=b2, hs=hs, col=col: e.tensor_scalar(out=xdt[k2][:], in0=xtok[b2][:, hs], scalar1=dt[:, col:col + 1], scalar2=None, op0=ALU.mult),
                          [xtok[b2], dt], [xdt[k2]])
                    cx.op("pool", lambda e, k2=k2, blk=blk: e.tensor_tensor(out=CsT[k2][:], in0=CT[:, blk], in1=eac[k2][:], op=ALU.mult), [CT, eac[k2]], [CsT[k2]])
                    act(cx, hTb[hh][:], hT[hh][:], AF.Copy, [hT[hh]], [hTb[hh]])
                    py = ps[5]
                    cx.op("pe", lambda e, k2=k2: e.matmul(py[:, hs.start:hs.stop], lhsT=MT[k2][:], rhs=xdt[k2][:], start=True, stop=False) if False else
                          e.matmul(py[:, 0:64], lhsT=MT[k2][:], rhs=xdt[k2][:], start=True, stop=False), [MT[k2], xdt[k2]], [py], inc=False)
                    cx.op("pe", lambda e, k2=k2, hh=hh: e.matmul(py[:, 0:64], lhsT=CsT[k2][:], rhs=hTb[hh][:], start=False, stop=True), [CsT[k2], hTb[hh]], [py])
                    cx.op("dve", lambda e, k2=k2: e.tensor_scalar(out=xw[k2][:], in0=xdt[k2][:], scalar1=dec[k2][:, 127:128], scalar2=None, op0=ALU.mult), [xdt[k2], dec[k2]], [xw[k2]])
                    pss_ = ps[6]
                    cx.op("pe", lambda e, k2=k2, b2=b2: e.matmul(pss_[:, 0:64], lhsT=btok[b2][:], rhs=xw[k2][:], start=True, stop=True), [btok[b2], xw[k2]], [pss_])
                    cx.op("dve", lambda e, k2=k2, hh=hh: e.scalar_tensor_tensor(out=hT[hh][:], in0=hT[hh][:], scalar=eac[k2][:, 127:128], in1=pss_[:, 0:64], op0=ALU.mult, op1=ALU.add),
                          [hT[hh], eac[k2], pss_], [hT[hh]])
                    cx.op("dve", lambda e, k2=k2, b2=b2, hs=hs, hh=hh: e.scalar_tensor_tensor(out=yt[k2][:], in0=xtok[b2][:, hs], scalar=sm[:, 3 + hh:4 + hh], in1=py[:, 0:64], op0=ALU.mult, op1=ALU.add),
                          [xtok[b2], sm, py], [yt[k2]])
                    ys = yst[(ck // 8) % 2]
                    cx.op("pool", lambda e, k2=k2, ys=ys, ck=ck, hs=hs: e.tensor_tensor(out=ys[:, ck % 8, hs], in0=yt[k2][:], in1=zs[:, ck, hs], op=ALU.mult), [yt[k2], zs], [ys])
                if ck % 8 == 7:
                    ys = yst[(ck // 8) % 2]
                    cx.dma("sp", yss_d[:, ck - 7:ck + 1, :], ys[:], ys, False)
            for ys in yst:
                if ys.d.dsem is not None:
                    cx.engs["sp"].e.wait_ge(ys.d.dsem, 16 * ys.d.dcount)
            spE = cx.engs["sp"]
            spE.e.engine_nop().then_inc(spE.sem, 1) if hasattr(spE.e, "engine_nop") else spE.e.nop().then_inc(spE.sem, 1)
            spE.count += 1
            _barrier(cx)

        skT = cx.sb("skT", [128, S], BF16)
        sqT = cx.sb("sqT", [128, NQB * 128], BF16)
        sv = cx.sb("sv", [128, NKB, 128], BF16)
        fkT = cx.sb("fkT", [128, S], BF16)
        fqT = cx.sb("fqT", [128, NQB * 128], BF16)
        fv = cx.sb("fv", [128, NKB, 130], BF16)
        logf = cx.sb("logf", [128, NKB], F32)
        tot = cx.sb("tot", [128, NKB], F32)
        pre = cx.sb("pre", [128, NKB + 2], F32)
        negfc = cx.sb("negfc", [128, NKB], F32)
        fcs = cx.sb("fcs", [128, NQB], F32)
        fdf = cx.sb("fdf", [128, NQB], F32)
        bias_all = cx.sb("bias_all", [128, NQB, NKB], F32)
        R = 3
        Wt = [cx.sb("W%d" % i, [128, 128], BF16) for i in range(R)]
        pfo = cx.ps("pfo")
        osb_st = [cx.sb("osbst%d" % i, [128, 8 * 128], BF16) for i in range(2)]
        ofx_st = [cx.sb("ofxst%d" % i, [128, 8, 128], BF16) for i in range(2)]
        rec = [cx.sb("rec%d" % i, [128, 1], F32) for i in range(2)]
        for t_, d_, q in ((skT, skT_d, "sp"), (sqT, sqT_d, "act"), (sv, sv_d, "sp"), (fkT, fkT_d, "act"), (fqT, fqT_d, "sp"), (fv, fv_d, "act"), (logf, logf_d, "sp")):
            cx.dma(q, t_[:], d_, t_, True)

        cx.op("pe", lambda e: e.matmul(ps[6][:, 0:NKB], lhsT=tri_i, rhs=logf[:], start=True, stop=True), [cst, logf], [ps[6]])
        cx.op("pe", lambda e: e.matmul(ps[6][:, 128:128 + NKB], lhsT=ones, rhs=logf[:], start=True, stop=True), [cst, logf], [ps[6]])
        cx.op("dve", lambda e: e.tensor_copy(out=tot[:], in_=ps[6][:, 128:128 + NKB]), [ps[6]], [tot])
        cx.op("dve", lambda e: e.memset(pre[:, 0:1], 0.0), [], [pre])
        for b in range(NKB):
            cx.op("dve", lambda e, b=b: e.tensor_tensor(out=pre[:, b + 1:b + 2], in0=pre[:, b:b + 1], in1=tot[:, b:b + 1], op=ALU.add), [pre, tot], [pre])
        cx.op("dve", lambda e: e.scalar_tensor_tensor(out=negfc[:], in0=ps[6][:, 0:NKB], scalar=-1.0, in1=pre[:, 0:NKB], op0=ALU.mult, op1=ALU.subtract), [ps[6], pre], [negfc])
        pre_e = pre[:, 1:1 + 2 * NQB].rearrange("p (i two) -> p i two", two=2)
        cx.op("dve", lambda e: e.tensor_tensor(out=fdf[:], in0=pre_e[:, :, 1], in1=pre_e[:, :, 0], op=ALU.subtract), [pre], [fdf])
        cx.op("dve", lambda e: e.scalar_tensor_tensor(out=fcs[:], in0=fdf[:], scalar=sm[:, 0:1], in1=pre_e[:, :, 0], op0=ALU.mult, op1=ALU.add), [fdf, sm, pre], [fcs])
        for i in range(NQB):
            cx.op("dve", lambda e, i=i: e.tensor_scalar(out=bias_all[:, i, :], in0=negfc[:], scalar1=fcs[:, i:i + 1], scalar2=0.0, op0=ALU.add, op1=ALU.min), [negfc, fcs], [bias_all])

        pairs = [(i, m) for i in range(NQB) for m in range(i + 1)]
        NP = len(pairs)
        Zp = [ps[0], ps[1]]
        Bp = [ps[2], ps[3]]
        Op = [ps[4], ps[4]]
        carry = ps[5]
        tri_b = cx.sb("tri_b", [128, 128], BF16)
        ones_b = cx.sb("ones_b", [128, 128], BF16)
        mbp = cx.sb("mbp", [128, 256], BF16)
        cx.op("dve", lambda e: e.tensor_copy(out=tri_b[:], in_=tri_s), [cst], [tri_b])
        cx.op("dve", lambda e: e.tensor_copy(out=ones_b[:], in_=ones), [cst], [ones_b])
        cx.op("dve", lambda e: e.tensor_copy(out=mbp[:, 0:128], in_=mf[:, 128:256]), [mf], [mbp])
        cx.op("dve", lambda e: e.tensor_copy(out=mbp[:, 128:256], in_=mf[:, 0:128]), [mf], [mbp])
        E2 = [cx.sb("E2%d" % i, [128, 256], F32) for i in range(R)]
        L2 = [cx.sb("L2%d" % i, [128, 256], BF16) for i in range(R)]
        t12 = [cx.sb("t12%d" % i, [128, 256], F32) for i in range(R)]
        t22 = [cx.sb("t22%d" % i, [128, 256], F32) for i in range(R)]
        W2 = [cx.sb("W2%d" % i, [128, 256], BF16) for i in range(R)]
        csb = [cx.sb("csb%d" % i, [128, 128], F32) for i in range(3)]

        def kbs(n):
            i, m = pairs[n]
            return i, m, 2 * i + 1 - 2 * m, 2 * i - 2 * m

        def sb_s0(n):
            i, m, ka, kb_ = kbs(n)
            z = Zp[n % 2]
            cx.op("pe", lambda e: e.matmul(z[:, 0:128], lhsT=skT[:, ka * 128:(ka + 1) * 128], rhs=sqT[:, i * 128:(i + 1) * 128], start=True, stop=True), [skT, sqT], [z], inc=False)
            cx.op("pe", lambda e: e.matmul(z[:, 128:256], lhsT=skT[:, kb_ * 128:(kb_ + 1) * 128], rhs=sqT[:, i * 128:(i + 1) * 128], start=True, stop=True), [skT, sqT], [z])

        def sb_s1(n):
            i, m, ka, kb_ = kbs(n)
            z = Zp[n % 2]
            r = n % R
            act(cx, E2[r][:], z[:, 0:256], AF.Exp, [z], [E2[r]], scale=SCALE)
            act(cx, L2[r][:], E2[r][:], AF.Ln, [E2[r]], [L2[r]], bias=1.0, scale=1.0)
            if m == 0:
                cx.op("pool", lambda e: e.tensor_tensor(out=L2[r][:], in0=L2[r][:], in1=mbp[:], op=ALU.mult), [L2[r], mbp], [L2[r]])
            cx.op("dve", lambda e: e.scalar_tensor_tensor(out=t12[r][:], in0=z[:, 0:256], scalar=SCALE, in1=L2[r][:], op0=ALU.mult, op1=ALU.subtract), [z, L2[r]], [t12[r]])

        def sb_s2(n):
            i, m, ka, kb_ = kbs(n)
            r = n % R
            bp = Bp[n % 2]
            cx.op("pe", lambda e: e.matmul(bp[:, 0:256], lhsT=tri_b[:], rhs=L2[r][:], start=True, stop=False), [tri_b, L2[r]], [bp], inc=False)
            cx.op("pe", lambda e: e.matmul(bp[:, 128:256], lhsT=ones_b[:], rhs=L2[r][:, 0:128], start=False, stop=True), [ones_b, L2[r]], [bp])
            if m < i:
                cx.op("pe", lambda e: e.matmul(carry[:, 0:128], lhsT=ones_b[:], rhs=L2[r][:, 0:128], start=(m == 0), stop=False), [ones_b, L2[r]], [carry], inc=False)
                cx.op("pe", lambda e: e.matmul(carry[:, 0:128], lhsT=ones_b[:], rhs=L2[r][:, 128:256], start=False, stop=True), [ones_b, L2[r]], [carry])
                c_ = csb[(n + 1) % 3]
                cx.op("dve", lambda e: e.tensor_copy(out=c_[:], in_=carry[:, 0:128]), [carry], [c_])

        def sb_s3(n):
            i, m, ka, kb_ = kbs(n)
            r = n % R
            bp = Bp[n % 2]
            if m > 0:
                c_ = csb[n % 3]
                t3 = t12[r][:].rearrange("p (a b) -> p a b", a=2, b=128)
                cx.op("pool", lambda e: e.tensor_tensor(out=t3, in0=t3, in1=c_[:].unsqueeze(1).to_broadcast([128, 2, 128]), op=ALU.subtract), [t12[r], c_], [t12[r]])
            cx.op("dve", lambda e: e.tensor_tensor(out=t22[r][:], in0=t12[r][:], in1=bp[:, 0:256], op=ALU.subtract), [t12[r], bp], [t22[r]])
            act(cx, W2[r][:], t22[r][:], AF.Exp, [t22[r]], [W2[r]])
            if m == 0:
                cx.op("pool", lambda e: e.tensor_tensor(out=W2[r][:], in0=W2[r][:], in1=mbp[:], op=ALU.mult), [W2[r], mbp], [W2[r]])

        def sb_s4(n):
            i, m, ka, kb_ = kbs(n)
            r = n % R
            o = Op[i % 2]
            last = (m == i)
            cx.op("pe", lambda e: e.matmul(o[:, 0:128], lhsT=sv[:, ka, :], rhs=W2[r][:, 0:128], start=(m == 0), stop=False), [sv, W2[r]], [o], inc=False)
            cx.op("pe", lambda e: e.matmul(o[:, 0:128], lhsT=sv[:, kb_, :], rhs=W2[r][:, 128:256], start=False, stop=last), [sv, W2[r]], [o])
            if last:
                st = osb_st[(i // 8) % 2]
                act(cx, st[:, (i % 8) * 128:(i % 8 + 1) * 128], o[:, 0:128], AF.Copy, [o], [st])
                if i % 8 == 7:
                    cx.dma("sp", osb_d[:, (i - 7) * 128:(i + 1) * 128], st[:], st, False)

        ftiles = [(i, kb) for i in range(NQB) for kb in range(0, 2 * i + 2)]
        NF = len(ftiles)
        Pt = Wt

        Zf = [ps[6], ps[6], ps[6]]

        def fx_s0(n):
            i, kb = ftiles[n]
            z = Zf[n % 3]
            cx.op("pe", lambda e: e.matmul(z[:, 0:128], lhsT=fkT[:, kb * 128:(kb + 1) * 128], rhs=fqT[:, i * 128:(i + 1) * 128], start=True, stop=True), [fkT, fqT], [z])

        def fx_s1(n):
            i, kb = ftiles[n]
            z = Zf[n % 3]
            r = n % R
            act(cx, Pt[r][:], z[:, 0:128], AF.Exp, [z, bias_all], [Pt[r]], scale=SCALE, bias=bias_all[:, i, kb:kb + 1])
            if kb >= 2 * i:
                mi = 2 if kb == 2 * i else 3
                cx.op("dve", lambda e: e.tensor_tensor(out=Pt[r][:], in0=Pt[r][:], in1=mb[:, mi * 128:(mi + 1) * 128], op=ALU.mult), [Pt[r], mb], [Pt[r]])

        def fx_s2(n):
            i, kb = ftiles[n]
            r = n % R
            o = pfo
            first = (kb == 0)
            last = (kb == 2 * i + 1)
            cx.op("pe", lambda e: e.matmul(o[:, 0:129], lhsT=Pt[r][:], rhs=fv[:, kb, 0:129], start=first, stop=last), [Pt[r], fv], [o], inc=last)
            if last:
                st = ofx_st[(i // 8) % 2]
                rc = rec[i % 2]
                cx.op("dve", lambda e: e.reciprocal(out=rc[:], in_=o[:, 128:129]), [o], [rc])
                cx.op("dve", lambda e: e.tensor_scalar(out=st[:, i % 8, :], in0=o[:, 0:128], scalar1=rc[:, 0:1], scalar2=None, op0=ALU.mult), [o, rc], [st])
                if i % 8 == 7:
                    cx.dma("sp", ofx_d[:, i - 7:i + 1, :], st[:], st, False)

        fi = 0

        def fox_emit(k):
            if B_PHASES["fox"] and 0 <= k < NF:
                fx_s0(k)
                fx_s1(k)
            if B_PHASES["fox"] and 0 <= k - 2 < NF:
                fx_s2(k - 2)

        nsteps = NP + 3 if B_PHASES["sb"] else 0
        for step in range(nsteps):
            if step < NP:
                sb_s0(step)
                sb_s1(step)
            fox_emit(fi)
            fi += 1
            if 0 <= step - 1 < NP:
                sb_s2(step - 1)
            fox_emit(fi)
            fi += 1
            if 0 <= step - 2 < NP:
                sb_s3(step - 2)
            if 0 <= step - 3 < NP:
                sb_s4(step - 3)
        while fi < NF + 2:
            fox_emit(fi)
            fi += 1
        cx.finish()
    return nc


_B_CACHE = {}


def _b_consts():
    j = np.arange(128)
    tri_s = (j[:, None] > j[None, :]).astype(np.float32)
    tri_i = (j[:, None] <= j[None, :]).astype(np.float32)
    ones = np.ones((128, 128), np.float32)
    ident = np.eye(128, dtype=np.float32)
    negm = np.where(j[None, :] >= j[:, None], 0.0, NEG).astype(np.float32)
    return np.concatenate([tri_s, tri_i, ones, ident, negm], axis=1)


def run_B(ob, of, conv_w_l, conv_b_l, a_log_l, d_skip_l, trace=False):
    if "nc" not in _B_CACHE:
        _B_CACHE["nc"] = build_B()
    nc = _B_CACHE["nc"]
    bf = ob.dtype
    cst = _b_consts()
    kk = np.arange(128)
    d_strict = (kk[:, None] < kk[None, :]).astype(np.float32)
    d_incl = (kk[:, None] <= kk[None, :]).astype(np.float32)
    zeros = np.zeros((128, 128), np.float32)
    onesm = np.ones((128, 128), np.float32)
    in_maps = []
    for c in range(NCORES):
        h, u, g = c % 4, c // 4, c // 4
        blk = lambda a: a.reshape(128, NKB, 128)
        sel = lambda a: np.ascontiguousarray(blk(a)[:, u::2, :]).reshape(128, NQB * 128)
        tokm = lambda a: np.ascontiguousarray(a.T.reshape(NKB, 128, 128).transpose(1, 0, 2))
        fvt = np.zeros((128, NKB, 130), bf)
        fvt[:, :, 0:128] = tokm(ob[2560 + h * 128:2560 + (h + 1) * 128])
        fvt[:, :, 128] = 1.0
        if u == 0:
            mf = np.concatenate([d_strict, zeros, d_incl, zeros], axis=1)
        else:
            mf = np.concatenate([onesm, d_strict, onesm, d_incl], axis=1)
        small = np.zeros((128, 32), np.float32)
        small[:, 0] = u
        for hh in range(2):
            small[:, 1 + hh] = a_log_l[2 * c + hh]
            small[:, 3 + hh] = d_skip_l[2 * c + hh]
        chs = [np.arange(2 * c * 64, 2 * c * 64 + 128), 1024 + g * 128 + np.arange(128), 1280 + g * 128 + np.arange(128)]
        for gi, ch in enumerate(chs):
            small[:, 8 + 4 * gi:12 + 4 * gi] = conv_w_l[:, ch].T
            small[:, 20 + gi] = conv_b_l[ch]
        raw = np.stack([of[ch, :] for ch in chs]).astype(np.float32)
        dtc = np.stack([of[1568 + 2 * c + hh, :].reshape(NKB, 128).T for hh in range(2)], axis=-1)
        in_maps.append({
            "sqT": sel(ob[h * 128:(h + 1) * 128]), "skT": np.ascontiguousarray(ob[512 + h * 128:512 + (h + 1) * 128]),
            "sv": tokm(ob[1024 + h * 128:1024 + (h + 1) * 128]),
            "fqT": sel(ob[1536 + h * 128:1536 + (h + 1) * 128]), "fkT": np.ascontiguousarray(ob[2048 + h * 128:2048 + (h + 1) * 128]),
            "fv": fvt, "logf": np.ascontiguousarray(of[1536 + h].reshape(NKB, 128).T),
            "mf": mf, "cst": cst, "small": small, "raw": np.ascontiguousarray(raw),
            "dt": np.ascontiguousarray(dtc.reshape(128, NKB * 2)),
            "zs": tokm(ob[3072 + 2 * c * 64:3072 + 2 * c * 64 + 128]),
        })
    res = run_bass_kernel_spmd(nc, in_maps, core_ids=list(range(NCORES)), **({"trace": True} if trace else {}))
    _B_CACHE["exec_ns"] = getattr(res, "exec_time_ns", None)
    o_sbT = np.zeros((512, NKB, 128), bf)
    o_fox = np.zeros((NKB, 128, 512), bf)
    y_ssm = np.zeros((NKB, 128, 1024), np.float32)
    for c in range(NCORES):
        h, u = c % 4, c // 4
        r = res.results[c]
        o_sbT[h * 128:(h + 1) * 128, u::2, :] = np.asarray(r["osb"]).reshape(128, NQB, 128)
        o_fox[u::2, :, h * 128:(h + 1) * 128] = np.asarray(r["ofx"]).transpose(1, 0, 2)
        y_ssm[:, :, 2 * c * 64:2 * c * 64 + 128] = np.asarray(r["yss"]).transpose(1, 0, 2)
    return o_sbT.reshape(512, S), o_fox.reshape(S, 512), y_ssm.reshape(S, 1024)


BIG = 1.0e4


def build_C():
    nc = bass.Bass("TRN2", target_bir_lowering=False)
    T = TPC
    dr = lambda n, s, dt, k="ExternalInput": nc.dram_tensor(n, list(s), dt, kind=k).ap()
    xT_d = dr("xT", [16, 128, T], F32)
    yT_d = dr("yT", [8, 128, T], F32)
    osb_d = dr("osbT", [4, 128, T], BF16)
    ofx_d = dr("ofxT", [4, 128, T], BF16)
    gat_d = dr("gates", [48, 128, T], BF16)
    vec_d = dr("vec", [128, 128], F32)
    cst_d = dr("cst", [128, 3 * 128], F32)
    selE_d = dr("selE", [16, 16 * 128], F32)
    wbr_d = dr("wbr", [16, 128, 16 * 128], F32)
    wo_d = dr("wo", [16, 128, 16 * 128], F32)
    wr_d = dr("wr", [128, 16 * 16], F32)
    wg_d = dr("wg", [NEXP, 8, 128, 16 * 128], F32)
    wu_d = dr("wu", [NEXP, 8, 128, 16 * 128], F32)
    wd_d = dr("wd", [NEXP, 2, 4, 128, D], F32)
    xo_d = dr("xo", [16, 128, T], F32, "ExternalOutput")
    with ExitStack() as es:
        cx = Ctx(nc, es)
        xt = cx.sb("xt", [128, 16, T], F32)
        h2 = cx.sb("h2", [128, 16, T], BF16)
        vt = cx.sb("vt", [128, 128], F32)
        cst = cx.sb("cst", [128, 3 * 128], F32)
        ones = cst[:, 0:128]
        ident = cst[:, 128:256]
        brt = cst[:, 256:384]
        selE = cx.sb("selE", [16, 16 * 128], F32)
        gs2 = cx.sb("gs2", [128, 16], F32)
        comb = cx.sb("comb", [128, 128], F32)
        combT = cx.sb("combT", [16, T], F32)
        ps = [cx.ps("ps%d" % i) for i in range(8)]
        xdeps = [Dep("x%d" % k) for k in range(16)]
        hdeps = [Dep("h%d" % k) for k in range(16)]
        cx.dma("sp", vt[:], vec_d, vt, True)
        cx.dma("sp", cst[:], cst_d, cst, True)
        cx.dma("sp", selE[:], selE_d, selE, True)
        for kc in range(16):
            cx.dma("sp" if kc % 2 else "act", xt[:, kc, :], xT_d[kc], xdeps[kc], True)
        cx.op("dve", lambda e: e.scalar_tensor_tensor(out=gs2[:], in0=vt[:, 16:32], scalar=1.0, in1=vt[:, 0:16], op0=ALU.add, op1=ALU.mult), [vt], [gs2])

        with ExitStack() as es2:
            outer = cx.es
            cx.es = es2
            osb = cx.sb("osb", [128, 4, T], BF16)
            ofx = cx.sb("ofx", [128, 4, T], BF16)
            oss = cx.sb("oss", [128, 8, T], BF16)
            mrg = h2
            sq = [cx.sb("sq%d" % i, [128, T], F32) for i in range(2)]
            rstd = cx.sb("rstd", [128, T], F32)
            tmp = [cx.sb("tmp%d" % i, [128, T], F32) for i in range(2)]
            gsl = [cx.sb("g%d" % i, [128, 3, T], BF16) for i in range(2)]
            wsl = [cx.sb("w%d" % i, [128, 16 * 128], BF16) for i in range(3)]
            m1 = [cx.sb("m1%d" % i, [128, 512], F32) for i in range(2)]
            m2 = [cx.sb("m2%d" % i, [128, 512], F32) for i in range(2)]
            wr = cx.sb("wr", [128, 16 * 16], F32)
            lgT = cx.sb("lgT", [16, T], F32)
            aff = cx.sb("aff", [128, 128], F32)
            sel = cx.sb("sel", [128, 128], F32)
            sel2 = cx.sb("sel2", [128, 128], F32)
            eq = cx.sb("eq", [128, 128], F32)
            r32 = [cx.sb("r32%d" % i, [128, 32], F32) for i in range(5)]
            r8 = [cx.sb("r8%d" % i, [128, 8], F32) for i in range(4)]
            cx.es = outer
            mdeps = hdeps
            cx.dma("sp", osb[:], osb_d.rearrange("k p t -> p k t"), osb, True)
            cx.dma("act", ofx[:], ofx_d.rearrange("k p t -> p k t"), ofx, True)
            cx.dma("sp", wr[:], wr_d, wr, True)
            wq = []

            def wload(src, i, n):
                wsx = wsl[n % 3]
                cx.dma("pool", wsx[:], src[i], wsx, True)
                return wsx
            for kc in range(8):
                s = sq[kc % 2]
                yc = tmp[kc % 2]
                cx.dma("sp" if kc % 2 else "act", yc[:], yT_d[kc], yc, True)
                act(cx, s[:], yc[:], AF.Square, [yc], [s])
                for tt in range(2):
                    cx.op("pe", lambda e, s=s, tt=tt, kc=kc: e.matmul(ps[tt][:, :], lhsT=ones, rhs=s[:, tt * 512:(tt + 1) * 512], start=(kc == 0), stop=(kc == 7)), [cst, s], [ps[tt]])
            for tt in range(2):
                act(cx, rstd[:, tt * 512:(tt + 1) * 512], ps[tt][:, :], AF.Sqrt, [ps[tt]], [rstd], scale=1.0 / 1024, bias=EPS)
            cx.op("dve", lambda e: e.reciprocal(out=rstd[:], in_=rstd[:]), [rstd], [rstd])
            for kc in range(8):
                yc = tmp[kc % 2]
                cx.dma("sp" if kc % 2 else "act", yc[:], yT_d[kc], yc, True)
                cx.op("dve", lambda e, kc=kc, yc=yc: e.scalar_tensor_tensor(out=oss[:, kc, :], in0=yc[:], scalar=vt[:, 80 + kc:81 + kc], in1=rstd[:], op0=ALU.mult, op1=ALU.mult),
                      [yc, vt, rstd], [oss])
            nw = 0
            pi = 2
            for dc in range(16):
                wsx = wload(wbr_d, dc, nw)
                nw += 1
                g = gsl[dc % 2]
                for br in range(3):
                    cx.dma("sp" if br % 2 else "act", g[:, br, :], gat_d[br * 16 + dc], g, True)
                for tt in range(2):
                    sl = slice(tt * 512, (tt + 1) * 512)
                    pp = [ps[(pi + k) % 8] for k in range(3)]
                    pi += 3
                    for br, (src, k0, nk) in enumerate(((osb, 0, 4), (ofx, 4, 4), (oss, 8, 8))):
                        for kk in range(nk):
                            cx.op("pe", lambda e, p=pp[br], src=src, kk=kk, k0=k0, nk=nk, wsx=wsx, sl=sl: e.matmul(p[:, :], lhsT=wsx[:, (k0 + kk) * 128:(k0 + kk + 1) * 128], rhs=src[:, kk, sl],
                                                                                                                     start=(kk == 0), stop=(kk == nk - 1)), [wsx, src], [pp[br]], inc=(kk == nk - 1))
                    a, b = m1[tt], m2[tt]
                    cx.op("dve", lambda e, a=a, g=g, sl=sl, p=pp[0]: e.tensor_tensor(out=a[:], in0=p[:, :], in1=g[:, 0, sl], op=ALU.mult), [pp[0], g], [a])
                    cx.op("dve", lambda e, b=b, g=g, sl=sl, p=pp[1]: e.tensor_tensor(out=b[:], in0=p[:, :], in1=g[:, 1, sl], op=ALU.mult), [pp[1], g], [b])
                    cx.op("pool", lambda e, a=a, b=b: e.tensor_tensor(out=a[:], in0=a[:], in1=b[:], op=ALU.add), [a, b], [a])
                    cx.op("dve", lambda e, b=b, g=g, sl=sl, p=pp[2]: e.tensor_tensor(out=b[:], in0=p[:, :], in1=g[:, 2, sl], op=ALU.mult), [pp[2], g], [b])
                    cx.op("pool", lambda e, a=a, b=b, dc=dc, sl=sl: e.tensor_tensor(out=mrg[:, dc, sl], in0=a[:], in1=b[:], op=ALU.add), [a, b], [mdeps[dc]])
            for dc in range(16):
                wsx = wload(wo_d, dc, nw)
                nw += 1
                for tt in range(2):
                    sl = slice(tt * 512, (tt + 1) * 512)
                    p = ps[pi % 8]
                    pi += 1
                    for kc in range(16):
                        cx.op("pe", lambda e, p=p, kc=kc, wsx=wsx, sl=sl: e.matmul(p[:, :], lhsT=wsx[:, kc * 128:(kc + 1) * 128], rhs=mrg[:, kc, sl], start=(kc == 0), stop=(kc == 15)),
                              [wsx, mdeps[kc]], [p], inc=(kc == 15))
                    cx.op("dve", lambda e, p=p, dc=dc, sl=sl: e.scalar_tensor_tensor(out=xt[:, dc, sl], in0=p[:, :], scalar=vt[:, 48 + dc:49 + dc], in1=xt[:, dc, sl], op0=ALU.mult, op1=ALU.add),
                          [p, vt, xdeps[dc]], [xdeps[dc]])
            for kc in range(16):
                s = sq[kc % 2]
                act(cx, s[:], xt[:, kc, :], AF.Square, [xdeps[kc]], [s])
                for tt in range(2):
                    cx.op("pe", lambda e, s=s, tt=tt, kc=kc: e.matmul(ps[tt][:, :], lhsT=ones, rhs=s[:, tt * 512:(tt + 1) * 512], start=(kc == 0), stop=(kc == 15)), [cst, s], [ps[tt]])
            for tt in range(2):
                act(cx, rstd[:, tt * 512:(tt + 1) * 512], ps[tt][:, :], AF.Sqrt, [ps[tt]], [rstd], scale=1.0 / D, bias=EPS)
            cx.op("dve", lambda e: e.reciprocal(out=rstd[:], in_=rstd[:]), [rstd], [rstd])
            for kc in range(16):
                tm = tmp[kc % 2]
                hf = sq[kc % 2]
                cx.op("pool", lambda e, kc=kc, tm=tm: e.tensor_tensor(out=tm[:], in0=xt[:, kc, :], in1=rstd[:], op=ALU.mult), [xdeps[kc], rstd], [tm])
                cx.op("dve", lambda e, kc=kc, tm=tm, hf=hf: e.tensor_scalar(out=hf[:], in0=tm[:], scalar1=gs2[:, kc:kc + 1], scalar2=vt[:, 32 + kc:33 + kc], op0=ALU.mult, op1=ALU.add),
                      [tm, gs2, vt], [hf])
                act(cx, h2[:, kc, :], hf[:], AF.Copy, [hf], [hdeps[kc]])
                for tt in range(2):
                    cx.op("pe", lambda e, hf=hf, tt=tt, kc=kc: e.matmul(ps[2 + tt][0:16, :], lhsT=wr[:, kc * 16:(kc + 1) * 16], rhs=hf[:, tt * 512:(tt + 1) * 512], start=(kc == 0), stop=(kc == 15)),
                          [wr, hf], [ps[2 + tt]])
            for tt in range(2):
                act(cx, lgT[:, tt * 512:(tt + 1) * 512], ps[2 + tt][0:16, :], AF.Copy, [ps[2 + tt]], [lgT])
            for b in range(8):
                cx.op("pe", lambda e, b=b: e.transpose(ps[4][:, b * 16:(b + 1) * 16], lgT[:, b * 128:(b + 1) * 128], ident[0:16, 0:16]), [lgT, cst], [ps[4]])
            act(cx, aff[:], ps[4][:, 0:128], AF.Sigmoid, [ps[4]], [aff])
            dv = lambda fn, r, w: cx.op("dve", fn, r, w)
            v3 = lambda t_, a, b: t_[:].rearrange("p (a b) -> p a b", a=a, b=b)
            bc = lambda t_, a, b: t_[:].unsqueeze(2).to_broadcast([128, a, b])
            dv(lambda e: e.tensor_tensor(out=sel[:], in0=aff[:], in1=brt, op=ALU.add), [aff, cst], [sel])
            dv(lambda e: e.tensor_reduce(out=r32[0][:], in_=v3(sel, 32, 4), axis=AX.X, op=ALU.max), [sel], [r32[0]])
            dv(lambda e: e.tensor_tensor(out=v3(eq, 32, 4), in0=v3(sel, 32, 4), in1=bc(r32[0], 32, 4), op=ALU.is_equal), [sel, r32[0]], [eq])
            dv(lambda e: e.scalar_tensor_tensor(out=sel2[:], in0=eq[:], scalar=-BIG, in1=sel[:], op0=ALU.mult, op1=ALU.add), [eq, sel], [sel2])
            dv(lambda e: e.tensor_reduce(out=r32[1][:], in_=v3(sel2, 32, 4), axis=AX.X, op=ALU.max), [sel2], [r32[1]])
            dv(lambda e: e.tensor_tensor(out=r32[2][:], in0=r32[0][:], in1=r32[1][:], op=ALU.add), [r32[0], r32[1]], [r32[2]])
            dv(lambda e: e.tensor_reduce(out=r8[0][:], in_=v3(r32[2], 8, 4), axis=AX.X, op=ALU.max), [r32[2]], [r8[0]])
            dv(lambda e: e.tensor_tensor(out=v3(r32[3], 8, 4), in0=v3(r32[2], 8, 4), in1=bc(r8[0], 8, 4), op=ALU.is_equal), [r32[2], r8[0]], [r32[3]])
            dv(lambda e: e.tensor_scalar(out=r32[4][:], in0=r32[3][:], scalar1=-1.0, scalar2=BIG, op0=ALU.add, op1=ALU.mult), [r32[3]], [r32[4]])
            dv(lambda e: e.tensor_tensor(out=v3(sel2, 32, 4), in0=v3(sel, 32, 4), in1=bc(r32[4], 32, 4), op=ALU.add), [sel, r32[4]], [sel2])
            dv(lambda e: e.tensor_reduce(out=r8[1][:], in_=v3(sel2, 8, 16), axis=AX.X, op=ALU.max), [sel2], [r8[1]])
            dv(lambda e: e.tensor_tensor(out=v3(eq, 8, 16), in0=v3(sel2, 8, 16), in1=bc(r8[1], 8, 16), op=ALU.is_equal), [sel2, r8[1]], [eq])
            dv(lambda e: e.scalar_tensor_tensor(out=sel[:], in0=eq[:], scalar=-BIG, in1=sel2[:], op0=ALU.mult, op1=ALU.add), [eq, sel2], [sel])
            dv(lambda e: e.tensor_reduce(out=r8[2][:], in_=v3(sel, 8, 16), axis=AX.X, op=ALU.max), [sel], [r8[2]])
            dv(lambda e: e.tensor_tensor(out=v3(sel2, 8, 16), in0=v3(sel, 8, 16), in1=bc(r8[2], 8, 16), op=ALU.is_equal), [sel, r8[2]], [sel2])
            dv(lambda e: e.tensor_tensor(out=eq[:], in0=eq[:], in1=sel2[:], op=ALU.add), [eq, sel2], [eq])
            dv(lambda e: e.tensor_tensor(out=sel[:], in0=eq[:], in1=aff[:], op=ALU.mult), [eq, aff], [sel])
            dv(lambda e: e.tensor_reduce(out=r8[3][:], in_=v3(sel, 8, 16), axis=AX.X, op=ALU.add), [sel], [r8[3]])
            dv(lambda e: e.reciprocal(out=r8[3][:], in_=r8[3][:]), [r8[3]], [r8[3]])
            dv(lambda e: e.tensor_tensor(out=v3(comb, 8, 16), in0=v3(sel, 8, 16), in1=bc(r8[3], 8, 16), op=ALU.mult), [sel, r8[3]], [comb])
            for b in range(8):
                cx.op("pe", lambda e, b=b: e.transpose(ps[5 + b // 4][0:16, (b % 4) * 128:(b % 4 + 1) * 128], comb[:, b * 16:(b + 1) * 16], ident), [comb, cst], [ps[5 + b // 4]])
            for tt in range(2):
                act(cx, combT[:, tt * 512:(tt + 1) * 512], ps[5 + tt][0:16, :], AF.Copy, [ps[5 + tt]], [combT])
            spE = cx.engs["sp"]
            _barrier(cx)

        wg = [cx.sb("wg%d" % i, [128, 16 * 128], BF16) for i in range(3)]
        wu = [cx.sb("wu%d" % i, [128, 16 * 128], BF16) for i in range(3)]
        wd = [cx.sb("wd%d" % i, [128, 4, D], BF16) for i in range(2)]
        actT = [cx.sb("actT%d" % i, [128, 4, T], BF16) for i in range(2)]
        cbc = [cx.sb("cbc%d" % i, [128, T], F32) for i in range(2)]
        sT = [cx.sb("sT%d" % i, [128, 512], F32) for i in range(2)]
        aT = [cx.sb("aT%d" % i, [128, 512], F32) for i in range(2)]
        nq = 0
        pi = 0
        units = [(e, hf) for e in range(NEXP) for hf in range(2)]

        def load_gu(e, fc, n):
            cx.dma("pool", wg[n % 3][:], wg_d[e, fc], wg[n % 3], True)
            cx.dma("pool", wu[n % 3][:], wu_d[e, fc], wu[n % 3], True)

        chunks = [(e, hf, f4) for (e, hf) in units for f4 in range(4)]
        for n in range(2):
            e, hf, f4 = chunks[n]
            load_gu(e, hf * 4 + f4, n)
        cx.dma("pool", wd[0][:], wd_d[0, 0].rearrange("f p d -> p f d"), wd[0], True)
        for ui, (e, hf) in enumerate(units):
            if hf == 0:
                cb = cbc[e % 2]
                for tt in range(2):
                    p = ps[6 + tt]
                    cx.op("pe", lambda e_, p=p, tt=tt, e=e: e_.matmul(p[:, :], lhsT=selE[:, e * 128:(e + 1) * 128], rhs=combT[:, tt * 512:(tt + 1) * 512], start=True, stop=True), [selE, combT], [p])
                    act(cx, cb[:, tt * 512:(tt + 1) * 512], p[:, :], AF.Copy, [p], [cb])
            cb = cbc[e % 2]
            at = actT[ui % 2]
            if ui + 1 < len(units):
                e2, hf2 = units[ui + 1]
                cx.dma("pool", wd[(ui + 1) % 2][:], wd_d[e2, hf2].rearrange("f p d -> p f d"), wd[(ui + 1) % 2], True)
            for f4 in range(4):
                n = ui * 4 + f4
                wgx, wux = wg[n % 3], wu[n % 3]
                for tt in range(2):
                    sl = slice(tt * 512, (tt + 1) * 512)
                    pg, pu = ps[pi % 4], ps[(pi + 1) % 4]
                    pi += 2
                    for kc in range(16):
                        cx.op("pe", lambda e_, pg=pg, kc=kc, wgx=wgx, sl=sl: e_.matmul(pg[:, :], lhsT=wgx[:, kc * 128:(kc + 1) * 128], rhs=h2[:, kc, sl], start=(kc == 0), stop=(kc == 15)),
                              [wgx, hdeps[kc]], [pg], inc=(kc == 15))
                    for kc in range(16):
                        cx.op("pe", lambda e_, pu=pu, kc=kc, wux=wux, sl=sl: e_.matmul(pu[:, :], lhsT=wux[:, kc * 128:(kc + 1) * 128], rhs=h2[:, kc, sl], start=(kc == 0), stop=(kc == 15)),
                              [wux, hdeps[kc]], [pu], inc=(kc == 15))
                    s_, a_ = sT[tt], aT[tt]
                    act(cx, s_[:], pg[:, :], AF.Silu, [pg], [s_])
                    cx.op("dve", lambda e_, a_=a_, s_=s_, pu=pu: e_.tensor_tensor(out=a_[:], in0=pu[:, :], in1=s_[:], op=ALU.mult), [pu, s_], [a_])
                    cx.op("pool", lambda e_, a_=a_, at=at, f4=f4, sl=sl, cb=cb: e_.tensor_tensor(out=at[:, f4, sl], in0=a_[:], in1=cb[:, sl], op=ALU.mult), [a_, cb], [at])
                if n + 2 < len(chunks):
                    e3, hf3, f43 = chunks[n + 2]
                    load_gu(e3, hf3 * 4 + f43, n + 2)
            wdx = wd[ui % 2]
            for dc in range(16):
                for tt in range(2):
                    sl = slice(tt * 512, (tt + 1) * 512)
                    p = ps[4 + (pi % 2)]
                    pi += 1
                    for f4 in range(4):
                        cx.op("pe", lambda e_, p=p, f4=f4, dc=dc, sl=sl, wdx=wdx, at=at: e_.matmul(p[:, :], lhsT=wdx[:, f4, dc * 128:(dc + 1) * 128], rhs=at[:, f4, sl], start=(f4 == 0), stop=(f4 == 3)),
                              [wdx, at], [p], inc=(f4 == 3))
                    cx.op("dve", lambda e_, p=p, dc=dc, sl=sl: e_.scalar_tensor_tensor(out=xt[:, dc, sl], in0=p[:, :], scalar=vt[:, 64 + dc:65 + dc], in1=xt[:, dc, sl], op0=ALU.mult, op1=ALU.add),
                          [p, vt, xdeps[dc]], [xdeps[dc]])
        for dc in range(16):
            cx.dma("sp" if dc % 2 else "act", xo_d[dc], xt[:, dc, :], xdeps[dc], False)
        cx.finish()
    return nc


_C_CACHE = {}


def _wlayout(wmat):
    K_, N_ = wmat.shape
    return np.ascontiguousarray(wmat.reshape(K_ // 128, 128, N_ // 128, 128).transpose(2, 1, 0, 3)).reshape(N_ // 128, 128, (K_ // 128) * 128)


def run_C(x_tok, o_sbT, o_fox, y_ssm, gatesT, mod_l, g_ffn_l, g_ssm_l, wb_sb, wb_fox, wb_ssm, w_out_l, w_router, b_router, wg_l, wu_l, wd_l):
    if "nc" not in _C_CACHE:
        _C_CACHE["nc"] = build_C()
    nc = _C_CACHE["nc"]
    T = TPC
    vec = np.zeros((128, 128), np.float32)
    col = lambda v: v.reshape(-1, 128).T
    vec[:, 0:16] = col(g_ffn_l)
    vec[:, 16:32] = col(mod_l[4 * D:5 * D])
    vec[:, 32:48] = col(mod_l[3 * D:4 * D])
    vec[:, 48:64] = col(mod_l[2 * D:3 * D])
    vec[:, 64:80] = col(mod_l[5 * D:6 * D])
    vec[:, 80:88] = col(g_ssm_l)
    cst = np.zeros((128, 384), np.float32)
    cst[:, 0:128] = 1.0
    cst[:, 128:256] = np.eye(128, dtype=np.float32)
    cst[:, 256:384] = np.tile(b_router[None, :], (128, 8))
    selE = np.zeros((16, 16 * 128), np.float32)
    for e in range(16):
        selE[e, e * 128:(e + 1) * 128] = 1.0
    wbr = _wlayout(np.concatenate([wb_sb, wb_fox, wb_ssm], axis=0))
    wo = _wlayout(w_out_l)
    wr = np.ascontiguousarray(w_router.reshape(16, 128, 16).transpose(1, 0, 2)).reshape(128, 256)
    lay = lambda w: np.ascontiguousarray(w.reshape(NEXP, 16, 128, 8, 128).transpose(0, 3, 2, 1, 4)).reshape(NEXP, 8, 128, 16 * 128)
    wg = lay(wg_l)
    wu = lay(wu_l)
    wd = np.ascontiguousarray(wd_l).reshape(NEXP, 2, 4, 128, D)
    in_maps = []
    for c in range(NCORES):
        ts = slice(c * T, (c + 1) * T)
        in_maps.append({
            "xT": np.ascontiguousarray(x_tok[ts].T).reshape(16, 128, T),
            "yT": np.ascontiguousarray(y_ssm[ts].T).reshape(8, 128, T),
            "osbT": np.ascontiguousarray(o_sbT[:, ts]).reshape(4, 128, T),
            "ofxT": np.ascontiguousarray(o_fox[ts].T).reshape(4, 128, T),
            "gates": np.ascontiguousarray(gatesT[:, ts]).reshape(48, 128, T),
            "vec": vec, "cst": cst, "selE": selE, "wbr": wbr, "wo": wo, "wr": wr, "wg": wg, "wu": wu, "wd": wd,
        })
    res = run_bass_kernel_spmd(nc, in_maps, core_ids=list(range(NCORES)))
    xo = np.zeros((S, D), np.float32)
    for c in range(NCORES):
        xo[c * T:(c + 1) * T] = np.asarray(res.results[c]["xo"]).reshape(D, T).T
    return xo


def kernel(x, c, w_ada, b_ada, g_norm_mix, w_in, b_fgate, g_q_fox, g_k_fox, conv_w, conv_b, dt_bias, a_log, d_skip,
           g_ssm_norm, w_branch_sb, w_branch_fox, w_branch_ssm, w_out, g_norm_ffn, w_router, b_router, w_e_gate, w_e_up, w_e_down):
    f = lambda a: np.asarray(a, dtype=np.float32)
    x_tok = f(x)[0]
    mod = run_M(f(c), f(w_ada), f(b_ada))
    for l in range(2):
        ob, of = run_A(x_tok, f(w_in[l]), mod[l], f(g_norm_mix[l]), f(b_fgate[l]), f(g_q_fox[l]), f(g_k_fox[l]), f(dt_bias[l]))
        o_sbT, o_fox, y_ssm = run_B(ob, of, f(conv_w[l]), f(conv_b[l]), f(a_log[l]), f(d_skip[l]))
        x_tok = run_C(x_tok, o_sbT, o_fox, y_ssm, ob[4096:], mod[l], f(g_norm_ffn[l]), f(g_ssm_norm[l]), f(w_branch_sb[l]), f(w_branch_fox[l]),
                      f(w_branch_ssm[l]), f(w_out[l]), f(w_router), f(b_router), f(w_e_gate[l]), f(w_e_up[l]), f(w_e_down[l]))
    return x_tok[None].astype(np.float32)
```

```python
from contextlib import ExitStack
import numpy as np
import concourse.bass as bass
import concourse.mybir as mybir
from concourse.bass_utils import run_bass_kernel_spmd

F32 = mybir.dt.float32
BF16 = mybir.dt.bfloat16
AF = mybir.ActivationFunctionType
ALU = mybir.AluOpType
AX = mybir.AxisListType

NCORES = 8
D = 2048
S = 8192
TPC = S // NCORES
EPS = 1e-6
NEXP = 16
FF = 1024


class Dep:
    __slots__ = ("lw", "rd", "dsem", "dcount", "name")

    def __init__(self, name=""):
        self.lw = None
        self.rd = []
        self.dsem = None
        self.dcount = 0
        self.name = name


class Tile:
    def __init__(self, t, name):
        self.t = t
        self.d = Dep(name)

    def __getitem__(self, k):
        return self.t[k]


class EngState:
    def __init__(self, e, sem, name):
        self.e = e
        self.sem = sem
        self.count = 0
        self.name = name
        self.seen = {}


class Ctx:
    def __init__(self, nc, es):
        self.nc = nc
        self.es = es
        self.engs = {}
        for nm, e in (("pe", nc.tensor), ("act", nc.scalar), ("dve", nc.vector), ("pool", nc.gpsimd), ("sp", nc.sync)):
            self.engs[nm] = EngState(e, es.enter_context(nc.semaphore("s_" + nm)), nm)
        self.outdeps = []
        self.nsem = 5

    def sb(self, name, shape, dtype):
        return Tile(self.es.enter_context(self.nc.sbuf_tensor("t_" + name, list(shape), dtype)), name)

    def ps(self, name, shape=(128, 512), dtype=F32):
        return Tile(self.es.enter_context(self.nc.psum_tensor("p_" + name, list(shape), dtype)), name)

    def _collect(self, E, reads, writes):
        need = []
        for d in reads:
            if d.lw is not None:
                need.append(d.lw)
        for d in writes:
            if d.lw is not None:
                need.append(d.lw)
            need.extend(d.rd)
        for src in need:
            if src[0] == "eng":
                _, en, ticket = src
                if en == E.name and en in ("pe", "sp"):
                    continue
                F = self.engs[en]
                assert ticket <= F.count, f"wait on pending ticket {en} {ticket} > {F.count}"
                key = "e_" + en
                if E.seen.get(key, 0) >= ticket:
                    continue
                E.e.wait_ge(F.sem, ticket)
                E.seen[key] = ticket
            else:
                _, dd, cnt = src
                key = id(dd)
                if E.seen.get(key, 0) >= cnt:
                    continue
                E.e.wait_ge(dd.dsem, 16 * cnt)
                E.seen[key] = cnt

    def op(self, eng, fn, reads=(), writes=(), inc=True):
        E = self.engs[eng]
        reads = [r.d if isinstance(r, Tile) else r for r in reads]
        writes = [w.d if isinstance(w, Tile) else w for w in writes]
        self._collect(E, reads, writes)
        inst = fn(E.e)
        if inc:
            inst.then_inc(E.sem, 1)
            E.count += 1
            ticket = E.count
        else:
            ticket = E.count + 1
        rec = ("eng", eng, ticket)
        for d in writes:
            d.lw = rec
            d.rd = []
        for d in reads:
            d.rd.append(rec)
        return inst

    def dma(self, queue, out, in_, dep, load, **kw):
        Q = self.engs[queue]
        dep = dep.d if isinstance(dep, Tile) else dep
        if dep.dsem is None:
            dep.dsem = self.es.enter_context(self.nc.semaphore("d%d" % self.nsem))
            self.nsem += 1
        if load:
            self._collect(Q, [], [dep])
        else:
            self._collect(Q, [dep], [])
        inst = Q.e.dma_start(out=out, in_=in_, **kw)
        inst.then_inc(dep.dsem, 16)
        dep.dcount += 1
        rec = ("dma", dep, dep.dcount)
        if load:
            dep.lw = rec
            dep.rd = []
        else:
            dep.rd.append(rec)
            if dep not in self.outdeps:
                self.outdeps.append(dep)
        return inst

    def finish(self):
        E = self.engs["sp"]
        for dep in self.outdeps:
            E.e.wait_ge(dep.dsem, 16 * dep.dcount)


def act(cx, out, in_, func, reads, writes, eng="act", **kw):
    return cx.op(eng, lambda e: e.activation(out=out, in_=in_, func=func, **kw), reads, writes)


MCOLS = 6 * D // NCORES


def build_M():
    nc = bass.Bass("TRN2", target_bir_lowering=False)
    c_in = nc.dram_tensor("c", [128, 16], F32, kind="ExternalInput").ap()
    w = nc.dram_tensor("w", [2, 16, 128, MCOLS], F32, kind="ExternalInput").ap()
    b = nc.dram_tensor("b", [1, 2 * MCOLS], F32, kind="ExternalInput").ap()
    o = nc.dram_tensor("o", [1, 2 * MCOLS], F32, kind="ExternalOutput").ap()
    with ExitStack() as es:
        cx = Ctx(nc, es)
        ct = cx.sb("ct", [128, 16], F32)
        cond = cx.sb("cond", [128, 16], F32)
        bt = cx.sb("bt", [1, 2 * MCOLS], F32)
        ot = cx.sb("ot", [1, 2 * MCOLS], F32)
        wsl = [cx.sb("w%d" % i, [128, MCOLS], F32) for i in range(4)]
        pss = [cx.ps("ps%d" % i) for i in range(3)]
        cx.dma("sp", ct[:], c_in, ct, True)
        cx.dma("sp", bt[:], b, bt, True)
        act(cx, cond[:], ct[:], AF.Silu, [ct], [cond])
        i = 0
        for l in range(2):
            for kc in range(16):
                ws = wsl[i % 4]
                i += 1
                cx.dma("sp" if i % 2 else "pool", ws[:], w[l, kc], ws, True)
                for n in range(3):
                    cx.op("pe", lambda e, n=n, ws=ws, kc=kc: e.matmul(pss[n][0:1, :], lhsT=cond[:, kc:kc + 1], rhs=ws[:, n * 512:(n + 1) * 512],
                                                                     start=(kc == 0), stop=(kc == 15)),
                          [cond, ws], [pss[n]], inc=True)
            for n in range(3):
                sl = slice(l * MCOLS + n * 512, l * MCOLS + (n + 1) * 512)
                cx.op("dve", lambda e, n=n, sl=sl: e.tensor_tensor(out=ot[0:1, sl], in0=pss[n][0:1, :], in1=bt[0:1, sl], op=ALU.add),
                      [pss[n], bt], [ot])
        cx.dma("sp", o, ot[:], ot, False)
        cx.finish()
    return nc


def run_M(c, w_ada, b_ada):
    nc = build_M()
    c_l = np.ascontiguousarray(c.reshape(16, 128).T)
    in_maps = []
    for k in range(NCORES):
        sl = slice(k * MCOLS, (k + 1) * MCOLS)
        in_maps.append({
            "c": c_l,
            "w": np.ascontiguousarray(w_ada[:, :, sl].reshape(2, 16, 128, MCOLS)),
            "b": np.ascontiguousarray(b_ada[:, sl].reshape(1, 2 * MCOLS)),
        })
    res = run_bass_kernel_spmd(nc, in_maps, core_ids=list(range(NCORES)))
    mod = np.zeros((2, 6 * D), np.float32)
    for k in range(NCORES):
        r = res.results[k]["o"].reshape(2, MCOLS)
        mod[:, k * MCOLS:(k + 1) * MCOLS] = r
    return mod


NFC = 93
A_KIND = (["copy"] * 12 + ["qkn"] * 8 + ["copy"] * 4 + ["silu"] * 8 + ["f32"] * 12 + ["sig"] * 48 + ["small"])
OB_ROWS = (24 + 8 + 48) * 128
OF_ROWS = 12 * 128 + 64


def a_col_index():
    idx = -np.ones(NFC * 128, np.int64)
    idx[0:3072] = np.arange(0, 3072)
    idx[3072:3072 + 1024] = np.arange(3076, 3076 + 1024)
    idx[4096:4096 + 1536] = np.arange(4100, 4100 + 1536)
    idx[5632:5632 + 6144] = np.arange(5652, 5652 + 6144)
    base = 92 * 128
    idx[base:base + 4] = np.arange(3072, 3076)
    idx[base + 32:base + 48] = np.arange(5636, 5652)
    return idx


def build_A():
    nc = bass.Bass("TRN2", target_bir_lowering=False)
    T = TPC
    xT = nc.dram_tensor("xT", [16, 128, T], F32, kind="ExternalInput").ap()
    w = nc.dram_tensor("w", [NFC, 128, 16 * 128], F32, kind="ExternalInput").ap()
    vec = nc.dram_tensor("vec", [128, 64], F32, kind="ExternalInput").ap()
    ones_d = nc.dram_tensor("ones", [128, 128], F32, kind="ExternalInput").ap()
    ob = nc.dram_tensor("ob", [OB_ROWS // 128, 128, T], BF16, kind="ExternalOutput").ap()
    of = nc.dram_tensor("of", [OF_ROWS, T], F32, kind="ExternalOutput").ap()
    with ExitStack() as es:
        cx = Ctx(nc, es)
        xt = cx.sb("xt", [128, 16, T], F32)
        hT = cx.sb("hT", [128, 16, T], BF16)
        vt = cx.sb("vt", [128, 64], F32)
        gs = cx.sb("gs", [128, 16], F32)
        nsb = cx.sb("nsb", [128, 1], F32)
        ones = cx.sb("onesf", [128, 128], F32)
        sq = [cx.sb("sq%d" % i, [128, T], F32) for i in range(2)]
        rstd = cx.sb("rstd", [128, T], F32)
        tmp = [cx.sb("tmp%d" % i, [128, T], F32) for i in range(2)]
        wsl = [cx.sb("w%d" % i, [128, 16 * 128], BF16) for i in range(4)]
        stb = [cx.sb("stb%d" % i, [128, T], BF16) for i in range(3)]
        stf = [cx.sb("stf%d" % i, [128, T], F32) for i in range(2)]
        ps = [cx.ps("ps%d" % i) for i in range(8)]
        xdeps = [Dep("x%d" % k) for k in range(16)]
        hdeps = [Dep("h%d" % k) for k in range(16)]

        cx.dma("sp", vt[:], vec, vt, True)
        cx.dma("sp", ones[:], ones_d, ones, True)
        for kc in range(16):
            cx.dma("sp" if kc % 2 else "act", xt[:, kc, :], xT[kc], xdeps[kc], True)
        def wload(fc):
            cx.dma("pool", wsl[fc % 4][:], w[fc], wsl[fc % 4], True)
        for fc in range(4):
            wload(fc)
        cx.op("dve", lambda e: e.scalar_tensor_tensor(out=gs[:], in0=vt[:, 16:32], scalar=1.0, in1=vt[:, 0:16], op0=ALU.add, op1=ALU.mult), [vt], [gs])
        cx.op("dve", lambda e: e.tensor_scalar(out=nsb[:], in0=vt[:, 50:51], scalar1=-1.0, scalar2=None, op0=ALU.mult), [vt], [nsb])
        for kc in range(16):
            s = sq[kc % 2]
            act(cx, s[:], xt[:, kc, :], AF.Square, [xdeps[kc]], [s])
            for tt in range(2):
                cx.op("pe", lambda e, s=s, tt=tt, kc=kc: e.matmul(ps[tt][:, :], lhsT=ones[:], rhs=s[:, tt * 512:(tt + 1) * 512], start=(kc == 0), stop=(kc == 15)),
                      [ones, s], [ps[tt]])
        for tt in range(2):
            act(cx, rstd[:, tt * 512:(tt + 1) * 512], ps[tt][:, :], AF.Sqrt, [ps[tt]], [rstd], scale=1.0 / D, bias=EPS)
        cx.op("dve", lambda e: e.reciprocal(out=rstd[:], in_=rstd[:]), [rstd], [rstd])
        for kc in range(16):
            tm = tmp[kc % 2]
            cx.op("pool", lambda e, kc=kc, tm=tm: e.tensor_tensor(out=tm[:], in0=xt[:, kc, :], in1=rstd[:], op=ALU.mult), [xdeps[kc], rstd], [tm])
            cx.op("dve", lambda e, kc=kc, tm=tm: e.tensor_scalar(out=hT[:, kc, :], in0=tm[:], scalar1=gs[:, kc:kc + 1], scalar2=vt[:, 32 + kc:33 + kc],
                                                                 op0=ALU.mult, op1=ALU.add), [tm, gs, vt], [hdeps[kc]])
        nb = 0
        nf = 0
        pi = 0
        for fc in range(NFC):
            kind = A_KIND[fc]
            ws = wsl[fc % 4]
            M = 64 if kind == "small" else 128
            pts = []
            for tt in range(2):
                p = ps[pi % 8]
                pi += 1
                pts.append(p)
                for kc in range(16):
                    cx.op("pe", lambda e, p=p, kc=kc, tt=tt, ws=ws, M=M: e.matmul(p[0:M, :], lhsT=ws[:, kc * 128:kc * 128 + M], rhs=hT[:, kc, tt * 512:(tt + 1) * 512],
                                                                                 start=(kc == 0), stop=(kc == 15)),
                          [ws, hdeps[kc]], [p], inc=(kc == 15))
            if fc + 4 < NFC:
                wload(fc + 4)
            if kind in ("copy", "silu", "sig", "qkn"):
                sb_ = stb[nb % 3]
                for tt in range(2):
                    p = pts[tt]
                    sl = slice(tt * 512, (tt + 1) * 512)
                    if kind == "copy":
                        if tt == 0:
                            act(cx, sb_[:, sl], p[:, :], AF.Copy, [p], [sb_])
                        else:
                            cx.op("dve", lambda e, p=p, sl=sl, sb_=sb_: e.tensor_copy(out=sb_[:, sl], in_=p[:, :]), [p], [sb_])
                    elif kind == "silu":
                        act(cx, sb_[:, sl], p[:, :], AF.Silu, [p], [sb_])
                    elif kind == "sig":
                        act(cx, sb_[:, sl], p[:, :], AF.Sigmoid, [p], [sb_])
                    else:
                        s = sq[tt]
                        p2 = ps[pi % 8]
                        pi += 1
                        act(cx, s[:, 0:512], p[:, :], AF.Square, [p], [s])
                        cx.op("pe", lambda e, s=s, p2=p2: e.matmul(p2[:, :], lhsT=ones[:], rhs=s[:, 0:512], start=True, stop=True), [ones, s], [p2])
                        act(cx, s[:, 512:1024], p2[:, :], AF.Sqrt, [p2], [s], scale=1.0 / 128, bias=EPS)
                        cx.op("dve", lambda e, s=s: e.reciprocal(out=s[:, 512:1024], in_=s[:, 512:1024]), [s], [s])
                        gcol = 48 if fc < 16 else 49
                        cx.op("dve", lambda e, s=s, p=p, sl=sl, sb_=sb_, gcol=gcol: e.scalar_tensor_tensor(out=sb_[:, sl], in0=p[:, :], scalar=vt[:, gcol:gcol + 1], in1=s[:, 512:1024],
                                                                                                     op0=ALU.mult, op1=ALU.mult), [p, s, vt], [sb_])
                cx.dma("sp", ob[nb], sb_[:], sb_, False)
                nb += 1
            elif kind == "f32":
                sf = stf[nf % 2]
                for tt in range(2):
                    p = pts[tt]
                    sl = slice(tt * 512, (tt + 1) * 512)
                    if tt == 0:
                        act(cx, sf[:, sl], p[:, :], AF.Copy, [p], [sf])
                    else:
                        cx.op("dve", lambda e, p=p, sl=sl, sf=sf: e.tensor_copy(out=sf[:, sl], in_=p[:, :]), [p], [sf])
                cx.dma("sp", of[nf * 128:(nf + 1) * 128, :], sf[:], sf, False)
                nf += 1
            else:
                sf = stf[nf % 2]
                for tt in range(2):
                    p = pts[tt]
                    sl = slice(tt * 512, (tt + 1) * 512)
                    act(cx, sf[0:32, sl], p[0:32, :], AF.Exp, [p], [sf], scale=-1.0, bias=nsb[0:32, :])
                    act(cx, sf[32:64, sl], p[32:64, :], AF.Exp, [p], [sf], scale=1.0, bias=vt[32:64, 50:51])
                    act(cx, sf[0:64, sl], sf[0:64, sl], AF.Ln, [sf], [sf], scale=1.0, bias=1.0)
                    cx.op("dve", lambda e, sf=sf, sl=sl: e.tensor_scalar(out=sf[0:32, sl], in0=sf[0:32, sl], scalar1=-1.0, scalar2=None, op0=ALU.mult), [sf], [sf])
                cx.dma("sp", of[12 * 128:12 * 128 + 64, :], sf[0:64, :], sf, False)
        cx.finish()
    return nc


_A_CACHE = {}


def run_A(x_tok, w_in_l, mod_l, g_norm_l, b_fgate_l, g_q_l, g_k_l, dt_bias_l):
    if "nc" not in _A_CACHE:
        _A_CACHE["nc"] = build_A()
    nc = _A_CACHE["nc"]
    idx = a_col_index()
    wp = np.zeros((D, NFC * 128), np.float32)
    wp[:, idx >= 0] = w_in_l[:, idx[idx >= 0]]
    wp = np.ascontiguousarray(wp.reshape(16, 128, NFC, 128).transpose(2, 1, 0, 3)).reshape(NFC, 128, 16 * 128)
    vec = np.zeros((128, 64), np.float32)
    vec[:, 0:16] = g_norm_l.reshape(16, 128).T
    vec[:, 16:32] = mod_l[D:2 * D].reshape(16, 128).T
    vec[:, 32:48] = mod_l[0:D].reshape(16, 128).T
    vec[:, 48] = g_q_l
    vec[:, 49] = g_k_l
    vec[0:4, 50] = b_fgate_l
    vec[32:48, 50] = dt_bias_l
    ones = np.ones((128, 128), np.float32)
    in_maps = []
    for k in range(NCORES):
        xs = x_tok[k * TPC:(k + 1) * TPC, :]
        in_maps.append({"xT": np.ascontiguousarray(xs.T).reshape(16, 128, TPC), "w": wp, "vec": vec, "ones": ones})
    res = run_bass_kernel_spmd(nc, in_maps, core_ids=list(range(NCORES)))
    ob = np.concatenate([np.asarray(res.results[k]["ob"]).reshape(OB_ROWS, TPC) for k in range(NCORES)], axis=1)
    of = np.concatenate([np.asarray(res.results[k]["of"]) for k in range(NCORES)], axis=1)
    return ob, of


NQB = 32
NKB = 64
SCALE = 128 ** -0.5
NEG = -30000.0


def _barrier(cx):
    names = ["pe", "act", "dve", "pool", "sp"]
    for a in names:
        E = cx.engs[a]
        for b in names:
            if a == b:
                continue
            F = cx.engs[b]
            if F.count > 0 and E.seen.get("e_" + b, 0) < F.count:
                E.e.wait_ge(F.sem, F.count)
                E.seen["e_" + b] = F.count


B_PHASES = {"ssm": True, "sb": True, "fox": True}


def build_B():
    nc = bass.Bass("TRN2", target_bir_lowering=False)
    dr = lambda n, s, dt, k="ExternalInput": nc.dram_tensor(n, list(s), dt, kind=k).ap()
    sqT_d = dr("sqT", [128, NQB * 128], BF16)
    skT_d = dr("skT", [128, S], BF16)
    sv_d = dr("sv", [128, NKB, 128], BF16)
    fqT_d = dr("fqT", [128, NQB * 128], BF16)
    fkT_d = dr("fkT", [128, S], BF16)
    fv_d = dr("fv", [128, NKB, 130], BF16)
    logf_d = dr("logf", [128, NKB], F32)
    mf_d = dr("mf", [128, 4 * 128], F32)
    cst_d = dr("cst", [128, 5 * 128], F32)
    sv_small_d = dr("small", [128, 32], F32)
    raw_d = dr("raw", [3, 128, S], F32)
    dt_d = dr("dt", [128, NKB * 2], F32)
    zs_d = dr("zs", [128, NKB, 128], BF16)
    osb_d = dr("osb", [128, NQB * 128], BF16, "ExternalOutput")
    ofx_d = dr("ofx", [128, NQB, 128], BF16, "ExternalOutput")
    yss_d = dr("yss", [128, NKB, 128], F32, "ExternalOutput")
    with ExitStack() as es:
        cx = Ctx(nc, es)
        cst = cx.sb("cst", [128, 5 * 128], F32)
        tri_s = cst[:, 0:128]
        tri_i = cst[:, 128:256]
        ones = cst[:, 256:384]
        ident = cst[:, 384:512]
        negm = cst[:, 512:640]
        mf = cx.sb("mf", [128, 4 * 128], F32)
        mb = cx.sb("mb", [128, 4 * 128], BF16)
        identb = cx.sb("identb", [128, 128], BF16)
        sm = cx.sb("sm", [128, 32], F32)
        ps = [cx.ps("ps%d" % i) for i in range(7)]
        cx.dma("sp", cst[:], cst_d, cst, True)
        cx.dma("sp", mf[:], mf_d, mf, True)
        cx.dma("sp", sm[:], sv_small_d, sm, True)
        cx.op("dve", lambda e: e.tensor_copy(out=mb[:], in_=mf[:]), [mf], [mb])
        cx.op("dve", lambda e: e.tensor_copy(out=identb[:], in_=ident), [cst], [identb])

        with ExitStack() as es2:
            outer = cx.es
            cx.es = es2
            raw = cx.sb("raw", [128, S + 3], F32)
            acc = cx.sb("acc", [128, S], F32)
            xs = cx.sb("xs", [128, S], F32)
            BT = cx.sb("BT", [128, S], BF16)
            CT = cx.sb("CT", [128, S], BF16)
            dt = cx.sb("dt", [128, NKB * 2], F32)
            dta = cx.sb("dta", [128, NKB * 2], F32)
            nacum = cx.sb("nacum", [128, NKB * 2], F32)
            zs = cx.sb("zs", [128, NKB, 128], BF16)
            aneg = cx.sb("aneg", [128, 2], F32)
            hT = [cx.sb("hT%d" % i, [128, 64], F32) for i in range(2)]
            hTb = [cx.sb("hTb%d" % i, [128, 64], BF16) for i in range(2)]
            cbs = [cx.sb("cbs%d" % i, [128, 128], F32) for i in range(2)]
            xtok = [cx.sb("xtok%d" % i, [128, 128], F32) for i in range(2)]
            btok = [cx.sb("btok%d" % i, [128, 128], BF16) for i in range(2)]
            dbc = [cx.sb("dbc%d" % i, [128, 128], F32) for i in range(2)]
            tng = [cx.sb("tng%d" % i, [128, 128], F32) for i in range(2)]
            dec = [cx.sb("dec%d" % i, [128, 128], F32) for i in range(2)]
            eac = [cx.sb("eac%d" % i, [128, 128], F32) for i in range(2)]
            MT = [cx.sb("MT%d" % i, [128, 128], BF16) for i in range(2)]
            CsT = [cx.sb("CsT%d" % i, [128, 128], BF16) for i in range(2)]
            xdt = [cx.sb("xdt%d" % i, [128, 64], BF16) for i in range(2)]
            xw = [cx.sb("xw%d" % i, [128, 64], BF16) for i in range(2)]
            yt = [cx.sb("yt%d" % i, [128, 64], F32) for i in range(2)]
            yst = [cx.sb("yst%d" % i, [128, 8, 128], F32) for i in range(2)]
            psb = cx.ps("psb", [128, 1024], BF16)
            cx.es = outer
            cx.dma("sp", dt[:], dt_d, dt, True)
            cx.dma("sp", zs[:], zs_d, zs, True)
            cx.op("pool", lambda e: e.memset(raw[:, 0:3], 0.0), [], [raw])
            for i in range(2):
                cx.op("pool", lambda e, i=i: e.memset(hT[i][:], 0.0), [], [hT[i]])
            act(cx, aneg[:], sm[:, 1:3], AF.Exp, [sm], [aneg])
            cx.op("dve", lambda e: e.tensor_scalar(out=aneg[:], in0=aneg[:], scalar1=-1.0, scalar2=None, op0=ALU.mult), [aneg], [aneg])
            dt3 = dt[:].rearrange("p (b h) -> p b h", h=2)
            dta3 = dta[:].rearrange("p (b h) -> p b h", h=2)
            for hh in range(2):
                cx.op("dve", lambda e, hh=hh: e.tensor_scalar(out=dta3[:, :, hh], in0=dt3[:, :, hh], scalar1=aneg[:, hh:hh + 1], scalar2=None, op0=ALU.mult), [dt, aneg], [dta])
            cx.op("pe", lambda e: e.matmul(ps[0][:, 0:128], lhsT=tri_i, rhs=dta[:], start=True, stop=True), [cst, dta], [ps[0]])
            cx.op("dve", lambda e: e.tensor_scalar(out=nacum[:], in0=ps[0][:, 0:128], scalar1=-1.0, scalar2=None, op0=ALU.mult), [ps[0]], [nacum])
            for g, dst in enumerate((xs, BT, CT)):
                cx.dma("sp", raw[:, 3:3 + S], raw_d[g], raw, True)
                c0 = 8 + 4 * g
                cx.op("dve", lambda e, c0=c0, g=g: e.tensor_scalar(out=acc[:], in0=raw[:, 3:3 + S], scalar1=sm[:, c0 + 3:c0 + 4], scalar2=sm[:, 20 + g:21 + g], op0=ALU.mult, op1=ALU.add),
                      [raw, sm], [acc])
                for wi in (2, 1, 0):
                    cx.op("dve", lambda e, c0=c0, wi=wi: e.scalar_tensor_tensor(out=acc[:], in0=raw[:, wi:wi + S], scalar=sm[:, c0 + wi:c0 + wi + 1], in1=acc[:], op0=ALU.mult, op1=ALU.add),
                          [raw, sm, acc], [acc])
                act(cx, dst[:], acc[:], AF.Silu, [acc], [dst])
            for ck in (range(NKB) if B_PHASES["ssm"] else []):
                b2 = ck % 2
                blk = slice(ck * 128, (ck + 1) * 128)
                cx.op("pe", lambda e, blk=blk: e.matmul(ps[1][:, 0:128], lhsT=BT[:, blk], rhs=CT[:, blk], start=True, stop=True), [BT, CT], [ps[1]])
                act(cx, cbs[b2][:], ps[1][:, 0:128], AF.Copy, [ps[1]], [cbs[b2]])
                cx.op("pe", lambda e, blk=blk: e.transpose(ps[2][:, 0:128], xs[:, blk], ident), [xs, cst], [ps[2]])
                cx.op("dve", lambda e, b2=b2: e.tensor_copy(out=xtok[b2][:], in_=ps[2][:, 0:128]), [ps[2]], [xtok[b2]])
                cx.op("pe", lambda e, blk=blk: e.transpose(psb[:, 0:128], BT[:, blk], identb[:]), [BT, identb], [psb])
                act(cx, btok[b2][:], psb[:, 0:128], AF.Copy, [psb], [btok[b2]])
                for hh in range(2):
                    col = ck * 2 + hh
                    hs = slice(hh * 64, (hh + 1) * 64)
                    k2 = col % 2
                    cx.op("dve", lambda e, k2=k2, col=col: e.tensor_scalar(out=dbc[k2][:], in0=ones, scalar1=dta[:, col:col + 1], scalar2=None, op0=ALU.mult), [cst, dta], [dbc[k2]])
                    cx.op("pe", lambda e, k2=k2: e.matmul(ps[3 + k2][:, 0:128], lhsT=dbc[k2][:], rhs=tri_i, start=True, stop=True), [dbc[k2], cst], [ps[3 + k2]])
                    pab = ps[3 + k2]
                    cx.op("dve", lambda e, k2=k2, pab=pab: e.tensor_tensor(out=tng[k2][:], in0=pab[:, 0:128], in1=negm, op=ALU.add), [pab, cst], [tng[k2]])
                    act(cx, dec[k2][:], tng[k2][:], AF.Exp, [tng[k2], nacum], [dec[k2]], bias=nacum[:, col:col + 1], scale=1.0)
                    act(cx, eac[k2][:], pab[:, 0:128], AF.Exp, [pab], [eac[k2]])
                    cx.op("dve", lambda e, k2=k2, b2=b2: e.tensor_tensor(out=MT[k2][:], in0=cbs[b2][:], in1=dec[k2][:], op=ALU.mult), [cbs[b2], dec[k2]], [MT[k2]])
                    cx.op("dve", lambda e, k2=k2, b2=b2, hs=hs, col=col: e.tensor_scalar(out=xdt[k2][:], in0=xtok[b2][:, hs], scalar1=dt[:, col:col + 1], scalar2=None, op0=ALU.mult),
                          [xtok[b2], dt], [xdt[k2]])
                    cx.op("pool", lambda e, k2=k2, blk=blk: e.tensor_tensor(out=CsT[k2][:], in0=CT[:, blk], in1=eac[k2][:], op=ALU.mult), [CT, eac[k2]], [CsT[k2]])
                    act(cx, hTb[hh][:], hT[hh][:], AF.Copy, [hT[hh]], [hTb[hh]])
                    py = ps[5]
                    cx.op("pe", lambda e, k2=k2: e.matmul(py[:, hs.start:hs.stop], lhsT=MT[k2][:], rhs=xdt[k2][:], start=True, stop=False) if False else
                          e.matmul(py[:, 0:64], lhsT=MT[k2][:], rhs=xdt[k2][:], start=True, stop=False), [MT[k2], xdt[k2]], [py], inc=False)
                    cx.op("pe", lambda e, k2=k2, hh=hh: e.matmul(py[:, 0:64], lhsT=CsT[k2][:], rhs=hTb[hh][:], start=False, stop=True), [CsT[k2], hTb[hh]], [py])
                    cx.op("dve", lambda e, k2=k2: e.tensor_scalar(out=xw[k2][:], in0=xdt[k2][:], scalar1=dec[k2][:, 127:128], scalar2=None, op0=ALU.mult), [xdt[k2], dec[k2]], [xw[k2]])
                    pss_ = ps[6]
                    cx.op("pe", lambda e, k2=k2, b2=b2: e.matmul(pss_[:, 0:64], lhsT=btok[b2][:], rhs=xw[k2][:], start=True, stop=True), [btok[b2], xw[k2]], [pss_])
                    cx.op("dve", lambda e, k2=k2, hh=hh: e.scalar_tensor_tensor(out=hT[hh][:], in0=hT[hh][:], scalar=eac[k2][:, 127:128], in1=pss_[:, 0:64], op0=ALU.mult, op1=ALU.add),
                          [hT[hh], eac[k2], pss_], [hT[hh]])
                    cx.op("dve", lambda e, k2=k2, b2=b2, hs=hs, hh=hh: e.scalar_tensor_tensor(out=yt[k2][:], in0=xtok[b2][:, hs], scalar=sm[:, 3 + hh:4 + hh], in1=py[:, 0:64], op0=ALU.mult, op1=ALU.add),
                          [xtok[b2], sm, py], [yt[k2]])
                    ys = yst[(ck // 8) % 2]
                    cx.op("pool", lambda e, k2=k2, ys=ys, ck=ck, hs=hs: e.tensor_tensor(out=ys[:, ck % 8, hs], in0=yt[k2][:], in1=zs[:, ck, hs], op=ALU.mult), [yt[k2], zs], [ys])
                if ck % 8 == 7:
                    ys = yst[(ck // 8) % 2]
                    cx.dma("sp", yss_d[:, ck - 7:ck + 1, :], ys[:], ys, False)
            for ys in yst:
                if ys.d.dsem is not None:
                    cx.engs["sp"].e.wait_ge(ys.d.dsem, 16 * ys.d.dcount)
            spE = cx.engs["sp"]
            spE.e.engine_nop().then_inc(spE.sem, 1) if hasattr(spE.e, "engine_nop") else spE.e.nop().then_inc(spE.sem, 1)
            spE.count += 1
            _barrier(cx)

        skT = cx.sb("skT", [128, S], BF16)
        sqT = cx.sb("sqT", [128, NQB * 128], BF16)
        sv = cx.sb("sv", [128, NKB, 128], BF16)
        fkT = cx.sb("fkT", [128, S], BF16)
        fqT = cx.sb("fqT", [128, NQB * 128], BF16)
        fv = cx.sb("fv", [128, NKB, 130], BF16)
        logf = cx.sb("logf", [128, NKB], F32)
        tot = cx.sb("tot", [128, NKB], F32)
        pre = cx.sb("pre", [128, NKB + 2], F32)
        negfc = cx.sb("negfc", [128, NKB], F32)
        fcs = cx.sb("fcs", [128, NQB], F32)
        fdf = cx.sb("fdf", [128, NQB], F32)
        bias_all = cx.sb("bias_all", [128, NQB, NKB], F32)
        R = 3
        Wt = [cx.sb("W%d" % i, [128, 128], BF16) for i in range(R)]
        pfo = cx.ps("pfo")
        osb_st = [cx.sb("osbst%d" % i, [128, 8 * 128], BF16) for i in range(2)]
        ofx_st = [cx.sb("ofxst%d" % i, [128, 8, 128], BF16) for i in range(2)]
        rec = [cx.sb("rec%d" % i, [128, 1], F32) for i in range(2)]
        for t_, d_, q in ((skT, skT_d, "sp"), (sqT, sqT_d, "act"), (sv, sv_d, "sp"), (fkT, fkT_d, "act"), (fqT, fqT_d, "sp"), (fv, fv_d, "act"), (logf, logf_d, "sp")):
            cx.dma(q, t_[:], d_, t_, True)

        cx.op("pe", lambda e: e.matmul(ps[6][:, 0:NKB], lhsT=tri_i, rhs=logf[:], start=True, stop=True), [cst, logf], [ps[6]])
        cx.op("pe", lambda e: e.matmul(ps[6][:, 128:128 + NKB], lhsT=ones, rhs=logf[:], start=True, stop=True), [cst, logf], [ps[6]])
        cx.op("dve", lambda e: e.tensor_copy(out=tot[:], in_=ps[6][:, 128:128 + NKB]), [ps[6]], [tot])
        cx.op("dve", lambda e: e.memset(pre[:, 0:1], 0.0), [], [pre])
        for b in range(NKB):
            cx.op("dve", lambda e, b=b: e.tensor_tensor(out=pre[:, b + 1:b + 2], in0=pre[:, b:b + 1], in1=tot[:, b:b + 1], op=ALU.add), [pre, tot], [pre])
        cx.op("dve", lambda e: e.scalar_tensor_tensor(out=negfc[:], in0=ps[6][:, 0:NKB], scalar=-1.0, in1=pre[:, 0:NKB], op0=ALU.mult, op1=ALU.subtract), [ps[6], pre], [negfc])
        pre_e = pre[:, 1:1 + 2 * NQB].rearrange("p (i two) -> p i two", two=2)
        cx.op("dve", lambda e: e.tensor_tensor(out=fdf[:], in0=pre_e[:, :, 1], in1=pre_e[:, :, 0], op=ALU.subtract), [pre], [fdf])
        cx.op("dve", lambda e: e.scalar_tensor_tensor(out=fcs[:], in0=fdf[:], scalar=sm[:, 0:1], in1=pre_e[:, :, 0], op0=ALU.mult, op1=ALU.add), [fdf, sm, pre], [fcs])
        for i in range(NQB):
            cx.op("dve", lambda e, i=i: e.tensor_scalar(out=bias_all[:, i, :], in0=negfc[:], scalar1=fcs[:, i:i + 1], scalar2=0.0, op0=ALU.add, op1=ALU.min), [negfc, fcs], [bias_all])

        NSB = NQB // 2
        pairs = [(I, m) for I in range(NSB) for m in range(2 * I + 2)]
        NP = len(pairs)
        Zp = [ps[0], ps[0]]
        Bp = [ps[2], ps[3]]
        Osb = ps[4]
        carry = ps[5]
        tri_b = cx.sb("tri_b", [128, 128], BF16)
        ones_b = cx.sb("ones_b", [128, 128], BF16)
        mk0 = cx.sb("mk0", [128, 512], BF16)
        mk1 = cx.sb("mk1", [128, 512], BF16)
        cx.op("dve", lambda e: e.tensor_copy(out=tri_b[:], in_=tri_s), [cst], [tri_b])
        cx.op("dve", lambda e: e.tensor_copy(out=ones_b[:], in_=ones), [cst], [ones_b])
        cx.op("dve", lambda e: e.memset(mk0[:], 0.0), [], [mk0])
        cx.op("dve", lambda e: e.tensor_copy(out=mk0[:, 128:256], in_=mf[:, 128:256]), [mf], [mk0])
        cx.op("dve", lambda e: e.tensor_copy(out=mk0[:, 384:512], in_=mf[:, 0:128]), [mf], [mk0])
        cx.op("dve", lambda e: e.memset(mk1[:], 1.0), [], [mk1])
        cx.op("dve", lambda e: e.tensor_copy(out=mk1[:, 0:128], in_=mf[:, 128:256]), [mf], [mk1])
        cx.op("dve", lambda e: e.tensor_copy(out=mk1[:, 256:384], in_=mf[:, 0:128]), [mf], [mk1])
        E2 = [cx.sb("E2%d" % i, [128, 512], F32) for i in range(R)]
        L2 = [cx.sb("L2%d" % i, [128, 512], BF16) for i in range(R)]
        t12 = [cx.sb("t12%d" % i, [128, 512], F32) for i in range(R)]
        t22 = [cx.sb("t22%d" % i, [128, 512], F32) for i in range(R)]
        W2 = [cx.sb("W2%d" % i, [128, 512], BF16) for i in range(R)]
        csb = [cx.sb("csb%d" % i, [128, 256], F32) for i in range(3)]

        def kbs(n):
            I, m = pairs[n]
            return I, m, 4 * I + 3 - 2 * m, 4 * I + 2 - 2 * m

        def sb_s0(n):
            I, m, ka, kb_ = kbs(n)
            z = Zp[n % 2]
            qs = slice(2 * I * 128, (2 * I + 2) * 128)
            cx.op("pe", lambda e: e.matmul(z[:, 0:256], lhsT=skT[:, ka * 128:(ka + 1) * 128], rhs=sqT[:, qs], start=True, stop=True), [skT, sqT], [z], inc=False)
            cx.op("pe", lambda e: e.matmul(z[:, 256:512], lhsT=skT[:, kb_ * 128:(kb_ + 1) * 128], rhs=sqT[:, qs], start=True, stop=True), [skT, sqT], [z])

        def sb_s1(n):
            I, m, ka, kb_ = kbs(n)
            z = Zp[n % 2]
            r = n % R
            act(cx, E2[r][:], z[:, :], AF.Exp, [z], [E2[r]], scale=SCALE)
            act(cx, L2[r][:], E2[r][:], AF.Ln, [E2[r]], [L2[r]], bias=1.0, scale=1.0)
            if m < 2:
                mk = mk0 if m == 0 else mk1
                cx.op("pool", lambda e: e.tensor_tensor(out=L2[r][:], in0=L2[r][:], in1=mk[:], op=ALU.mult), [L2[r], mk], [L2[r]])
            cx.op("dve", lambda e: e.scalar_tensor_tensor(out=t12[r][:], in0=z[:, :], scalar=SCALE, in1=L2[r][:], op0=ALU.mult, op1=ALU.subtract), [z, L2[r]], [t12[r]])

        def sb_s2(n):
            I, m, ka, kb_ = kbs(n)
            r = n % R
            bp = Bp[n % 2]
            last = (m == 2 * I + 1)
            cx.op("pe", lambda e: e.matmul(bp[:, :], lhsT=tri_b[:], rhs=L2[r][:], start=True, stop=False), [tri_b, L2[r]], [bp], inc=False)
            cx.op("pe", lambda e: e.matmul(bp[:, 256:512], lhsT=ones_b[:], rhs=L2[r][:, 0:256], start=False, stop=True), [ones_b, L2[r]], [bp])
            if not last:
                cx.op("pe", lambda e: e.matmul(carry[:, 0:256], lhsT=ones_b[:], rhs=L2[r][:, 0:256], start=(m == 0), stop=False), [ones_b, L2[r]], [carry], inc=False)
                cx.op("pe", lambda e: e.matmul(carry[:, 0:256], lhsT=ones_b[:], rhs=L2[r][:, 256:512], start=False, stop=True), [ones_b, L2[r]], [carry])
                c_ = csb[(n + 1) % 3]
                cx.op("dve", lambda e: e.tensor_copy(out=c_[:], in_=carry[:, 0:256]), [carry], [c_])

        def sb_s3(n):
            I, m, ka, kb_ = kbs(n)
            r = n % R
            bp = Bp[n % 2]
            if m > 0:
                c_ = csb[n % 3]
                t3 = t12[r][:].rearrange("p (a b) -> p a b", a=2, b=256)
                cx.op("pool", lambda e: e.tensor_tensor(out=t3, in0=t3, in1=c_[:].unsqueeze(1).to_broadcast([128, 2, 256]), op=ALU.subtract), [t12[r], c_], [t12[r]])
            cx.op("dve", lambda e: e.tensor_tensor(out=t22[r][:], in0=t12[r][:], in1=bp[:, :], op=ALU.subtract), [t12[r], bp], [t22[r]])
            act(cx, W2[r][:], t22[r][:], AF.Exp, [t22[r]], [W2[r]])
            if m < 2:
                mk = mk0 if m == 0 else mk1
                cx.op("pool", lambda e: e.tensor_tensor(out=W2[r][:], in0=W2[r][:], in1=mk[:], op=ALU.mult), [W2[r], mk], [W2[r]])

        def sb_s4(n):
            I, m, ka, kb_ = kbs(n)
            r = n % R
            o = Osb
            last = (m == 2 * I + 1)
            cx.op("pe", lambda e: e.matmul(o[:, 0:256], lhsT=sv[:, ka, :], rhs=W2[r][:, 0:256], start=(m == 0), stop=False), [sv, W2[r]], [o], inc=False)
            cx.op("pe", lambda e: e.matmul(o[:, 0:256], lhsT=sv[:, kb_, :], rhs=W2[r][:, 256:512], start=False, stop=last), [sv, W2[r]], [o])
            if last:
                st = osb_st[(I // 4) % 2]
                act(cx, st[:, (I % 4) * 256:(I % 4 + 1) * 256], o[:, 0:256], AF.Copy, [o], [st])
                if I % 4 == 3:
                    cx.dma("sp", osb_d[:, (2 * I - 6) * 128:(2 * I + 2) * 128], st[:], st, False)

        ftiles = [(i, kb) for i in range(NQB) for kb in range(0, 2 * i + 2)]
        NF = len(ftiles)
        Pt = Wt

        Zf = [ps[1], ps[6]]

        def fx_s0(n):
            i, kb = ftiles[n]
            z = Zf[n % 2]
            cx.op("pe", lambda e: e.matmul(z[:, 0:128], lhsT=fkT[:, kb * 128:(kb + 1) * 128], rhs=fqT[:, i * 128:(i + 1) * 128], start=True, stop=True), [fkT, fqT], [z])

        def fx_s1(n):
            i, kb = ftiles[n]
            z = Zf[n % 2]
            r = n % R
            act(cx, Pt[r][:], z[:, 0:128], AF.Exp, [z, bias_all], [Pt[r]], scale=SCALE, bias=bias_all[:, i, kb:kb + 1])
            if kb >= 2 * i:
                mi = 2 if kb == 2 * i else 3
                cx.op("dve", lambda e: e.tensor_tensor(out=Pt[r][:], in0=Pt[r][:], in1=mb[:, mi * 128:(mi + 1) * 128], op=ALU.mult), [Pt[r], mb], [Pt[r]])

        def fx_s2(n):
            i, kb = ftiles[n]
            r = n % R
            o = pfo
            first = (kb == 0)
            last = (kb == 2 * i + 1)
            cx.op("pe", lambda e: e.matmul(o[:, 0:129], lhsT=Pt[r][:], rhs=fv[:, kb, 0:129], start=first, stop=last), [Pt[r], fv], [o], inc=last)
            if last:
                st = ofx_st[(i // 8) % 2]
                rc = rec[i % 2]
                cx.op("dve", lambda e: e.reciprocal(out=rc[:], in_=o[:, 128:129]), [o], [rc])
                cx.op("dve", lambda e: e.tensor_scalar(out=st[:, i % 8, :], in0=o[:, 0:128], scalar1=rc[:, 0:1], scalar2=None, op0=ALU.mult), [o, rc], [st])
                if i % 8 == 7:
                    cx.dma("sp", ofx_d[:, i - 7:i + 1, :], st[:], st, False)

        fi = 0

        def fox_emit(k):
            if B_PHASES["fox"] and 0 <= k < NF:
                fx_s0(k)
                fx_s1(k)
            if B_PHASES["fox"] and 0 <= k - 2 < NF:
                fx_s2(k - 2)

        nsteps = NP + 3 if B_PHASES["sb"] else 0
        for step in range(nsteps):
            if step < NP:
                sb_s0(step)
                sb_s1(step)
            fox_emit(fi)
            fi += 1
            if 0 <= step - 1 < NP:
                sb_s2(step - 1)
            fox_emit(fi)
            fi += 1
            if 0 <= step - 2 < NP:
                sb_s3(step - 2)
            fox_emit(fi)
            fi += 1
            if 0 <= step - 3 < NP:
                sb_s4(step - 3)
            fox_emit(fi)
            fi += 1
        while fi < NF + 2:
            fox_emit(fi)
            fi += 1
        cx.finish()
    return nc


_B_CACHE = {}


def _b_consts():
    j = np.arange(128)
    tri_s = (j[:, None] > j[None, :]).astype(np.float32)
    tri_i = (j[:, None] <= j[None, :]).astype(np.float32)
    ones = np.ones((128, 128), np.float32)
    ident = np.eye(128, dtype=np.float32)
    negm = np.where(j[None, :] >= j[:, None], 0.0, NEG).astype(np.float32)
    return np.concatenate([tri_s, tri_i, ones, ident, negm], axis=1)


def run_B(ob, of, conv_w_l, conv_b_l, a_log_l, d_skip_l, trace=False):
    if "nc" not in _B_CACHE:
        _B_CACHE["nc"] = build_B()
    nc = _B_CACHE["nc"]
    bf = ob.dtype
    cst = _b_consts()
    kk = np.arange(128)
    d_strict = (kk[:, None] < kk[None, :]).astype(np.float32)
    d_incl = (kk[:, None] <= kk[None, :]).astype(np.float32)
    zeros = np.zeros((128, 128), np.float32)
    onesm = np.ones((128, 128), np.float32)
    in_maps = []
    for c in range(NCORES):
        h, u, g = c % 4, c // 4, c // 4
        blk = lambda a: a.reshape(128, NKB, 128)
        sel = lambda a: np.ascontiguousarray(blk(a)[:, u::2, :]).reshape(128, NQB * 128)
        tokm = lambda a: np.ascontiguousarray(a.T.reshape(NKB, 128, 128).transpose(1, 0, 2))
        fvt = np.zeros((128, NKB, 130), bf)
        fvt[:, :, 0:128] = tokm(ob[2560 + h * 128:2560 + (h + 1) * 128])
        fvt[:, :, 128] = 1.0
        if u == 0:
            mf = np.concatenate([d_strict, zeros, d_incl, zeros], axis=1)
        else:
            mf = np.concatenate([onesm, d_strict, onesm, d_incl], axis=1)
        small = np.zeros((128, 32), np.float32)
        small[:, 0] = u
        for hh in range(2):
            small[:, 1 + hh] = a_log_l[2 * c + hh]
            small[:, 3 + hh] = d_skip_l[2 * c + hh]
        chs = [np.arange(2 * c * 64, 2 * c * 64 + 128), 1024 + g * 128 + np.arange(128), 1280 + g * 128 + np.arange(128)]
        for gi, ch in enumerate(chs):
            small[:, 8 + 4 * gi:12 + 4 * gi] = conv_w_l[:, ch].T
            small[:, 20 + gi] = conv_b_l[ch]
        raw = np.stack([of[ch, :] for ch in chs]).astype(np.float32)
        dtc = np.stack([of[1568 + 2 * c + hh, :].reshape(NKB, 128).T for hh in range(2)], axis=-1)
        in_maps.append({
            "sqT": sel(ob[h * 128:(h + 1) * 128]), "skT": np.ascontiguousarray(ob[512 + h * 128:512 + (h + 1) * 128]),
            "sv": tokm(ob[1024 + h * 128:1024 + (h + 1) * 128]),
            "fqT": sel(ob[1536 + h * 128:1536 + (h + 1) * 128]), "fkT": np.ascontiguousarray(ob[2048 + h * 128:2048 + (h + 1) * 128]),
            "fv": fvt, "logf": np.ascontiguousarray(of[1536 + h].reshape(NKB, 128).T),
            "mf": mf, "cst": cst, "small": small, "raw": np.ascontiguousarray(raw),
            "dt": np.ascontiguousarray(dtc.reshape(128, NKB * 2)),
            "zs": tokm(ob[3072 + 2 * c * 64:3072 + 2 * c * 64 + 128]),
        })
    res = run_bass_kernel_spmd(nc, in_maps, core_ids=list(range(NCORES)), **({"trace": True} if trace else {}))
    _B_CACHE["exec_ns"] = getattr(res, "exec_time_ns", None)
    o_sbT = np.zeros((512, NKB, 128), bf)
    o_fox = np.zeros((NKB, 128, 512), bf)
    y_ssm = np.zeros((NKB, 128, 1024), np.float32)
    for c in range(NCORES):
        h, u = c % 4, c // 4
        r = res.results[c]
        o_sbT[h * 128:(h + 1) * 128, u::2, :] = np.asarray(r["osb"]).reshape(128, NQB, 128)
        o_fox[u::2, :, h * 128:(h + 1) * 128] = np.asarray(r["ofx"]).transpose(1, 0, 2)
        y_ssm[:, :, 2 * c * 64:2 * c * 64 + 128] = np.asarray(r["yss"]).transpose(1, 0, 2)
    return o_sbT.reshape(512, S), o_fox.reshape(S, 512), y_ssm.reshape(S, 1024)


BIG = 1.0e4


def build_C():
    nc = bass.Bass("TRN2", target_bir_lowering=False)
    T = TPC
    dr = lambda n, s, dt, k="ExternalInput": nc.dram_tensor(n, list(s), dt, kind=k).ap()
    xT_d = dr("xT", [16, 128, T], F32)
    yT_d = dr("yT", [8, 128, T], F32)
    osb_d = dr("osbT", [4, 128, T], BF16)
    ofx_d = dr("ofxT", [4, 128, T], BF16)
    gat_d = dr("gates", [48, 128, T], BF16)
    vec_d = dr("vec", [128, 128], F32)
    cst_d = dr("cst", [128, 3 * 128], F32)
    selE_d = dr("selE", [16, 16 * 128], F32)
    wbr_d = dr("wbr", [16, 128, 16 * 128], F32)
    wo_d = dr("wo", [16, 128, 16 * 128], F32)
    wr_d = dr("wr", [128, 16 * 16], F32)
    wg_d = dr("wg", [NEXP, 8, 128, 16 * 128], F32)
    wu_d = dr("wu", [NEXP, 8, 128, 16 * 128], F32)
    wd_d = dr("wd", [NEXP, 2, 4, 128, D], F32)
    xo_d = dr("xo", [16, 128, T], F32, "ExternalOutput")
    with ExitStack() as es:
        cx = Ctx(nc, es)
        xt = cx.sb("xt", [128, 16, T], F32)
        h2 = cx.sb("h2", [128, 16, T], BF16)
        vt = cx.sb("vt", [128, 128], F32)
        cst = cx.sb("cst", [128, 3 * 128], F32)
        ones = cst[:, 0:128]
        ident = cst[:, 128:256]
        brt = cst[:, 256:384]
        selE = cx.sb("selE", [16, 16 * 128], F32)
        gs2 = cx.sb("gs2", [128, 16], F32)
        comb = cx.sb("comb", [128, 128], F32)
        combT = cx.sb("combT", [16, T], F32)
        ps = [cx.ps("ps%d" % i) for i in range(8)]
        xdeps = [Dep("x%d" % k) for k in range(16)]
        hdeps = [Dep("h%d" % k) for k in range(16)]
        cx.dma("sp", vt[:], vec_d, vt, True)
        cx.dma("sp", cst[:], cst_d, cst, True)
        cx.dma("sp", selE[:], selE_d, selE, True)
        for kc in range(16):
            cx.dma("sp" if kc % 2 else "act", xt[:, kc, :], xT_d[kc], xdeps[kc], True)
        cx.op("dve", lambda e: e.scalar_tensor_tensor(out=gs2[:], in0=vt[:, 16:32], scalar=1.0, in1=vt[:, 0:16], op0=ALU.add, op1=ALU.mult), [vt], [gs2])

        with ExitStack() as es2:
            outer = cx.es
            cx.es = es2
            osb = cx.sb("osb", [128, 4, T], BF16)
            ofx = cx.sb("ofx", [128, 4, T], BF16)
            oss = cx.sb("oss", [128, 8, T], BF16)
            mrg = h2
            sq = [cx.sb("sq%d" % i, [128, T], F32) for i in range(2)]
            rstd = cx.sb("rstd", [128, T], F32)
            tmp = [cx.sb("tmp%d" % i, [128, T], F32) for i in range(2)]
            gsl = [cx.sb("g%d" % i, [128, 3, T], BF16) for i in range(2)]
            wsl = [cx.sb("w%d" % i, [128, 16 * 128], BF16) for i in range(3)]
            m1 = [cx.sb("m1%d" % i, [128, 512], F32) for i in range(2)]
            m2 = [cx.sb("m2%d" % i, [128, 512], F32) for i in range(2)]
            wr = cx.sb("wr", [128, 16 * 16], F32)
            lgT = cx.sb("lgT", [16, T], F32)
            aff = cx.sb("aff", [128, 128], F32)
            sel = cx.sb("sel", [128, 128], F32)
            sel2 = cx.sb("sel2", [128, 128], F32)
            eq = cx.sb("eq", [128, 128], F32)
            r32 = [cx.sb("r32%d" % i, [128, 32], F32) for i in range(5)]
            r8 = [cx.sb("r8%d" % i, [128, 8], F32) for i in range(4)]
            cx.es = outer
            mdeps = hdeps
            cx.dma("sp", osb[:], osb_d.rearrange("k p t -> p k t"), osb, True)
            cx.dma("act", ofx[:], ofx_d.rearrange("k p t -> p k t"), ofx, True)
            cx.dma("sp", wr[:], wr_d, wr, True)
            wq = []

            def wload(src, i, n):
                wsx = wsl[n % 3]
                cx.dma("pool", wsx[:], src[i], wsx, True)
                return wsx
            for kc in range(8):
                s = sq[kc % 2]
                yc = tmp[kc % 2]
                cx.dma("sp" if kc % 2 else "act", yc[:], yT_d[kc], yc, True)
                act(cx, s[:], yc[:], AF.Square, [yc], [s])
                for tt in range(2):
                    cx.op("pe", lambda e, s=s, tt=tt, kc=kc: e.matmul(ps[tt][:, :], lhsT=ones, rhs=s[:, tt * 512:(tt + 1) * 512], start=(kc == 0), stop=(kc == 7)), [cst, s], [ps[tt]])
            for tt in range(2):
                act(cx, rstd[:, tt * 512:(tt + 1) * 512], ps[tt][:, :], AF.Sqrt, [ps[tt]], [rstd], scale=1.0 / 1024, bias=EPS)
            cx.op("dve", lambda e: e.reciprocal(out=rstd[:], in_=rstd[:]), [rstd], [rstd])
            for kc in range(8):
                yc = tmp[kc % 2]
                cx.dma("sp" if kc % 2 else "act", yc[:], yT_d[kc], yc, True)
                cx.op("dve", lambda e, kc=kc, yc=yc: e.scalar_tensor_tensor(out=oss[:, kc, :], in0=yc[:], scalar=vt[:, 80 + kc:81 + kc], in1=rstd[:], op0=ALU.mult, op1=ALU.mult),
                      [yc, vt, rstd], [oss])
            nw = 0
            pi = 2
            for dc in range(16):
                wsx = wload(wbr_d, dc, nw)
                nw += 1
                g = gsl[dc % 2]
                for br in range(3):
                    cx.dma("sp" if br % 2 else "act", g[:, br, :], gat_d[br * 16 + dc], g, True)
                for tt in range(2):
                    sl = slice(tt * 512, (tt + 1) * 512)
                    pp = [ps[(pi + k) % 8] for k in range(3)]
                    pi += 3
                    for br, (src, k0, nk) in enumerate(((osb, 0, 4), (ofx, 4, 4), (oss, 8, 8))):
                        for kk in range(nk):
                            cx.op("pe", lambda e, p=pp[br], src=src, kk=kk, k0=k0, nk=nk, wsx=wsx, sl=sl: e.matmul(p[:, :], lhsT=wsx[:, (k0 + kk) * 128:(k0 + kk + 1) * 128], rhs=src[:, kk, sl],
                                                                                                                     start=(kk == 0), stop=(kk == nk - 1)), [wsx, src], [pp[br]], inc=(kk == nk - 1))
                    a, b = m1[tt], m2[tt]
                    cx.op("dve", lambda e, a=a, g=g, sl=sl, p=pp[0]: e.tensor_tensor(out=a[:], in0=p[:, :], in1=g[:, 0, sl], op=ALU.mult), [pp[0], g], [a])
                    cx.op("dve", lambda e, b=b, g=g, sl=sl, p=pp[1]: e.tensor_tensor(out=b[:], in0=p[:, :], in1=g[:, 1, sl], op=ALU.mult), [pp[1], g], [b])
                    cx.op("pool", lambda e, a=a, b=b: e.tensor_tensor(out=a[:], in0=a[:], in1=b[:], op=ALU.add), [a, b], [a])
                    cx.op("dve", lambda e, b=b, g=g, sl=sl, p=pp[2]: e.tensor_tensor(out=b[:], in0=p[:, :], in1=g[:, 2, sl], op=ALU.mult), [pp[2], g], [b])
                    cx.op("pool", lambda e, a=a, b=b, dc=dc, sl=sl: e.tensor_tensor(out=mrg[:, dc, sl], in0=a[:], in1=b[:], op=ALU.add), [a, b], [mdeps[dc]])
            for dc in range(16):
                wsx = wload(wo_d, dc, nw)
                nw += 1
                for tt in range(2):
                    sl = slice(tt * 512, (tt + 1) * 512)
                    p = ps[pi % 8]
                    pi += 1
                    for kc in range(16):
                        cx.op("pe", lambda e, p=p, kc=kc, wsx=wsx, sl=sl: e.matmul(p[:, :], lhsT=wsx[:, kc * 128:(kc + 1) * 128], rhs=mrg[:, kc, sl], start=(kc == 0), stop=(kc == 15)),
                              [wsx, mdeps[kc]], [p], inc=(kc == 15))
                    cx.op("dve", lambda e, p=p, dc=dc, sl=sl: e.scalar_tensor_tensor(out=xt[:, dc, sl], in0=p[:, :], scalar=vt[:, 48 + dc:49 + dc], in1=xt[:, dc, sl], op0=ALU.mult, op1=ALU.add),
                          [p, vt, xdeps[dc]], [xdeps[dc]])
            for kc in range(16):
                s = sq[kc % 2]
                act(cx, s[:], xt[:, kc, :], AF.Square, [xdeps[kc]], [s])
                for tt in range(2):
                    cx.op("pe", lambda e, s=s, tt=tt, kc=kc: e.matmul(ps[tt][:, :], lhsT=ones, rhs=s[:, tt * 512:(tt + 1) * 512], start=(kc == 0), stop=(kc == 15)), [cst, s], [ps[tt]])
            for tt in range(2):
                act(cx, rstd[:, tt * 512:(tt + 1) * 512], ps[tt][:, :], AF.Sqrt, [ps[tt]], [rstd], scale=1.0 / D, bias=EPS)
            cx.op("dve", lambda e: e.reciprocal(out=rstd[:], in_=rstd[:]), [rstd], [rstd])
            for kc in range(16):
                tm = tmp[kc % 2]
                hf = sq[kc % 2]
                cx.op("pool", lambda e, kc=kc, tm=tm: e.tensor_tensor(out=tm[:], in0=xt[:, kc, :], in1=rstd[:], op=ALU.mult), [xdeps[kc], rstd], [tm])
                cx.op("dve", lambda e, kc=kc, tm=tm, hf=hf: e.tensor_scalar(out=hf[:], in0=tm[:], scalar1=gs2[:, kc:kc + 1], scalar2=vt[:, 32 + kc:33 + kc], op0=ALU.mult, op1=ALU.add),
                      [tm, gs2, vt], [hf])
                act(cx, h2[:, kc, :], hf[:], AF.Copy, [hf], [hdeps[kc]])
                for tt in range(2):
                    cx.op("pe", lambda e, hf=hf, tt=tt, kc=kc: e.matmul(ps[2 + tt][0:16, :], lhsT=wr[:, kc * 16:(kc + 1) * 16], rhs=hf[:, tt * 512:(tt + 1) * 512], start=(kc == 0), stop=(kc == 15)),
                          [wr, hf], [ps[2 + tt]])
            for tt in range(2):
                act(cx, lgT[:, tt * 512:(tt + 1) * 512], ps[2 + tt][0:16, :], AF.Copy, [ps[2 + tt]], [lgT])
            for b in range(8):
                cx.op("pe", lambda e, b=b: e.transpose(ps[4][:, b * 16:(b + 1) * 16], lgT[:, b * 128:(b + 1) * 128], ident[0:16, 0:16]), [lgT, cst], [ps[4]])
            act(cx, aff[:], ps[4][:, 0:128], AF.Sigmoid, [ps[4]], [aff])
            dv = lambda fn, r, w: cx.op("dve", fn, r, w)
            v3 = lambda t_, a, b: t_[:].rearrange("p (a b) -> p a b", a=a, b=b)
            bc = lambda t_, a, b: t_[:].unsqueeze(2).to_broadcast([128, a, b])
            dv(lambda e: e.tensor_tensor(out=sel[:], in0=aff[:], in1=brt, op=ALU.add), [aff, cst], [sel])
            dv(lambda e: e.tensor_reduce(out=r32[0][:], in_=v3(sel, 32, 4), axis=AX.X, op=ALU.max), [sel], [r32[0]])
            dv(lambda e: e.tensor_tensor(out=v3(eq, 32, 4), in0=v3(sel, 32, 4), in1=bc(r32[0], 32, 4), op=ALU.is_equal), [sel, r32[0]], [eq])
            dv(lambda e: e.scalar_tensor_tensor(out=sel2[:], in0=eq[:], scalar=-BIG, in1=sel[:], op0=ALU.mult, op1=ALU.add), [eq, sel], [sel2])
            dv(lambda e: e.tensor_reduce(out=r32[1][:], in_=v3(sel2, 32, 4), axis=AX.X, op=ALU.max), [sel2], [r32[1]])
            dv(lambda e: e.tensor_tensor(out=r32[2][:], in0=r32[0][:], in1=r32[1][:], op=ALU.add), [r32[0], r32[1]], [r32[2]])
            dv(lambda e: e.tensor_reduce(out=r8[0][:], in_=v3(r32[2], 8, 4), axis=AX.X, op=ALU.max), [r32[2]], [r8[0]])
            dv(lambda e: e.tensor_tensor(out=v3(r32[3], 8, 4), in0=v3(r32[2], 8, 4), in1=bc(r8[0], 8, 4), op=ALU.is_equal), [r32[2], r8[0]], [r32[3]])
            dv(lambda e: e.tensor_scalar(out=r32[4][:], in0=r32[3][:], scalar1=-1.0, scalar2=BIG, op0=ALU.add, op1=ALU.mult), [r32[3]], [r32[4]])
            dv(lambda e: e.tensor_tensor(out=v3(sel2, 32, 4), in0=v3(sel, 32, 4), in1=bc(r32[4], 32, 4), op=ALU.add), [sel, r32[4]], [sel2])
            dv(lambda e: e.tensor_reduce(out=r8[1][:], in_=v3(sel2, 8, 16), axis=AX.X, op=ALU.max), [sel2], [r8[1]])
            dv(lambda e: e.tensor_tensor(out=v3(eq, 8, 16), in0=v3(sel2, 8, 16), in1=bc(r8[1], 8, 16), op=ALU.is_equal), [sel2, r8[1]], [eq])
            dv(lambda e: e.scalar_tensor_tensor(out=sel[:], in0=eq[:], scalar=-BIG, in1=sel2[:], op0=ALU.mult, op1=ALU.add), [eq, sel2], [sel])
            dv(lambda e: e.tensor_reduce(out=r8[2][:], in_=v3(sel, 8, 16), axis=AX.X, op=ALU.max), [sel], [r8[2]])
            dv(lambda e: e.tensor_tensor(out=v3(sel2, 8, 16), in0=v3(sel, 8, 16), in1=bc(r8[2], 8, 16), op=ALU.is_equal), [sel, r8[2]], [sel2])
            dv(lambda e: e.tensor_tensor(out=eq[:], in0=eq[:], in1=sel2[:], op=ALU.add), [eq, sel2], [eq])
            dv(lambda e: e.tensor_tensor(out=sel[:], in0=eq[:], in1=aff[:], op=ALU.mult), [eq, aff], [sel])
            dv(lambda e: e.tensor_reduce(out=r8[3][:], in_=v3(sel, 8, 16), axis=AX.X, op=ALU.add), [sel], [r8[3]])
            dv(lambda e: e.reciprocal(out=r8[3][:], in_=r8[3][:]), [r8[3]], [r8[3]])
            dv(lambda e: e.tensor_tensor(out=v3(comb, 8, 16), in0=v3(sel, 8, 16), in1=bc(r8[3], 8, 16), op=ALU.mult), [sel, r8[3]], [comb])
            for b in range(8):
                cx.op("pe", lambda e, b=b: e.transpose(ps[5 + b // 4][0:16, (b % 4) * 128:(b % 4 + 1) * 128], comb[:, b * 16:(b + 1) * 16], ident), [comb, cst], [ps[5 + b // 4]])
            for tt in range(2):
                act(cx, combT[:, tt * 512:(tt + 1) * 512], ps[5 + tt][0:16, :], AF.Copy, [ps[5 + tt]], [combT])
            spE = cx.engs["sp"]
            _barrier(cx)

        wg = [cx.sb("wg%d" % i, [128, 16 * 128], BF16) for i in range(3)]
        wu = [cx.sb("wu%d" % i, [128, 16 * 128], BF16) for i in range(3)]
        wd = [cx.sb("wd%d" % i, [128, 4, D], BF16) for i in range(2)]
        actT = [cx.sb("actT%d" % i, [128, 4, T], BF16) for i in range(2)]
        cbc = [cx.sb("cbc%d" % i, [128, T], F32) for i in range(2)]
        sT = [cx.sb("sT%d" % i, [128, 512], F32) for i in range(2)]
        aT = [cx.sb("aT%d" % i, [128, 512], F32) for i in range(2)]
        nq = 0
        pi = 0
        units = [(e, hf) for e in range(NEXP) for hf in range(2)]

        def load_gu(e, fc, n):
            cx.dma("pool", wg[n % 3][:], wg_d[e, fc], wg[n % 3], True)
            cx.dma("pool", wu[n % 3][:], wu_d[e, fc], wu[n % 3], True)

        chunks = [(e, hf, f4) for (e, hf) in units for f4 in range(4)]
        for n in range(2):
            e, hf, f4 = chunks[n]
            load_gu(e, hf * 4 + f4, n)
        cx.dma("pool", wd[0][:], wd_d[0, 0].rearrange("f p d -> p f d"), wd[0], True)
        for ui, (e, hf) in enumerate(units):
            if hf == 0:
                cb = cbc[e % 2]
                for tt in range(2):
                    p = ps[6 + tt]
                    cx.op("pe", lambda e_, p=p, tt=tt, e=e: e_.matmul(p[:, :], lhsT=selE[:, e * 128:(e + 1) * 128], rhs=combT[:, tt * 512:(tt + 1) * 512], start=True, stop=True), [selE, combT], [p])
                    act(cx, cb[:, tt * 512:(tt + 1) * 512], p[:, :], AF.Copy, [p], [cb])
            cb = cbc[e % 2]
            at = actT[ui % 2]
            if ui + 1 < len(units):
                e2, hf2 = units[ui + 1]
                cx.dma("pool", wd[(ui + 1) % 2][:], wd_d[e2, hf2].rearrange("f p d -> p f d"), wd[(ui + 1) % 2], True)
            for f4 in range(4):
                n = ui * 4 + f4
                wgx, wux = wg[n % 3], wu[n % 3]
                for tt in range(2):
                    sl = slice(tt * 512, (tt + 1) * 512)
                    pg, pu = ps[pi % 4], ps[(pi + 1) % 4]
                    pi += 2
                    for kc in range(16):
                        cx.op("pe", lambda e_, pg=pg, kc=kc, wgx=wgx, sl=sl: e_.matmul(pg[:, :], lhsT=wgx[:, kc * 128:(kc + 1) * 128], rhs=h2[:, kc, sl], start=(kc == 0), stop=(kc == 15)),
                              [wgx, hdeps[kc]], [pg], inc=(kc == 15))
                    for kc in range(16):
                        cx.op("pe", lambda e_, pu=pu, kc=kc, wux=wux, sl=sl: e_.matmul(pu[:, :], lhsT=wux[:, kc * 128:(kc + 1) * 128], rhs=h2[:, kc, sl], start=(kc == 0), stop=(kc == 15)),
                              [wux, hdeps[kc]], [pu], inc=(kc == 15))
                    s_, a_ = sT[tt], aT[tt]
                    act(cx, s_[:], pg[:, :], AF.Silu, [pg], [s_])
                    cx.op("dve", lambda e_, a_=a_, s_=s_, pu=pu: e_.tensor_tensor(out=a_[:], in0=pu[:, :], in1=s_[:], op=ALU.mult), [pu, s_], [a_])
                    cx.op("pool", lambda e_, a_=a_, at=at, f4=f4, sl=sl, cb=cb: e_.tensor_tensor(out=at[:, f4, sl], in0=a_[:], in1=cb[:, sl], op=ALU.mult), [a_, cb], [at])
                if n + 2 < len(chunks):
                    e3, hf3, f43 = chunks[n + 2]
                    load_gu(e3, hf3 * 4 + f43, n + 2)
            wdx = wd[ui % 2]
            for dc in range(16):
                for tt in range(2):
                    sl = slice(tt * 512, (tt + 1) * 512)
                    p = ps[4 + (pi % 2)]
                    pi += 1
                    for f4 in range(4):
                        cx.op("pe", lambda e_, p=p, f4=f4, dc=dc, sl=sl, wdx=wdx, at=at: e_.matmul(p[:, :], lhsT=wdx[:, f4, dc * 128:(dc + 1) * 128], rhs=at[:, f4, sl], start=(f4 == 0), stop=(f4 == 3)),
                              [wdx, at], [p], inc=(f4 == 3))
                    cx.op("dve", lambda e_, p=p, dc=dc, sl=sl: e_.scalar_tensor_tensor(out=xt[:, dc, sl], in0=p[:, :], scalar=vt[:, 64 + dc:65 + dc], in1=xt[:, dc, sl], op0=ALU.mult, op1=ALU.add),
                          [p, vt, xdeps[dc]], [xdeps[dc]])
        for dc in range(16):
            cx.dma("sp" if dc % 2 else "act", xo_d[dc], xt[:, dc, :], xdeps[dc], False)
        cx.finish()
    return nc


_C_CACHE = {}


def _wlayout(wmat):
    K_, N_ = wmat.shape
    return np.ascontiguousarray(wmat.reshape(K_ // 128, 128, N_ // 128, 128).transpose(2, 1, 0, 3)).reshape(N_ // 128, 128, (K_ // 128) * 128)


def run_C(x_tok, o_sbT, o_fox, y_ssm, gatesT, mod_l, g_ffn_l, g_ssm_l, wb_sb, wb_fox, wb_ssm, w_out_l, w_router, b_router, wg_l, wu_l, wd_l):
    if "nc" not in _C_CACHE:
        _C_CACHE["nc"] = build_C()
    nc = _C_CACHE["nc"]
    T = TPC
    vec = np.zeros((128, 128), np.float32)
    col = lambda v: v.reshape(-1, 128).T
    vec[:, 0:16] = col(g_ffn_l)
    vec[:, 16:32] = col(mod_l[4 * D:5 * D])
    vec[:, 32:48] = col(mod_l[3 * D:4 * D])
    vec[:, 48:64] = col(mod_l[2 * D:3 * D])
    vec[:, 64:80] = col(mod_l[5 * D:6 * D])
    vec[:, 80:88] = col(g_ssm_l)
    cst = np.zeros((128, 384), np.float32)
    cst[:, 0:128] = 1.0
    cst[:, 128:256] = np.eye(128, dtype=np.float32)
    cst[:, 256:384] = np.tile(b_router[None, :], (128, 8))
    selE = np.zeros((16, 16 * 128), np.float32)
    for e in range(16):
        selE[e, e * 128:(e + 1) * 128] = 1.0
    wbr = _wlayout(np.concatenate([wb_sb, wb_fox, wb_ssm], axis=0))
    wo = _wlayout(w_out_l)
    wr = np.ascontiguousarray(w_router.reshape(16, 128, 16).transpose(1, 0, 2)).reshape(128, 256)
    lay = lambda w: np.ascontiguousarray(w.reshape(NEXP, 16, 128, 8, 128).transpose(0, 3, 2, 1, 4)).reshape(NEXP, 8, 128, 16 * 128)
    wg = lay(wg_l)
    wu = lay(wu_l)
    wd = np.ascontiguousarray(wd_l).reshape(NEXP, 2, 4, 128, D)
    in_maps = []
    for c in range(NCORES):
        ts = slice(c * T, (c + 1) * T)
        in_maps.append({
            "xT": np.ascontiguousarray(x_tok[ts].T).reshape(16, 128, T),
            "yT": np.ascontiguousarray(y_ssm[ts].T).reshape(8, 128, T),
            "osbT": np.ascontiguousarray(o_sbT[:, ts]).reshape(4, 128, T),
            "ofxT": np.ascontiguousarray(o_fox[ts].T).reshape(4, 128, T),
            "gates": np.ascontiguousarray(gatesT[:, ts]).reshape(48, 128, T),
            "vec": vec, "cst": cst, "selE": selE, "wbr": wbr, "wo": wo, "wr": wr, "wg": wg, "wu": wu, "wd": wd,
        })
    res = run_bass_kernel_spmd(nc, in_maps, core_ids=list(range(NCORES)))
    xo = np.zeros((S, D), np.float32)
    for c in range(NCORES):
        xo[c * T:(c + 1) * T] = np.asarray(res.results[c]["xo"]).reshape(D, T).T
    return xo


def kernel(x, c, w_ada, b_ada, g_norm_mix, w_in, b_fgate, g_q_fox, g_k_fox, conv_w, conv_b, dt_bias, a_log, d_skip,
           g_ssm_norm, w_branch_sb, w_branch_fox, w_branch_ssm, w_out, g_norm_ffn, w_router, b_router, w_e_gate, w_e_up, w_e_down):
    f = lambda a: np.asarray(a, dtype=np.float32)
    x_tok = f(x)[0]
    mod = run_M(f(c), f(w_ada), f(b_ada))
    for l in range(2):
        ob, of = run_A(x_tok, f(w_in[l]), mod[l], f(g_norm_mix[l]), f(b_fgate[l]), f(g_q_fox[l]), f(g_k_fox[l]), f(dt_bias[l]))
        o_sbT, o_fox, y_ssm = run_B(ob, of, f(conv_w[l]), f(conv_b[l]), f(a_log[l]), f(d_skip[l]))
        x_tok = run_C(x_tok, o_sbT, o_fox, y_ssm, ob[4096:], mod[l], f(g_norm_ffn[l]), f(g_ssm_norm[l]), f(w_branch_sb[l]), f(w_branch_fox[l]),
                      f(w_branch_ssm[l]), f(w_out[l]), f(w_router), f(b_router), f(w_e_gate[l]), f(w_e_up[l]), f(w_e_down[l]))
    return x_tok[None].astype(np.float32)
```

```python
from contextlib import ExitStack
import numpy as np
import concourse.bass as bass
import concourse.mybir as mybir
from concourse.bass_utils import run_bass_kernel_spmd

F32 = mybir.dt.float32
BF16 = mybir.dt.bfloat16
AF = mybir.ActivationFunctionType
ALU = mybir.AluOpType
AX = mybir.AxisListType

NCORES = 8
D = 2048
S = 8192
TPC = S // NCORES
EPS = 1e-6
NEXP = 16
FF = 1024


class Dep:
    __slots__ = ("lw", "rd", "dsem", "dcount", "name")

    def __init__(self, name=""):
        self.lw = None
        self.rd = []
        self.dsem = None
        self.dcount = 0
        self.name = name


class Tile:
    def __init__(self, t, name):
        self.t = t
        self.d = Dep(name)

    def __getitem__(self, k):
        return self.t[k]


class EngState:
    def __init__(self, e, sem, name):
        self.e = e
        self.sem = sem
        self.count = 0
        self.name = name
        self.seen = {}


class Ctx:
    def __init__(self, nc, es):
        self.nc = nc
        self.es = es
        self.engs = {}
        for nm, e in (("pe", nc.tensor), ("act", nc.scalar), ("dve", nc.vector), ("pool", nc.gpsimd), ("sp", nc.sync)):
            self.engs[nm] = EngState(e, es.enter_context(nc.semaphore("s_" + nm)), nm)
        self.outdeps = []
        self.nsem = 5

    def sb(self, name, shape, dtype):
        return Tile(self.es.enter_context(self.nc.sbuf_tensor("t_" + name, list(shape), dtype)), name)

    def ps(self, name, shape=(128, 512), dtype=F32):
        return Tile(self.es.enter_context(self.nc.psum_tensor("p_" + name, list(shape), dtype)), name)

    def _collect(self, E, reads, writes):
        need = []
        for d in reads:
            if d.lw is not None:
                need.append(d.lw)
        for d in writes:
            if d.lw is not None:
                need.append(d.lw)
            need.extend(d.rd)
        for src in need:
            if src[0] == "eng":
                _, en, ticket = src
                if en == E.name and en in ("pe", "sp"):
                    continue
                F = self.engs[en]
                assert ticket <= F.count, f"wait on pending ticket {en} {ticket} > {F.count}"
                key = "e_" + en
                if E.seen.get(key, 0) >= ticket:
                    continue
                E.e.wait_ge(F.sem, ticket)
                E.seen[key] = ticket
            else:
                _, dd, cnt = src
                key = id(dd)
                if E.seen.get(key, 0) >= cnt:
                    continue
                E.e.wait_ge(dd.dsem, 16 * cnt)
                E.seen[key] = cnt

    def op(self, eng, fn, reads=(), writes=(), inc=True):
        E = self.engs[eng]
        reads = [r.d if isinstance(r, Tile) else r for r in reads]
        writes = [w.d if isinstance(w, Tile) else w for w in writes]
        self._collect(E, reads, writes)
        inst = fn(E.e)
        if inc:
            inst.then_inc(E.sem, 1)
            E.count += 1
            ticket = E.count
        else:
            ticket = E.count + 1
        rec = ("eng", eng, ticket)
        for d in writes:
            d.lw = rec
            d.rd = []
        for d in reads:
            d.rd.append(rec)
        return inst

    def dma(self, queue, out, in_, dep, load, **kw):
        Q = self.engs[queue]
        dep = dep.d if isinstance(dep, Tile) else dep
        if dep.dsem is None:
            dep.dsem = self.es.enter_context(self.nc.semaphore("d%d" % self.nsem))
            self.nsem += 1
        if load:
            self._collect(Q, [], [dep])
        else:
            self._collect(Q, [dep], [])
        inst = Q.e.dma_start(out=out, in_=in_, **kw)
        inst.then_inc(dep.dsem, 16)
        dep.dcount += 1
        rec = ("dma", dep, dep.dcount)
        if load:
            dep.lw = rec
            dep.rd = []
        else:
            dep.rd.append(rec)
            if dep not in self.outdeps:
                self.outdeps.append(dep)
        return inst

    def finish(self):
        E = self.engs["sp"]
        for dep in self.outdeps:
            E.e.wait_ge(dep.dsem, 16 * dep.dcount)


def act(cx, out, in_, func, reads, writes, eng="act", **kw):
    return cx.op(eng, lambda e: e.activation(out=out, in_=in_, func=func, **kw), reads, writes)


MCOLS = 6 * D // NCORES


def build_M():
    nc = bass.Bass("TRN2", target_bir_lowering=False)
    c_in = nc.dram_tensor("c", [128, 16], F32, kind="ExternalInput").ap()
    w = nc.dram_tensor("w", [2, 16, 128, MCOLS], F32, kind="ExternalInput").ap()
    b = nc.dram_tensor("b", [1, 2 * MCOLS], F32, kind="ExternalInput").ap()
    o = nc.dram_tensor("o", [1, 2 * MCOLS], F32, kind="ExternalOutput").ap()
    with ExitStack() as es:
        cx = Ctx(nc, es)
        ct = cx.sb("ct", [128, 16], F32)
        cond = cx.sb("cond", [128, 16], F32)
        bt = cx.sb("bt", [1, 2 * MCOLS], F32)
        ot = cx.sb("ot", [1, 2 * MCOLS], F32)
        wsl = [cx.sb("w%d" % i, [128, MCOLS], F32) for i in range(4)]
        pss = [cx.ps("ps%d" % i) for i in range(3)]
        cx.dma("sp", ct[:], c_in, ct, True)
        cx.dma("sp", bt[:], b, bt, True)
        act(cx, cond[:], ct[:], AF.Silu, [ct], [cond])
        i = 0
        for l in range(2):
            for kc in range(16):
                ws = wsl[i % 4]
                i += 1
                cx.dma("sp" if i % 2 else "pool", ws[:], w[l, kc], ws, True)
                for n in range(3):
                    cx.op("pe", lambda e, n=n, ws=ws, kc=kc: e.matmul(pss[n][0:1, :], lhsT=cond[:, kc:kc + 1], rhs=ws[:, n * 512:(n + 1) * 512],
                                                                     start=(kc == 0), stop=(kc == 15)),
                          [cond, ws], [pss[n]], inc=True)
            for n in range(3):
                sl = slice(l * MCOLS + n * 512, l * MCOLS + (n + 1) * 512)
                cx.op("dve", lambda e, n=n, sl=sl: e.tensor_tensor(out=ot[0:1, sl], in0=pss[n][0:1, :], in1=bt[0:1, sl], op=ALU.add),
                      [pss[n], bt], [ot])
        cx.dma("sp", o, ot[:], ot, False)
        cx.finish()
    return nc


def run_M(c, w_ada, b_ada):
    nc = build_M()
    c_l = np.ascontiguousarray(c.reshape(16, 128).T)
    in_maps = []
    for k in range(NCORES):
        sl = slice(k * MCOLS, (k + 1) * MCOLS)
        in_maps.append({
            "c": c_l,
            "w": np.ascontiguousarray(w_ada[:, :, sl].reshape(2, 16, 128, MCOLS)),
            "b": np.ascontiguousarray(b_ada[:, sl].reshape(1, 2 * MCOLS)),
        })
    res = run_bass_kernel_spmd(nc, in_maps, core_ids=list(range(NCORES)))
    mod = np.zeros((2, 6 * D), np.float32)
    for k in range(NCORES):
        r = res.results[k]["o"].reshape(2, MCOLS)
        mod[:, k * MCOLS:(k + 1) * MCOLS] = r
    return mod


NFC = 93
A_KIND = (["copy"] * 12 + ["qkn"] * 8 + ["copy"] * 4 + ["silu"] * 8 + ["f32"] * 12 + ["sig"] * 48 + ["small"])
OB_ROWS = (24 + 8 + 48) * 128
OF_ROWS = 12 * 128 + 64


def a_col_index():
    idx = -np.ones(NFC * 128, np.int64)
    idx[0:3072] = np.arange(0, 3072)
    idx[3072:3072 + 1024] = np.arange(3076, 3076 + 1024)
    idx[4096:4096 + 1536] = np.arange(4100, 4100 + 1536)
    idx[5632:5632 + 6144] = np.arange(5652, 5652 + 6144)
    base = 92 * 128
    idx[base:base + 4] = np.arange(3072, 3076)
    idx[base + 32:base + 48] = np.arange(5636, 5652)
    return idx


def build_A():
    nc = bass.Bass("TRN2", target_bir_lowering=False)
    T = TPC
    xT = nc.dram_tensor("xT", [16, 128, T], F32, kind="ExternalInput").ap()
    w = nc.dram_tensor("w", [NFC, 128, 16 * 128], F32, kind="ExternalInput").ap()
    vec = nc.dram_tensor("vec", [128, 64], F32, kind="ExternalInput").ap()
    ones_d = nc.dram_tensor("ones", [128, 128], F32, kind="ExternalInput").ap()
    ob = nc.dram_tensor("ob", [OB_ROWS // 128, 128, T], BF16, kind="ExternalOutput").ap()
    of = nc.dram_tensor("of", [OF_ROWS, T], F32, kind="ExternalOutput").ap()
    with ExitStack() as es:
        cx = Ctx(nc, es)
        xt = cx.sb("xt", [128, 16, T], F32)
        hT = cx.sb("hT", [128, 16, T], BF16)
        vt = cx.sb("vt", [128, 64], F32)
        gs = cx.sb("gs", [128, 16], F32)
        nsb = cx.sb("nsb", [128, 1], F32)
        ones = cx.sb("onesf", [128, 128], F32)
        sq = [cx.sb("sq%d" % i, [128, T], F32) for i in range(2)]
        rstd = cx.sb("rstd", [128, T], F32)
        tmp = [cx.sb("tmp%d" % i, [128, T], F32) for i in range(2)]
        wsl = [cx.sb("w%d" % i, [128, 16 * 128], BF16) for i in range(4)]
        stb = [cx.sb("stb%d" % i, [128, T], BF16) for i in range(3)]
        stf = [cx.sb("stf%d" % i, [128, T], F32) for i in range(2)]
        ps = [cx.ps("ps%d" % i) for i in range(8)]
        xdeps = [Dep("x%d" % k) for k in range(16)]
        hdeps = [Dep("h%d" % k) for k in range(16)]

        cx.dma("sp", vt[:], vec, vt, True)
        cx.dma("sp", ones[:], ones_d, ones, True)
        for kc in range(16):
            cx.dma("sp" if kc % 2 else "act", xt[:, kc, :], xT[kc], xdeps[kc], True)
        def wload(fc):
            cx.dma("pool", wsl[fc % 4][:], w[fc], wsl[fc % 4], True)
        for fc in range(4):
            wload(fc)
        cx.op("dve", lambda e: e.scalar_tensor_tensor(out=gs[:], in0=vt[:, 16:32], scalar=1.0, in1=vt[:, 0:16], op0=ALU.add, op1=ALU.mult), [vt], [gs])
        cx.op("dve", lambda e: e.tensor_scalar(out=nsb[:], in0=vt[:, 50:51], scalar1=-1.0, scalar2=None, op0=ALU.mult), [vt], [nsb])
        for kc in range(16):
            s = sq[kc % 2]
            act(cx, s[:], xt[:, kc, :], AF.Square, [xdeps[kc]], [s])
            for tt in range(2):
                cx.op("pe", lambda e, s=s, tt=tt, kc=kc: e.matmul(ps[tt][:, :], lhsT=ones[:], rhs=s[:, tt * 512:(tt + 1) * 512], start=(kc == 0), stop=(kc == 15)),
                      [ones, s], [ps[tt]])
        for tt in range(2):
            act(cx, rstd[:, tt * 512:(tt + 1) * 512], ps[tt][:, :], AF.Sqrt, [ps[tt]], [rstd], scale=1.0 / D, bias=EPS)
        cx.op("dve", lambda e: e.reciprocal(out=rstd[:], in_=rstd[:]), [rstd], [rstd])
        for kc in range(16):
            tm = tmp[kc % 2]
            cx.op("pool", lambda e, kc=kc, tm=tm: e.tensor_tensor(out=tm[:], in0=xt[:, kc, :], in1=rstd[:], op=ALU.mult), [xdeps[kc], rstd], [tm])
            cx.op("dve", lambda e, kc=kc, tm=tm: e.tensor_scalar(out=hT[:, kc, :], in0=tm[:], scalar1=gs[:, kc:kc + 1], scalar2=vt[:, 32 + kc:33 + kc],
                                                                 op0=ALU.mult, op1=ALU.add), [tm, gs, vt], [hdeps[kc]])
        nb = 0
        nf = 0
        pi = 0
        for fc in range(NFC):
            kind = A_KIND[fc]
            ws = wsl[fc % 4]
            M = 64 if kind == "small" else 128
            pts = []
            for tt in range(2):
                p = ps[pi % 8]
                pi += 1
                pts.append(p)
                for kc in range(16):
                    cx.op("pe", lambda e, p=p, kc=kc, tt=tt, ws=ws, M=M: e.matmul(p[0:M, :], lhsT=ws[:, kc * 128:kc * 128 + M], rhs=hT[:, kc, tt * 512:(tt + 1) * 512],
                                                                                 start=(kc == 0), stop=(kc == 15)),
                          [ws, hdeps[kc]], [p], inc=(kc == 15))
            if fc + 4 < NFC:
                wload(fc + 4)
            if kind in ("copy", "silu", "sig", "qkn"):
                sb_ = stb[nb % 3]
                for tt in range(2):
                    p = pts[tt]
                    sl = slice(tt * 512, (tt + 1) * 512)
                    if kind == "copy":
                        if tt == 0:
                            act(cx, sb_[:, sl], p[:, :], AF.Copy, [p], [sb_])
                        else:
                            cx.op("dve", lambda e, p=p, sl=sl, sb_=sb_: e.tensor_copy(out=sb_[:, sl], in_=p[:, :]), [p], [sb_])
                    elif kind == "silu":
                        act(cx, sb_[:, sl], p[:, :], AF.Silu, [p], [sb_])
                    elif kind == "sig":
                        act(cx, sb_[:, sl], p[:, :], AF.Sigmoid, [p], [sb_])
                    else:
                        s = sq[tt]
                        p2 = ps[pi % 8]
                        pi += 1
                        act(cx, s[:, 0:512], p[:, :], AF.Square, [p], [s])
                        cx.op("pe", lambda e, s=s, p2=p2: e.matmul(p2[:, :], lhsT=ones[:], rhs=s[:, 0:512], start=True, stop=True), [ones, s], [p2])
                        act(cx, s[:, 512:1024], p2[:, :], AF.Sqrt, [p2], [s], scale=1.0 / 128, bias=EPS)
                        cx.op("dve", lambda e, s=s: e.reciprocal(out=s[:, 512:1024], in_=s[:, 512:1024]), [s], [s])
                        gcol = 48 if fc < 16 else 49
                        cx.op("dve", lambda e, s=s, p=p, sl=sl, sb_=sb_, gcol=gcol: e.scalar_tensor_tensor(out=sb_[:, sl], in0=p[:, :], scalar=vt[:, gcol:gcol + 1], in1=s[:, 512:1024],
                                                                                                     op0=ALU.mult, op1=ALU.mult), [p, s, vt], [sb_])
                cx.dma("sp", ob[nb], sb_[:], sb_, False)
                nb += 1
            elif kind == "f32":
                sf = stf[nf % 2]
                for tt in range(2):
                    p = pts[tt]
                    sl = slice(tt * 512, (tt + 1) * 512)
                    if tt == 0:
                        act(cx, sf[:, sl], p[:, :], AF.Copy, [p], [sf])
                    else:
                        cx.op("dve", lambda e, p=p, sl=sl, sf=sf: e.tensor_copy(out=sf[:, sl], in_=p[:, :]), [p], [sf])
                cx.dma("sp", of[nf * 128:(nf + 1) * 128, :], sf[:], sf, False)
                nf += 1
            else:
                sf = stf[nf % 2]
                for tt in range(2):
                    p = pts[tt]
                    sl = slice(tt * 512, (tt + 1) * 512)
                    act(cx, sf[0:32, sl], p[0:32, :], AF.Exp, [p], [sf], scale=-1.0, bias=nsb[0:32, :])
                    act(cx, sf[32:64, sl], p[32:64, :], AF.Exp, [p], [sf], scale=1.0, bias=vt[32:64, 50:51])
                    act(cx, sf[0:64, sl], sf[0:64, sl], AF.Ln, [sf], [sf], scale=1.0, bias=1.0)
                    cx.op("dve", lambda e, sf=sf, sl=sl: e.tensor_scalar(out=sf[0:32, sl], in0=sf[0:32, sl], scalar1=-1.0, scalar2=None, op0=ALU.mult), [sf], [sf])
                cx.dma("sp", of[12 * 128:12 * 128 + 64, :], sf[0:64, :], sf, False)
        cx.finish()
    return nc


_A_CACHE = {}


def run_A(x_tok, w_in_l, mod_l, g_norm_l, b_fgate_l, g_q_l, g_k_l, dt_bias_l):
    if "nc" not in _A_CACHE:
        _A_CACHE["nc"] = build_A()
    nc = _A_CACHE["nc"]
    idx = a_col_index()
    wp = np.zeros((D, NFC * 128), np.float32)
    wp[:, idx >= 0] = w_in_l[:, idx[idx >= 0]]
    wp = np.ascontiguousarray(wp.reshape(16, 128, NFC, 128).transpose(2, 1, 0, 3)).reshape(NFC, 128, 16 * 128)
    vec = np.zeros((128, 64), np.float32)
    vec[:, 0:16] = g_norm_l.reshape(16, 128).T
    vec[:, 16:32] = mod_l[D:2 * D].reshape(16, 128).T
    vec[:, 32:48] = mod_l[0:D].reshape(16, 128).T
    vec[:, 48] = g_q_l
    vec[:, 49] = g_k_l
    vec[0:4, 50] = b_fgate_l
    vec[32:48, 50] = dt_bias_l
    ones = np.ones((128, 128), np.float32)
    in_maps = []
    for k in range(NCORES):
        xs = x_tok[k * TPC:(k + 1) * TPC, :]
        in_maps.append({"xT": np.ascontiguousarray(xs.T).reshape(16, 128, TPC), "w": wp, "vec": vec, "ones": ones})
    res = run_bass_kernel_spmd(nc, in_maps, core_ids=list(range(NCORES)))
    ob = np.concatenate([np.asarray(res.results[k]["ob"]).reshape(OB_ROWS, TPC) for k in range(NCORES)], axis=1)
    of = np.concatenate([np.asarray(res.results[k]["of"]) for k in range(NCORES)], axis=1)
    return ob, of


NQB = 32
NKB = 64
SCALE = 128 ** -0.5
NEG = -30000.0


def _barrier(cx):
    names = ["pe", "act", "dve", "pool", "sp"]
    for a in names:
        E = cx.engs[a]
        for b in names:
            if a == b:
                continue
            F = cx.engs[b]
            if F.count > 0 and E.seen.get("e_" + b, 0) < F.count:
                E.e.wait_ge(F.sem, F.count)
                E.seen["e_" + b] = F.count


B_PHASES = {"ssm": True, "sb": True, "fox": True}


def build_B():
    nc = bass.Bass("TRN2", target_bir_lowering=False)
    dr = lambda n, s, dt, k="ExternalInput": nc.dram_tensor(n, list(s), dt, kind=k).ap()
    sqT_d = dr("sqT", [128, NQB * 128], BF16)
    skT_d = dr("skT", [128, S], BF16)
    sv_d = dr("sv", [128, NKB, 128], BF16)
    fqT_d = dr("fqT", [128, NQB * 128], BF16)
    fkT_d = dr("fkT", [128, S], BF16)
    fv_d = dr("fv", [128, NKB, 130], BF16)
    logf_d = dr("logf", [128, NKB], F32)
    mf_d = dr("mf", [128, 4 * 128], F32)
    cst_d = dr("cst", [128, 5 * 128], F32)
    sv_small_d = dr("small", [128, 32], F32)
    raw_d = dr("raw", [3, 128, S], F32)
    dt_d = dr("dt", [128, NKB * 2], F32)
    zs_d = dr("zs", [128, NKB, 128], BF16)
    osb_d = dr("osb", [128, NQB * 128], BF16, "ExternalOutput")
    ofx_d = dr("ofx", [128, NQB, 128], BF16, "ExternalOutput")
    yss_d = dr("yss", [128, NKB, 128], F32, "ExternalOutput")
    with ExitStack() as es:
        cx = Ctx(nc, es)
        cst = cx.sb("cst", [128, 5 * 128], F32)
        tri_s = cst[:, 0:128]
        tri_i = cst[:, 128:256]
        ones = cst[:, 256:384]
        ident = cst[:, 384:512]
        negm = cst[:, 512:640]
        mf = cx.sb("mf", [128, 4 * 128], F32)
        mb = cx.sb("mb", [128, 4 * 128], BF16)
        identb = cx.sb("identb", [128, 128], BF16)
        sm = cx.sb("sm", [128, 32], F32)
        ps = [cx.ps("ps%d" % i) for i in range(7)]
        cx.dma("sp", cst[:], cst_d, cst, True)
        cx.dma("sp", mf[:], mf_d, mf, True)
        cx.dma("sp", sm[:], sv_small_d, sm, True)
        cx.op("dve", lambda e: e.tensor_copy(out=mb[:], in_=mf[:]), [mf], [mb])
        cx.op("dve", lambda e: e.tensor_copy(out=identb[:], in_=ident), [cst], [identb])

        with ExitStack() as es2:
            outer = cx.es
            cx.es = es2
            raw = cx.sb("raw", [128, S + 3], F32)
            acc = cx.sb("acc", [128, S], F32)
            xs = cx.sb("xs", [128, S], F32)
            BT = cx.sb("BT", [128, S], BF16)
            CT = cx.sb("CT", [128, S], BF16)
            dt = cx.sb("dt", [128, NKB * 2], F32)
            dta = cx.sb("dta", [128, NKB * 2], F32)
            nacum = cx.sb("nacum", [128, NKB * 2], F32)
            zs = cx.sb("zs", [128, NKB, 128], BF16)
            aneg = cx.sb("aneg", [128, 2], F32)
            hT = [cx.sb("hT%d" % i, [128, 64], F32) for i in range(2)]
            hTb = [cx.sb("hTb%d" % i, [128, 64], BF16) for i in range(2)]
            cbs = [cx.sb("cbs%d" % i, [128, 128], F32) for i in range(2)]
            xtok = [cx.sb("xtok%d" % i, [128, 128], F32) for i in range(2)]
            btok = [cx.sb("btok%d" % i, [128, 128], BF16) for i in range(2)]
            dbc = [cx.sb("dbc%d" % i, [128, 128], F32) for i in range(2)]
            tng = [cx.sb("tng%d" % i, [128, 128], F32) for i in range(2)]
            dec = [cx.sb("dec%d" % i, [128, 128], F32) for i in range(2)]
            eac = [cx.sb("eac%d" % i, [128, 128], F32) for i in range(2)]
            MT = [cx.sb("MT%d" % i, [128, 128], BF16) for i in range(2)]
            CsT = [cx.sb("CsT%d" % i, [128, 128], BF16) for i in range(2)]
            xdt = [cx.sb("xdt%d" % i, [128, 64], BF16) for i in range(2)]
            xw = [cx.sb("xw%d" % i, [128, 64], BF16) for i in range(2)]
            yt = [cx.sb("yt%d" % i, [128, 64], F32) for i in range(2)]
            yst = [cx.sb("yst%d" % i, [128, 8, 128], F32) for i in range(2)]
            psb = cx.ps("psb", [128, 1024], BF16)
            cx.es = outer
            cx.dma("sp", dt[:], dt_d, dt, True)
            cx.dma("sp", zs[:], zs_d, zs, True)
            cx.op("pool", lambda e: e.memset(raw[:, 0:3], 0.0), [], [raw])
            for i in range(2):
                cx.op("pool", lambda e, i=i: e.memset(hT[i][:], 0.0), [], [hT[i]])
            act(cx, aneg[:], sm[:, 1:3], AF.Exp, [sm], [aneg])
            cx.op("dve", lambda e: e.tensor_scalar(out=aneg[:], in0=aneg[:], scalar1=-1.0, scalar2=None, op0=ALU.mult), [aneg], [aneg])
            dt3 = dt[:].rearrange("p (b h) -> p b h", h=2)
            dta3 = dta[:].rearrange("p (b h) -> p b h", h=2)
            for hh in range(2):
                cx.op("dve", lambda e, hh=hh: e.tensor_scalar(out=dta3[:, :, hh], in0=dt3[:, :, hh], scalar1=aneg[:, hh:hh + 1], scalar2=None, op0=ALU.mult), [dt, aneg], [dta])
            cx.op("pe", lambda e: e.matmul(ps[0][:, 0:128], lhsT=tri_i, rhs=dta[:], start=True, stop=True), [cst, dta], [ps[0]])
            cx.op("dve", lambda e: e.tensor_scalar(out=nacum[:], in0=ps[0][:, 0:128], scalar1=-1.0, scalar2=None, op0=ALU.mult), [ps[0]], [nacum])
            for g, dst in enumerate((xs, BT, CT)):
                cx.dma("sp", raw[:, 3:3 + S], raw_d[g], raw, True)
                c0 = 8 + 4 * g
                cx.op("dve", lambda e, c0=c0, g=g: e.tensor_scalar(out=acc[:], in0=raw[:, 3:3 + S], scalar1=sm[:, c0 + 3:c0 + 4], scalar2=sm[:, 20 + g:21 + g], op0=ALU.mult, op1=ALU.add),
                      [raw, sm], [acc])
                for wi in (2, 1, 0):
                    cx.op("dve", lambda e, c0=c0, wi=wi: e.scalar_tensor_tensor(out=acc[:], in0=raw[:, wi:wi + S], scalar=sm[:, c0 + wi:c0 + wi + 1], in1=acc[:], op0=ALU.mult, op1=ALU.add),
                          [raw, sm, acc], [acc])
                act(cx, dst[:], acc[:], AF.Silu, [acc], [dst])
            for ck in (range(NKB) if B_PHASES["ssm"] else []):
                b2 = ck % 2
                blk = slice(ck * 128, (ck + 1) * 128)
                cx.op("pe", lambda e, blk=blk: e.matmul(ps[1][:, 0:128], lhsT=BT[:, blk], rhs=CT[:, blk], start=True, stop=True), [BT, CT], [ps[1]])
                act(cx, cbs[b2][:], ps[1][:, 0:128], AF.Copy, [ps[1]], [cbs[b2]])
                cx.op("pe", lambda e, blk=blk: e.transpose(ps[2][:, 0:128], xs[:, blk], ident), [xs, cst], [ps[2]])
                cx.op("dve", lambda e, b2=b2: e.tensor_copy(out=xtok[b2][:], in_=ps[2][:, 0:128]), [ps[2]], [xtok[b2]])
                cx.op("pe", lambda e, blk=blk: e.transpose(psb[:, 0:128], BT[:, blk], identb[:]), [BT, identb], [psb])
                act(cx, btok[b2][:], psb[:, 0:128], AF.Copy, [psb], [btok[b2]])
                def head_ops(hh):
                    col = ck * 2 + hh
                    hs = slice(hh * 64, (hh + 1) * 64)
                    k2 = hh
                    pab = ps[3 + k2]
                    bank = ps[5 + hh]
                    ys = yst[(ck // 8) % 2]
                    ops = []
                    A = ops.append
                    A(("dve", lambda e: e.tensor_scalar(out=dbc[k2][:], in0=ones, scalar1=dta[:, col:col + 1], scalar2=None, op0=ALU.mult), [cst, dta], [dbc[k2]], True))
                    A(("pe", lambda e: e.matmul(pab[:, 0:128], lhsT=dbc[k2][:], rhs=tri_i, start=True, stop=True), [dbc[k2], cst], [pab], True))
                    A(("dve", lambda e: e.tensor_tensor(out=tng[k2][:], in0=pab[:, 0:128], in1=negm, op=ALU.add), [pab, cst], [tng[k2]], True))
                    A(("act", lambda e: e.activation(out=dec[k2][:], in_=tng[k2][:], func=AF.Exp, bias=nacum[:, col:col + 1], scale=1.0), [tng[k2], nacum], [dec[k2]], True))
                    A(("act", lambda e: e.activation(out=eac[k2][:], in_=pab[:, 0:128], func=AF.Exp), [pab], [eac[k2]], True))
                    A(("dve", lambda e: e.tensor_tensor(out=MT[k2][:], in0=cbs[b2][:], in1=dec[k2][:], op=ALU.mult), [cbs[b2], dec[k2]], [MT[k2]], True))
                    A(("dve", lambda e: e.tensor_scalar(out=xdt[k2][:], in0=xtok[b2][:, hs], scalar1=dt[:, col:col + 1], scalar2=None, op0=ALU.mult), [xtok[b2], dt], [xdt[k2]], True))
                    A(("pool", lambda e: e.tensor_tensor(out=CsT[k2][:], in0=CT[:, blk], in1=eac[k2][:], op=ALU.mult), [CT, eac[k2]], [CsT[k2]], True))
                    A(("act", lambda e: e.activation(out=hTb[hh][:], in_=hT[hh][:], func=AF.Copy), [hT[hh]], [hTb[hh]], True))
                    A(("pe", lambda e: e.matmul(bank[:, 0:64], lhsT=MT[k2][:], rhs=xdt[k2][:], start=True, stop=False), [MT[k2], xdt[k2]], [bank], False))
                    A(("pe", lambda e: e.matmul(bank[:, 0:64], lhsT=CsT[k2][:], rhs=hTb[hh][:], start=False, stop=True), [CsT[k2], hTb[hh]], [bank], True))
                    A(("dve", lambda e: e.tensor_scalar(out=xw[k2][:], in0=xdt[k2][:], scalar1=dec[k2][:, 127:128], scalar2=None, op0=ALU.mult), [xdt[k2], dec[k2]], [xw[k2]], True))
                    A(("pe", lambda e: e.matmul(bank[:, 64:128], lhsT=btok[b2][:], rhs=xw[k2][:], start=True, stop=True), [btok[b2], xw[k2]], [bank], True))
                    A(("dve", lambda e: e.scalar_tensor_tensor(out=hT[hh][:], in0=hT[hh][:], scalar=eac[k2][:, 127:128], in1=bank[:, 64:128], op0=ALU.mult, op1=ALU.add),
                       [hT[hh], eac[k2], bank], [hT[hh]], True))
                    A(("dve", lambda e: e.scalar_tensor_tensor(out=yt[k2][:], in0=xtok[b2][:, hs], scalar=sm[:, 3 + hh:4 + hh], in1=bank[:, 0:64], op0=ALU.mult, op1=ALU.add),
                       [xtok[b2], sm, bank], [yt[k2]], True))
                    A(("pool", lambda e: e.tensor_tensor(out=ys[:, ck % 8, hs], in0=yt[k2][:], in1=zs[:, ck, hs], op=ALU.mult), [yt[k2], zs], [ys], True))
                    return ops
                o0, o1 = head_ops(0), head_ops(1)
                for a_, b_ in zip(o0, o1):
                    for (eng_, fn_, rd_, wr_, inc_) in (a_, b_):
                        cx.op(eng_, fn_, rd_, wr_, inc=inc_)
                if ck % 8 == 7:
                    ys = yst[(ck // 8) % 2]
                    cx.dma("sp", yss_d[:, ck - 7:ck + 1, :], ys[:], ys, False)
            for ys in yst:
                if ys.d.dsem is not None:
                    cx.engs["sp"].e.wait_ge(ys.d.dsem, 16 * ys.d.dcount)
            spE = cx.engs["sp"]
            spE.e.engine_nop().then_inc(spE.sem, 1) if hasattr(spE.e, "engine_nop") else spE.e.nop().then_inc(spE.sem, 1)
            spE.count += 1
            _barrier(cx)

        skT = cx.sb("skT", [128, S], BF16)
        sqT = cx.sb("sqT", [128, NQB * 128], BF16)
        sv = cx.sb("sv", [128, NKB, 128], BF16)
        fkT = cx.sb("fkT", [128, S], BF16)
        fqT = cx.sb("fqT", [128, NQB * 128], BF16)
        fv = cx.sb("fv", [128, NKB, 130], BF16)
        logf = cx.sb("logf", [128, NKB], F32)
        tot = cx.sb("tot", [128, NKB], F32)
        pre = cx.sb("pre", [128, NKB + 2], F32)
        negfc = cx.sb("negfc", [128, NKB], F32)
        fcs = cx.sb("fcs", [128, NQB], F32)
        fdf = cx.sb("fdf", [128, NQB], F32)
        bias_all = cx.sb("bias_all", [128, NQB, NKB], F32)
        R = 3
        Wt = [cx.sb("W%d" % i, [128, 128], BF16) for i in range(R)]
        pfo = cx.ps("pfo")
        osb_st = [cx.sb("osbst%d" % i, [128, 8 * 128], BF16) for i in range(2)]
        ofx_st = [cx.sb("ofxst%d" % i, [128, 8, 128], BF16) for i in range(2)]
        rec = [cx.sb("rec%d" % i, [128, 1], F32) for i in range(2)]
        for t_, d_, q in ((skT, skT_d, "sp"), (sqT, sqT_d, "act"), (sv, sv_d, "sp"), (fkT, fkT_d, "act"), (fqT, fqT_d, "sp"), (fv, fv_d, "act"), (logf, logf_d, "sp")):
            cx.dma(q, t_[:], d_, t_, True)

        cx.op("pe", lambda e: e.matmul(ps[6][:, 0:NKB], lhsT=tri_i, rhs=logf[:], start=True, stop=True), [cst, logf], [ps[6]])
        cx.op("pe", lambda e: e.matmul(ps[6][:, 128:128 + NKB], lhsT=ones, rhs=logf[:], start=True, stop=True), [cst, logf], [ps[6]])
        cx.op("dve", lambda e: e.tensor_copy(out=tot[:], in_=ps[6][:, 128:128 + NKB]), [ps[6]], [tot])
        cx.op("dve", lambda e: e.memset(pre[:, 0:1], 0.0), [], [pre])
        for b in range(NKB):
            cx.op("dve", lambda e, b=b: e.tensor_tensor(out=pre[:, b + 1:b + 2], in0=pre[:, b:b + 1], in1=tot[:, b:b + 1], op=ALU.add), [pre, tot], [pre])
        cx.op("dve", lambda e: e.scalar_tensor_tensor(out=negfc[:], in0=ps[6][:, 0:NKB], scalar=-1.0, in1=pre[:, 0:NKB], op0=ALU.mult, op1=ALU.subtract), [ps[6], pre], [negfc])
        pre_e = pre[:, 1:1 + 2 * NQB].rearrange("p (i two) -> p i two", two=2)
        cx.op("dve", lambda e: e.tensor_tensor(out=fdf[:], in0=pre_e[:, :, 1], in1=pre_e[:, :, 0], op=ALU.subtract), [pre], [fdf])
        cx.op("dve", lambda e: e.scalar_tensor_tensor(out=fcs[:], in0=fdf[:], scalar=sm[:, 0:1], in1=pre_e[:, :, 0], op0=ALU.mult, op1=ALU.add), [fdf, sm, pre], [fcs])
        for i in range(NQB):
            cx.op("dve", lambda e, i=i: e.tensor_scalar(out=bias_all[:, i, :], in0=negfc[:], scalar1=fcs[:, i:i + 1], scalar2=0.0, op0=ALU.add, op1=ALU.min), [negfc, fcs], [bias_all])

        NSB = NQB // 2
        pairs = [(I, m) for I in range(NSB) for m in range(2 * I + 2)]
        NP = len(pairs)
        Zp = [ps[0], ps[0]]
        Bp = [ps[2], ps[3]]
        Osb = ps[4]
        carry = ps[5]
        tri_b = cx.sb("tri_b", [128, 128], BF16)
        ones_b = cx.sb("ones_b", [128, 128], BF16)
        mk0 = cx.sb("mk0", [128, 512], BF16)
        mk1 = cx.sb("mk1", [128, 512], BF16)
        cx.op("dve", lambda e: e.tensor_copy(out=tri_b[:], in_=tri_s), [cst], [tri_b])
        cx.op("dve", lambda e: e.tensor_copy(out=ones_b[:], in_=ones), [cst], [ones_b])
        cx.op("dve", lambda e: e.memset(mk0[:], 0.0), [], [mk0])
        cx.op("dve", lambda e: e.tensor_copy(out=mk0[:, 128:256], in_=mf[:, 128:256]), [mf], [mk0])
        cx.op("dve", lambda e: e.tensor_copy(out=mk0[:, 384:512], in_=mf[:, 0:128]), [mf], [mk0])
        cx.op("dve", lambda e: e.memset(mk1[:], 1.0), [], [mk1])
        cx.op("dve", lambda e: e.tensor_copy(out=mk1[:, 0:128], in_=mf[:, 128:256]), [mf], [mk1])
        cx.op("dve", lambda e: e.tensor_copy(out=mk1[:, 256:384], in_=mf[:, 0:128]), [mf], [mk1])
        E2 = [cx.sb("E2%d" % i, [128, 512], F32) for i in range(R)]
        L2 = [cx.sb("L2%d" % i, [128, 512], BF16) for i in range(R)]
        t12 = [cx.sb("t12%d" % i, [128, 512], F32) for i in range(R)]
        t22 = [cx.sb("t22%d" % i, [128, 512], F32) for i in range(R)]
        W2 = [cx.sb("W2%d" % i, [128, 512], BF16) for i in range(R)]
        csb = [cx.sb("csb%d" % i, [128, 256], F32) for i in range(3)]

        def kbs(n):
            I, m = pairs[n]
            return I, m, 4 * I + 3 - 2 * m, 4 * I + 2 - 2 * m

        def sb_s0(n):
            I, m, ka, kb_ = kbs(n)
            z = Zp[n % 2]
            qs = slice(2 * I * 128, (2 * I + 2) * 128)
            cx.op("pe", lambda e: e.matmul(z[:, 0:256], lhsT=skT[:, ka * 128:(ka + 1) * 128], rhs=sqT[:, qs], start=True, stop=True), [skT, sqT], [z], inc=False)
            cx.op("pe", lambda e: e.matmul(z[:, 256:512], lhsT=skT[:, kb_ * 128:(kb_ + 1) * 128], rhs=sqT[:, qs], start=True, stop=True), [skT, sqT], [z])

        def sb_s1(n):
            I, m, ka, kb_ = kbs(n)
            z = Zp[n % 2]
            r = n % R
            act(cx, E2[r][:], z[:, :], AF.Exp, [z], [E2[r]], scale=SCALE)
            act(cx, L2[r][:], E2[r][:], AF.Ln, [E2[r]], [L2[r]], bias=1.0, scale=1.0)
            if m < 2:
                mk = mk0 if m == 0 else mk1
                cx.op("pool", lambda e: e.tensor_tensor(out=L2[r][:], in0=L2[r][:], in1=mk[:], op=ALU.mult), [L2[r], mk], [L2[r]])
            cx.op("dve", lambda e: e.scalar_tensor_tensor(out=t12[r][:], in0=z[:, :], scalar=SCALE, in1=L2[r][:], op0=ALU.mult, op1=ALU.subtract), [z, L2[r]], [t12[r]])

        def sb_s2(n):
            I, m, ka, kb_ = kbs(n)
            r = n % R
            bp = Bp[n % 2]
            last = (m == 2 * I + 1)
            cx.op("pe", lambda e: e.matmul(bp[:, :], lhsT=tri_b[:], rhs=L2[r][:], start=True, stop=False), [tri_b, L2[r]], [bp], inc=False)
            cx.op("pe", lambda e: e.matmul(bp[:, 256:512], lhsT=ones_b[:], rhs=L2[r][:, 0:256], start=False, stop=True), [ones_b, L2[r]], [bp])
            if not last:
                cx.op("pe", lambda e: e.matmul(carry[:, 0:256], lhsT=ones_b[:], rhs=L2[r][:, 0:256], start=(m == 0), stop=False), [ones_b, L2[r]], [carry], inc=False)
                cx.op("pe", lambda e: e.matmul(carry[:, 0:256], lhsT=ones_b[:], rhs=L2[r][:, 256:512], start=False, stop=True), [ones_b, L2[r]], [carry])
                c_ = csb[(n + 1) % 3]
                cx.op("dve", lambda e: e.tensor_copy(out=c_[:], in_=carry[:, 0:256]), [carry], [c_])

        def sb_s3(n):
            I, m, ka, kb_ = kbs(n)
            r = n % R
            bp = Bp[n % 2]
            if m > 0:
                c_ = csb[n % 3]
                t3 = t12[r][:].rearrange("p (a b) -> p a b", a=2, b=256)
                cx.op("pool", lambda e: e.tensor_tensor(out=t3, in0=t3, in1=c_[:].unsqueeze(1).to_broadcast([128, 2, 256]), op=ALU.subtract), [t12[r], c_], [t12[r]])
            cx.op("dve", lambda e: e.tensor_tensor(out=t22[r][:], in0=t12[r][:], in1=bp[:, :], op=ALU.subtract), [t12[r], bp], [t22[r]])
            act(cx, W2[r][:], t22[r][:], AF.Exp, [t22[r]], [W2[r]])
            if m < 2:
                mk = mk0 if m == 0 else mk1
                cx.op("pool", lambda e: e.tensor_tensor(out=W2[r][:], in0=W2[r][:], in1=mk[:], op=ALU.mult), [W2[r], mk], [W2[r]])

        def sb_s4(n):
            I, m, ka, kb_ = kbs(n)
            r = n % R
            o = Osb
            last = (m == 2 * I + 1)
            cx.op("pe", lambda e: e.matmul(o[:, 0:256], lhsT=sv[:, ka, :], rhs=W2[r][:, 0:256], start=(m == 0), stop=False), [sv, W2[r]], [o], inc=False)
            cx.op("pe", lambda e: e.matmul(o[:, 0:256], lhsT=sv[:, kb_, :], rhs=W2[r][:, 256:512], start=False, stop=last), [sv, W2[r]], [o])
            if last:
                st = osb_st[(I // 4) % 2]
                act(cx, st[:, (I % 4) * 256:(I % 4 + 1) * 256], o[:, 0:256], AF.Copy, [o], [st])
                if I % 4 == 3:
                    cx.dma("sp", osb_d[:, (2 * I - 6) * 128:(2 * I + 2) * 128], st[:], st, False)

        ftiles = [(i, kb) for i in range(NQB) for kb in range(0, 2 * i + 2)]
        NF = len(ftiles)
        Pt = Wt

        Zf = [ps[1], ps[6]]

        def fx_s0(n):
            i, kb = ftiles[n]
            z = Zf[n % 2]
            cx.op("pe", lambda e: e.matmul(z[:, 0:128], lhsT=fkT[:, kb * 128:(kb + 1) * 128], rhs=fqT[:, i * 128:(i + 1) * 128], start=True, stop=True), [fkT, fqT], [z])

        def fx_s1(n):
            i, kb = ftiles[n]
            z = Zf[n % 2]
            r = n % R
            act(cx, Pt[r][:], z[:, 0:128], AF.Exp, [z, bias_all], [Pt[r]], scale=SCALE, bias=bias_all[:, i, kb:kb + 1])
            if kb >= 2 * i:
                mi = 2 if kb == 2 * i else 3
                cx.op("dve", lambda e: e.tensor_tensor(out=Pt[r][:], in0=Pt[r][:], in1=mb[:, mi * 128:(mi + 1) * 128], op=ALU.mult), [Pt[r], mb], [Pt[r]])

        def fx_s2(n):
            i, kb = ftiles[n]
            r = n % R
            o = pfo
            first = (kb == 0)
            last = (kb == 2 * i + 1)
            cx.op("pe", lambda e: e.matmul(o[:, 0:129], lhsT=Pt[r][:], rhs=fv[:, kb, 0:129], start=first, stop=last), [Pt[r], fv], [o], inc=last)
            if last:
                st = ofx_st[(i // 8) % 2]
                rc = rec[i % 2]
                cx.op("dve", lambda e: e.reciprocal(out=rc[:], in_=o[:, 128:129]), [o], [rc])
                cx.op("dve", lambda e: e.tensor_scalar(out=st[:, i % 8, :], in0=o[:, 0:128], scalar1=rc[:, 0:1], scalar2=None, op0=ALU.mult), [o, rc], [st])
                if i % 8 == 7:
                    cx.dma("sp", ofx_d[:, i - 7:i + 1, :], st[:], st, False)

        fi = 0

        def fox_emit(k):
            if B_PHASES["fox"] and 0 <= k < NF:
                fx_s0(k)
                fx_s1(k)
            if B_PHASES["fox"] and 0 <= k - 2 < NF:
                fx_s2(k - 2)

        nsteps = NP + 3 if B_PHASES["sb"] else 0
        for step in range(nsteps):
            if step < NP:
                sb_s0(step)
                sb_s1(step)
            fox_emit(fi)
            fi += 1
            if 0 <= step - 1 < NP:
                sb_s2(step - 1)
            fox_emit(fi)
            fi += 1
            if 0 <= step - 2 < NP:
                sb_s3(step - 2)
            fox_emit(fi)
            fi += 1
            if 0 <= step - 3 < NP:
                sb_s4(step - 3)
            fox_emit(fi)
            fi += 1
        while fi < NF + 2:
            fox_emit(fi)
            fi += 1
        cx.finish()
    return nc


_B_CACHE = {}


def _b_consts():
    j = np.arange(128)
    tri_s = (j[:, None] > j[None, :]).astype(np.float32)
    tri_i = (j[:, None] <= j[None, :]).astype(np.float32)
    ones = np.ones((128, 128), np.float32)
    ident = np.eye(128, dtype=np.float32)
    negm = np.where(j[None, :] >= j[:, None], 0.0, NEG).astype(np.float32)
    return np.concatenate([tri_s, tri_i, ones, ident, negm], axis=1)


def run_B(ob, of, conv_w_l, conv_b_l, a_log_l, d_skip_l, trace=False):
    if "nc" not in _B_CACHE:
        _B_CACHE["nc"] = build_B()
    nc = _B_CACHE["nc"]
    bf = ob.dtype
    cst = _b_consts()
    kk = np.arange(128)
    d_strict = (kk[:, None] < kk[None, :]).astype(np.float32)
    d_incl = (kk[:, None] <= kk[None, :]).astype(np.float32)
    zeros = np.zeros((128, 128), np.float32)
    onesm = np.ones((128, 128), np.float32)
    in_maps = []
    for c in range(NCORES):
        h, u, g = c % 4, c // 4, c // 4
        blk = lambda a: a.reshape(128, NKB, 128)
        sel = lambda a: np.ascontiguousarray(blk(a)[:, u::2, :]).reshape(128, NQB * 128)
        tokm = lambda a: np.ascontiguousarray(a.T.reshape(NKB, 128, 128).transpose(1, 0, 2))
        fvt = np.zeros((128, NKB, 130), bf)
        fvt[:, :, 0:128] = tokm(ob[2560 + h * 128:2560 + (h + 1) * 128])
        fvt[:, :, 128] = 1.0
        if u == 0:
            mf = np.concatenate([d_strict, zeros, d_incl, zeros], axis=1)
        else:
            mf = np.concatenate([onesm, d_strict, onesm, d_incl], axis=1)
        small = np.zeros((128, 32), np.float32)
        small[:, 0] = u
        for hh in range(2):
            small[:, 1 + hh] = a_log_l[2 * c + hh]
            small[:, 3 + hh] = d_skip_l[2 * c + hh]
        chs = [np.arange(2 * c * 64, 2 * c * 64 + 128), 1024 + g * 128 + np.arange(128), 1280 + g * 128 + np.arange(128)]
        for gi, ch in enumerate(chs):
            small[:, 8 + 4 * gi:12 + 4 * gi] = conv_w_l[:, ch].T
            small[:, 20 + gi] = conv_b_l[ch]
        raw = np.stack([of[ch, :] for ch in chs]).astype(np.float32)
        dtc = np.stack([of[1568 + 2 * c + hh, :].reshape(NKB, 128).T for hh in range(2)], axis=-1)
        in_maps.append({
            "sqT": sel(ob[h * 128:(h + 1) * 128]), "skT": np.ascontiguousarray(ob[512 + h * 128:512 + (h + 1) * 128]),
            "sv": tokm(ob[1024 + h * 128:1024 + (h + 1) * 128]),
            "fqT": sel(ob[1536 + h * 128:1536 + (h + 1) * 128]), "fkT": np.ascontiguousarray(ob[2048 + h * 128:2048 + (h + 1) * 128]),
            "fv": fvt, "logf": np.ascontiguousarray(of[1536 + h].reshape(NKB, 128).T),
            "mf": mf, "cst": cst, "small": small, "raw": np.ascontiguousarray(raw),
            "dt": np.ascontiguousarray(dtc.reshape(128, NKB * 2)),
            "zs": tokm(ob[3072 + 2 * c * 64:3072 + 2 * c * 64 + 128]),
        })
    res = run_bass_kernel_spmd(nc, in_maps, core_ids=list(range(NCORES)), **({"trace": True} if trace else {}))
    _B_CACHE["exec_ns"] = getattr(res, "exec_time_ns", None)
    o_sbT = np.zeros((512, NKB, 128), bf)
    o_fox = np.zeros((NKB, 128, 512), bf)
    y_ssm = np.zeros((NKB, 128, 1024), np.float32)
    for c in range(NCORES):
        h, u = c % 4, c // 4
        r = res.results[c]
        o_sbT[h * 128:(h + 1) * 128, u::2, :] = np.asarray(r["osb"]).reshape(128, NQB, 128)
        o_fox[u::2, :, h * 128:(h + 1) * 128] = np.asarray(r["ofx"]).transpose(1, 0, 2)
        y_ssm[:, :, 2 * c * 64:2 * c * 64 + 128] = np.asarray(r["yss"]).transpose(1, 0, 2)
    return o_sbT.reshape(512, S), o_fox.reshape(S, 512), y_ssm.reshape(S, 1024)


BIG = 1.0e4


def build_C():
    nc = bass.Bass("TRN2", target_bir_lowering=False)
    T = TPC
    dr = lambda n, s, dt, k="ExternalInput": nc.dram_tensor(n, list(s), dt, kind=k).ap()
    xT_d = dr("xT", [16, 128, T], F32)
    yT_d = dr("yT", [8, 128, T], F32)
    osb_d = dr("osbT", [4, 128, T], BF16)
    ofx_d = dr("ofxT", [4, 128, T], BF16)
    gat_d = dr("gates", [48, 128, T], BF16)
    vec_d = dr("vec", [128, 128], F32)
    cst_d = dr("cst", [128, 3 * 128], F32)
    selE_d = dr("selE", [16, 16 * 128], F32)
    wbr_d = dr("wbr", [16, 128, 16 * 128], F32)
    wo_d = dr("wo", [16, 128, 16 * 128], F32)
    wr_d = dr("wr", [128, 16 * 16], F32)
    wg_d = dr("wg", [NEXP, 8, 128, 16 * 128], F32)
    wu_d = dr("wu", [NEXP, 8, 128, 16 * 128], F32)
    wd_d = dr("wd", [NEXP, 2, 4, 128, D], F32)
    xo_d = dr("xo", [16, 128, T], F32, "ExternalOutput")
    with ExitStack() as es:
        cx = Ctx(nc, es)
        xt = cx.sb("xt", [128, 16, T], F32)
        h2 = cx.sb("h2", [128, 16, T], BF16)
        vt = cx.sb("vt", [128, 128], F32)
        cst = cx.sb("cst", [128, 3 * 128], F32)
        ones = cst[:, 0:128]
        ident = cst[:, 128:256]
        brt = cst[:, 256:384]
        selE = cx.sb("selE", [16, 16 * 128], F32)
        gs2 = cx.sb("gs2", [128, 16], F32)
        comb = cx.sb("comb", [128, 128], F32)
        combT = cx.sb("combT", [16, T], F32)
        ps = [cx.ps("ps%d" % i) for i in range(8)]
        xdeps = [Dep("x%d" % k) for k in range(16)]
        hdeps = [Dep("h%d" % k) for k in range(16)]
        cx.dma("sp", vt[:], vec_d, vt, True)
        cx.dma("sp", cst[:], cst_d, cst, True)
        cx.dma("sp", selE[:], selE_d, selE, True)
        for kc in range(16):
            cx.dma("sp" if kc % 2 else "act", xt[:, kc, :], xT_d[kc], xdeps[kc], True)
        cx.op("dve", lambda e: e.scalar_tensor_tensor(out=gs2[:], in0=vt[:, 16:32], scalar=1.0, in1=vt[:, 0:16], op0=ALU.add, op1=ALU.mult), [vt], [gs2])

        with ExitStack() as es2:
            outer = cx.es
            cx.es = es2
            osb = cx.sb("osb", [128, 4, T], BF16)
            ofx = cx.sb("ofx", [128, 4, T], BF16)
            oss = cx.sb("oss", [128, 8, T], BF16)
            mrg = h2
            sq = [cx.sb("sq%d" % i, [128, T], F32) for i in range(2)]
            rstd = cx.sb("rstd", [128, T], F32)
            tmp = [cx.sb("tmp%d" % i, [128, T], F32) for i in range(2)]
            gsl = [cx.sb("g%d" % i, [128, 3, T], BF16) for i in range(2)]
            wsl = [cx.sb("w%d" % i, [128, 16 * 128], BF16) for i in range(3)]
            m1 = [cx.sb("m1%d" % i, [128, 512], F32) for i in range(2)]
            m2 = [cx.sb("m2%d" % i, [128, 512], F32) for i in range(2)]
            wr = cx.sb("wr", [128, 16 * 16], F32)
            lgT = cx.sb("lgT", [16, T], F32)
            aff = cx.sb("aff", [128, 128], F32)
            sel = cx.sb("sel", [128, 128], F32)
            sel2 = cx.sb("sel2", [128, 128], F32)
            eq = cx.sb("eq", [128, 128], F32)
            r32 = [cx.sb("r32%d" % i, [128, 32], F32) for i in range(5)]
            r8 = [cx.sb("r8%d" % i, [128, 8], F32) for i in range(4)]
            cx.es = outer
            mdeps = hdeps
            cx.dma("sp", osb[:], osb_d.rearrange("k p t -> p k t"), osb, True)
            cx.dma("act", ofx[:], ofx_d.rearrange("k p t -> p k t"), ofx, True)
            cx.dma("sp", wr[:], wr_d, wr, True)
            wq = []

            def wload(src, i, n):
                wsx = wsl[n % 3]
                cx.dma("pool", wsx[:], src[i], wsx, True)
                return wsx
            for kc in range(8):
                s = sq[kc % 2]
                yc = tmp[kc % 2]
                cx.dma("sp" if kc % 2 else "act", yc[:], yT_d[kc], yc, True)
                act(cx, s[:], yc[:], AF.Square, [yc], [s])
                for tt in range(2):
                    cx.op("pe", lambda e, s=s, tt=tt, kc=kc: e.matmul(ps[tt][:, :], lhsT=ones, rhs=s[:, tt * 512:(tt + 1) * 512], start=(kc == 0), stop=(kc == 7)), [cst, s], [ps[tt]])
            for tt in range(2):
                act(cx, rstd[:, tt * 512:(tt + 1) * 512], ps[tt][:, :], AF.Sqrt, [ps[tt]], [rstd], scale=1.0 / 1024, bias=EPS)
            cx.op("dve", lambda e: e.reciprocal(out=rstd[:], in_=rstd[:]), [rstd], [rstd])
            for kc in range(8):
                yc = tmp[kc % 2]
                cx.dma("sp" if kc % 2 else "act", yc[:], yT_d[kc], yc, True)
                cx.op("dve", lambda e, kc=kc, yc=yc: e.scalar_tensor_tensor(out=oss[:, kc, :], in0=yc[:], scalar=vt[:, 80 + kc:81 + kc], in1=rstd[:], op0=ALU.mult, op1=ALU.mult),
                      [yc, vt, rstd], [oss])
            nw = 0
            pi = 2
            for dc in range(16):
                wsx = wload(wbr_d, dc, nw)
                nw += 1
                g = gsl[dc % 2]
                for br in range(3):
                    cx.dma("sp" if br % 2 else "act", g[:, br, :], gat_d[br * 16 + dc], g, True)
                for tt in range(2):
                    sl = slice(tt * 512, (tt + 1) * 512)
                    pp = [ps[(pi + k) % 8] for k in range(3)]
                    pi += 3
                    for br, (src, k0, nk) in enumerate(((osb, 0, 4), (ofx, 4, 4), (oss, 8, 8))):
                        for kk in range(nk):
                            cx.op("pe", lambda e, p=pp[br], src=src, kk=kk, k0=k0, nk=nk, wsx=wsx, sl=sl: e.matmul(p[:, :], lhsT=wsx[:, (k0 + kk) * 128:(k0 + kk + 1) * 128], rhs=src[:, kk, sl],
                                                                                                                     start=(kk == 0), stop=(kk == nk - 1)), [wsx, src], [pp[br]], inc=(kk == nk - 1))
                    a, b = m1[tt], m2[tt]
                    cx.op("dve", lambda e, a=a, g=g, sl=sl, p=pp[0]: e.tensor_tensor(out=a[:], in0=p[:, :], in1=g[:, 0, sl], op=ALU.mult), [pp[0], g], [a])
                    cx.op("dve", lambda e, b=b, g=g, sl=sl, p=pp[1]: e.tensor_tensor(out=b[:], in0=p[:, :], in1=g[:, 1, sl], op=ALU.mult), [pp[1], g], [b])
                    cx.op("pool", lambda e, a=a, b=b: e.tensor_tensor(out=a[:], in0=a[:], in1=b[:], op=ALU.add), [a, b], [a])
                    cx.op("dve", lambda e, b=b, g=g, sl=sl, p=pp[2]: e.tensor_tensor(out=b[:], in0=p[:, :], in1=g[:, 2, sl], op=ALU.mult), [pp[2], g], [b])
                    cx.op("pool", lambda e, a=a, b=b, dc=dc, sl=sl: e.tensor_tensor(out=mrg[:, dc, sl], in0=a[:], in1=b[:], op=ALU.add), [a, b], [mdeps[dc]])
            for dc in range(16):
                wsx = wload(wo_d, dc, nw)
                nw += 1
                for tt in range(2):
                    sl = slice(tt * 512, (tt + 1) * 512)
                    p = ps[pi % 8]
                    pi += 1
                    for kc in range(16):
                        cx.op("pe", lambda e, p=p, kc=kc, wsx=wsx, sl=sl: e.matmul(p[:, :], lhsT=wsx[:, kc * 128:(kc + 1) * 128], rhs=mrg[:, kc, sl], start=(kc == 0), stop=(kc == 15)),
                              [wsx, mdeps[kc]], [p], inc=(kc == 15))
                    cx.op("dve", lambda e, p=p, dc=dc, sl=sl: e.scalar_tensor_tensor(out=xt[:, dc, sl], in0=p[:, :], scalar=vt[:, 48 + dc:49 + dc], in1=xt[:, dc, sl], op0=ALU.mult, op1=ALU.add),
                          [p, vt, xdeps[dc]], [xdeps[dc]])
            for kc in range(16):
                s = sq[kc % 2]
                act(cx, s[:], xt[:, kc, :], AF.Square, [xdeps[kc]], [s])
                for tt in range(2):
                    cx.op("pe", lambda e, s=s, tt=tt, kc=kc: e.matmul(ps[tt][:, :], lhsT=ones, rhs=s[:, tt * 512:(tt + 1) * 512], start=(kc == 0), stop=(kc == 15)), [cst, s], [ps[tt]])
            for tt in range(2):
                act(cx, rstd[:, tt * 512:(tt + 1) * 512], ps[tt][:, :], AF.Sqrt, [ps[tt]], [rstd], scale=1.0 / D, bias=EPS)
            cx.op("dve", lambda e: e.reciprocal(out=rstd[:], in_=rstd[:]), [rstd], [rstd])
            for kc in range(16):
                tm = tmp[kc % 2]
                hf = sq[kc % 2]
                cx.op("pool", lambda e, kc=kc, tm=tm: e.tensor_tensor(out=tm[:], in0=xt[:, kc, :], in1=rstd[:], op=ALU.mult), [xdeps[kc], rstd], [tm])
                cx.op("dve", lambda e, kc=kc, tm=tm, hf=hf: e.tensor_scalar(out=hf[:], in0=tm[:], scalar1=gs2[:, kc:kc + 1], scalar2=vt[:, 32 + kc:33 + kc], op0=ALU.mult, op1=ALU.add),
                      [tm, gs2, vt], [hf])
                act(cx, h2[:, kc, :], hf[:], AF.Copy, [hf], [hdeps[kc]])
                for tt in range(2):
                    cx.op("pe", lambda e, hf=hf, tt=tt, kc=kc: e.matmul(ps[2 + tt][0:16, :], lhsT=wr[:, kc * 16:(kc + 1) * 16], rhs=hf[:, tt * 512:(tt + 1) * 512], start=(kc == 0), stop=(kc == 15)),
                          [wr, hf], [ps[2 + tt]])
            for tt in range(2):
                act(cx, lgT[:, tt * 512:(tt + 1) * 512], ps[2 + tt][0:16, :], AF.Copy, [ps[2 + tt]], [lgT])
            for b in range(8):
                cx.op("pe", lambda e, b=b: e.transpose(ps[4][:, b * 16:(b + 1) * 16], lgT[:, b * 128:(b + 1) * 128], ident[0:16, 0:16]), [lgT, cst], [ps[4]])
            act(cx, aff[:], ps[4][:, 0:128], AF.Sigmoid, [ps[4]], [aff])
            dv = lambda fn, r, w: cx.op("dve", fn, r, w)
            v3 = lambda t_, a, b: t_[:].rearrange("p (a b) -> p a b", a=a, b=b)
            bc = lambda t_, a, b: t_[:].unsqueeze(2).to_broadcast([128, a, b])
            dv(lambda e: e.tensor_tensor(out=sel[:], in0=aff[:], in1=brt, op=ALU.add), [aff, cst], [sel])
            dv(lambda e: e.tensor_reduce(out=r32[0][:], in_=v3(sel, 32, 4), axis=AX.X, op=ALU.max), [sel], [r32[0]])
            dv(lambda e: e.tensor_tensor(out=v3(eq, 32, 4), in0=v3(sel, 32, 4), in1=bc(r32[0], 32, 4), op=ALU.is_equal), [sel, r32[0]], [eq])
            dv(lambda e: e.scalar_tensor_tensor(out=sel2[:], in0=eq[:], scalar=-BIG, in1=sel[:], op0=ALU.mult, op1=ALU.add), [eq, sel], [sel2])
            dv(lambda e: e.tensor_reduce(out=r32[1][:], in_=v3(sel2, 32, 4), axis=AX.X, op=ALU.max), [sel2], [r32[1]])
            dv(lambda e: e.tensor_tensor(out=r32[2][:], in0=r32[0][:], in1=r32[1][:], op=ALU.add), [r32[0], r32[1]], [r32[2]])
            dv(lambda e: e.tensor_reduce(out=r8[0][:], in_=v3(r32[2], 8, 4), axis=AX.X, op=ALU.max), [r32[2]], [r8[0]])
            dv(lambda e: e.tensor_tensor(out=v3(r32[3], 8, 4), in0=v3(r32[2], 8, 4), in1=bc(r8[0], 8, 4), op=ALU.is_equal), [r32[2], r8[0]], [r32[3]])
            dv(lambda e: e.tensor_scalar(out=r32[4][:], in0=r32[3][:], scalar1=-1.0, scalar2=BIG, op0=ALU.add, op1=ALU.mult), [r32[3]], [r32[4]])
            dv(lambda e: e.tensor_tensor(out=v3(sel2, 32, 4), in0=v3(sel, 32, 4), in1=bc(r32[4], 32, 4), op=ALU.add), [sel, r32[4]], [sel2])
            dv(lambda e: e.tensor_reduce(out=r8[1][:], in_=v3(sel2, 8, 16), axis=AX.X, op=ALU.max), [sel2], [r8[1]])
            dv(lambda e: e.tensor_tensor(out=v3(eq, 8, 16), in0=v3(sel2, 8, 16), in1=bc(r8[1], 8, 16), op=ALU.is_equal), [sel2, r8[1]], [eq])
            dv(lambda e: e.scalar_tensor_tensor(out=sel[:], in0=eq[:], scalar=-BIG, in1=sel2[:], op0=ALU.mult, op1=ALU.add), [eq, sel2], [sel])
            dv(lambda e: e.tensor_reduce(out=r8[2][:], in_=v3(sel, 8, 16), axis=AX.X, op=ALU.max), [sel], [r8[2]])
            dv(lambda e: e.tensor_tensor(out=v3(sel2, 8, 16), in0=v3(sel, 8, 16), in1=bc(r8[2], 8, 16), op=ALU.is_equal), [sel, r8[2]], [sel2])
            dv(lambda e: e.tensor_tensor(out=eq[:], in0=eq[:], in1=sel2[:], op=ALU.add), [eq, sel2], [eq])
            dv(lambda e: e.tensor_tensor(out=sel[:], in0=eq[:], in1=aff[:], op=ALU.mult), [eq, aff], [sel])
            dv(lambda e: e.tensor_reduce(out=r8[3][:], in_=v3(sel, 8, 16), axis=AX.X, op=ALU.add), [sel], [r8[3]])
            dv(lambda e: e.reciprocal(out=r8[3][:], in_=r8[3][:]), [r8[3]], [r8[3]])
            dv(lambda e: e.tensor_tensor(out=v3(comb, 8, 16), in0=v3(sel, 8, 16), in1=bc(r8[3], 8, 16), op=ALU.mult), [sel, r8[3]], [comb])
            for b in range(8):
                cx.op("pe", lambda e, b=b: e.transpose(ps[5 + b // 4][0:16, (b % 4) * 128:(b % 4 + 1) * 128], comb[:, b * 16:(b + 1) * 16], ident), [comb, cst], [ps[5 + b // 4]])
            for tt in range(2):
                act(cx, combT[:, tt * 512:(tt + 1) * 512], ps[5 + tt][0:16, :], AF.Copy, [ps[5 + tt]], [combT])
            spE = cx.engs["sp"]
            _barrier(cx)

        wg = [cx.sb("wg%d" % i, [128, 16 * 128], BF16) for i in range(3)]
        wu = [cx.sb("wu%d" % i, [128, 16 * 128], BF16) for i in range(3)]
        wd = [cx.sb("wd%d" % i, [128, 4, D], BF16) for i in range(2)]
        actT = [cx.sb("actT%d" % i, [128, 4, T], BF16) for i in range(2)]
        cbc = [cx.sb("cbc%d" % i, [128, T], F32) for i in range(2)]
        sT = [cx.sb("sT%d" % i, [128, 512], F32) for i in range(2)]
        aT = [cx.sb("aT%d" % i, [128, 512], F32) for i in range(2)]
        nq = 0
        pi = 0
        units = [(e, hf) for e in range(NEXP) for hf in range(2)]

        def load_gu(e, fc, n):
            cx.dma("pool", wg[n % 3][:], wg_d[e, fc], wg[n % 3], True)
            cx.dma("pool", wu[n % 3][:], wu_d[e, fc], wu[n % 3], True)

        chunks = [(e, hf, f4) for (e, hf) in units for f4 in range(4)]
        for n in range(2):
            e, hf, f4 = chunks[n]
            load_gu(e, hf * 4 + f4, n)
        cx.dma("pool", wd[0][:], wd_d[0, 0].rearrange("f p d -> p f d"), wd[0], True)
        for ui, (e, hf) in enumerate(units):
            if hf == 0:
                cb = cbc[e % 2]
                for tt in range(2):
                    p = ps[6 + tt]
                    cx.op("pe", lambda e_, p=p, tt=tt, e=e: e_.matmul(p[:, :], lhsT=selE[:, e * 128:(e + 1) * 128], rhs=combT[:, tt * 512:(tt + 1) * 512], start=True, stop=True), [selE, combT], [p])
                    act(cx, cb[:, tt * 512:(tt + 1) * 512], p[:, :], AF.Copy, [p], [cb])
            cb = cbc[e % 2]
            at = actT[ui % 2]
            if ui + 1 < len(units):
                e2, hf2 = units[ui + 1]
                cx.dma("pool", wd[(ui + 1) % 2][:], wd_d[e2, hf2].rearrange("f p d -> p f d"), wd[(ui + 1) % 2], True)
            for f4 in range(4):
                n = ui * 4 + f4
                wgx, wux = wg[n % 3], wu[n % 3]
                for tt in range(2):
                    sl = slice(tt * 512, (tt + 1) * 512)
                    pg, pu = ps[pi % 4], ps[(pi + 1) % 4]
                    pi += 2
                    for kc in range(16):
                        cx.op("pe", lambda e_, pg=pg, kc=kc, wgx=wgx, sl=sl: e_.matmul(pg[:, :], lhsT=wgx[:, kc * 128:(kc + 1) * 128], rhs=h2[:, kc, sl], start=(kc == 0), stop=(kc == 15)),
                              [wgx, hdeps[kc]], [pg], inc=(kc == 15))
                    for kc in range(16):
                        cx.op("pe", lambda e_, pu=pu, kc=kc, wux=wux, sl=sl: e_.matmul(pu[:, :], lhsT=wux[:, kc * 128:(kc + 1) * 128], rhs=h2[:, kc, sl], start=(kc == 0), stop=(kc == 15)),
                              [wux, hdeps[kc]], [pu], inc=(kc == 15))
                    s_, a_ = sT[tt], aT[tt]
                    act(cx, s_[:], pg[:, :], AF.Silu, [pg], [s_])
                    cx.op("dve", lambda e_, a_=a_, s_=s_, pu=pu: e_.tensor_tensor(out=a_[:], in0=pu[:, :], in1=s_[:], op=ALU.mult), [pu, s_], [a_])
                    cx.op("pool", lambda e_, a_=a_, at=at, f4=f4, sl=sl, cb=cb: e_.tensor_tensor(out=at[:, f4, sl], in0=a_[:], in1=cb[:, sl], op=ALU.mult), [a_, cb], [at])
                if n + 2 < len(chunks):
                    e3, hf3, f43 = chunks[n + 2]
                    load_gu(e3, hf3 * 4 + f43, n + 2)
            wdx = wd[ui % 2]
            for dc in range(16):
                for tt in range(2):
                    sl = slice(tt * 512, (tt + 1) * 512)
                    p = ps[4 + (pi % 2)]
                    pi += 1
                    for f4 in range(4):
                        cx.op("pe", lambda e_, p=p, f4=f4, dc=dc, sl=sl, wdx=wdx, at=at: e_.matmul(p[:, :], lhsT=wdx[:, f4, dc * 128:(dc + 1) * 128], rhs=at[:, f4, sl], start=(f4 == 0), stop=(f4 == 3)),
                              [wdx, at], [p], inc=(f4 == 3))
                    cx.op("dve", lambda e_, p=p, dc=dc, sl=sl: e_.scalar_tensor_tensor(out=xt[:, dc, sl], in0=p[:, :], scalar=vt[:, 64 + dc:65 + dc], in1=xt[:, dc, sl], op0=ALU.mult, op1=ALU.add),
                          [p, vt, xdeps[dc]], [xdeps[dc]])
        for dc in range(16):
            cx.dma("sp" if dc % 2 else "act", xo_d[dc], xt[:, dc, :], xdeps[dc], False)
        cx.finish()
    return nc


_C_CACHE = {}


def _wlayout(wmat):
    K_, N_ = wmat.shape
    return np.ascontiguousarray(wmat.reshape(K_ // 128, 128, N_ // 128, 128).transpose(2, 1, 0, 3)).reshape(N_ // 128, 128, (K_ // 128) * 128)


def run_C(x_tok, o_sbT, o_fox, y_ssm, gatesT, mod_l, g_ffn_l, g_ssm_l, wb_sb, wb_fox, wb_ssm, w_out_l, w_router, b_router, wg_l, wu_l, wd_l):
    if "nc" not in _C_CACHE:
        _C_CACHE["nc"] = build_C()
    nc = _C_CACHE["nc"]
    T = TPC
    vec = np.zeros((128, 128), np.float32)
    col = lambda v: v.reshape(-1, 128).T
    vec[:, 0:16] = col(g_ffn_l)
    vec[:, 16:32] = col(mod_l[4 * D:5 * D])
    vec[:, 32:48] = col(mod_l[3 * D:4 * D])
    vec[:, 48:64] = col(mod_l[2 * D:3 * D])
    vec[:, 64:80] = col(mod_l[5 * D:6 * D])
    vec[:, 80:88] = col(g_ssm_l)
    cst = np.zeros((128, 384), np.float32)
    cst[:, 0:128] = 1.0
    cst[:, 128:256] = np.eye(128, dtype=np.float32)
    cst[:, 256:384] = np.tile(b_router[None, :], (128, 8))
    selE = np.zeros((16, 16 * 128), np.float32)
    for e in range(16):
        selE[e, e * 128:(e + 1) * 128] = 1.0
    wbr = _wlayout(np.concatenate([wb_sb, wb_fox, wb_ssm], axis=0))
    wo = _wlayout(w_out_l)
    wr = np.ascontiguousarray(w_router.reshape(16, 128, 16).transpose(1, 0, 2)).reshape(128, 256)
    lay = lambda w: np.ascontiguousarray(w.reshape(NEXP, 16, 128, 8, 128).transpose(0, 3, 2, 1, 4)).reshape(NEXP, 8, 128, 16 * 128)
    wg = lay(wg_l)
    wu = lay(wu_l)
    wd = np.ascontiguousarray(wd_l).reshape(NEXP, 2, 4, 128, D)
    in_maps = []
    for c in range(NCORES):
        ts = slice(c * T, (c + 1) * T)
        in_maps.append({
            "xT": np.ascontiguousarray(x_tok[ts].T).reshape(16, 128, T),
            "yT": np.ascontiguousarray(y_ssm[ts].T).reshape(8, 128, T),
            "osbT": np.ascontiguousarray(o_sbT[:, ts]).reshape(4, 128, T),
            "ofxT": np.ascontiguousarray(o_fox[ts].T).reshape(4, 128, T),
            "gates": np.ascontiguousarray(gatesT[:, ts]).reshape(48, 128, T),
            "vec": vec, "cst": cst, "selE": selE, "wbr": wbr, "wo": wo, "wr": wr, "wg": wg, "wu": wu, "wd": wd,
        })
    res = run_bass_kernel_spmd(nc, in_maps, core_ids=list(range(NCORES)))
    xo = np.zeros((S, D), np.float32)
    for c in range(NCORES):
        xo[c * T:(c + 1) * T] = np.asarray(res.results[c]["xo"]).reshape(D, T).T
    return xo


def kernel(x, c, w_ada, b_ada, g_norm_mix, w_in, b_fgate, g_q_fox, g_k_fox, conv_w, conv_b, dt_bias, a_log, d_skip,
           g_ssm_norm, w_branch_sb, w_branch_fox, w_branch_ssm, w_out, g_norm_ffn, w_router, b_router, w_e_gate, w_e_up, w_e_down):
    f = lambda a: np.asarray(a, dtype=np.float32)
    x_tok = f(x)[0]
    mod = run_M(f(c), f(w_ada), f(b_ada))
    for l in range(2):
        ob, of = run_A(x_tok, f(w_in[l]), mod[l], f(g_norm_mix[l]), f(b_fgate[l]), f(g_q_fox[l]), f(g_k_fox[l]), f(dt_bias[l]))
        o_sbT, o_fox, y_ssm = run_B(ob, of, f(conv_w[l]), f(conv_b[l]), f(a_log[l]), f(d_skip[l]))
        x_tok = run_C(x_tok, o_sbT, o_fox, y_ssm, ob[4096:], mod[l], f(g_norm_ffn[l]), f(g_ssm_norm[l]), f(w_branch_sb[l]), f(w_branch_fox[l]),
                      f(w_branch_ssm[l]), f(w_out[l]), f(w_router), f(b_router), f(w_e_gate[l]), f(w_e_up[l]), f(w_e_down[l]))
    return x_tok[None].astype(np.float32)
```
